# Optimizing a Trainium2 kernel written in Bass

```python
import math, functools
import jax
import jax.numpy as jnp
from jax import lax
import numpy as np

D_MODEL = 2048
BATCH = 4
SEQ = 4096
DEPTH = 2

GRID_W = 64
CTX_LEN = 256
N_GROUPS = 4
MIX_W = D_MODEL
GROUP_W = MIX_W // N_GROUPS
N_HEADS = 4
HEAD_DIM = GROUP_W // N_HEADS
KV_HEADS = N_HEADS // 2
CHUNK = 64
Q_BLOCK = 128
CONV_K = 5
ROPE_BASE = 10000.0
NORM_EPS = 1e-6
F32 = jnp.float32

IN_LAYOUT = (
    ('m_q', GROUP_W), ('m_k', GROUP_W), ('m_v', GROUP_W), ('m_o', GROUP_W), ('m_z', GROUP_W),
    ('m_i', 2 * N_HEADS), ('m_f', 2 * N_HEADS),
    ('r_q', GROUP_W), ('r_k', GROUP_W), ('r_v', GROUP_W), ('r_z', GROUP_W),
    ('a_q', GROUP_W), ('a_k', KV_HEADS * HEAD_DIM), ('a_v', KV_HEADS * HEAD_DIM), ('a_z', GROUP_W),
    ('d_qkv', 3 * GROUP_W), ('d_z', GROUP_W), ('d_a', 2 * N_HEADS), ('d_b', 2 * N_HEADS),
)
IN_W = sum(size for _, size in IN_LAYOUT)

kernel_name = 'hybrid_mlstm_retention_gqa_deltanet_dit'


def _rms_norm(x, w):
    xf = x.astype(F32)
    y = xf * lax.rsqrt(jnp.mean(xf * xf, axis=-1, keepdims=True) + NORM_EPS)
    return (y * w.astype(F32)).astype(x.dtype)


def _l2_norm(t):
    tf = t.astype(F32)
    return (tf * lax.rsqrt(jnp.sum(tf * tf, axis=-1, keepdims=True) + NORM_EPS)).astype(t.dtype)


def _head_rms(y, w):
    b, n, _ = y.shape
    yf = y.astype(F32).reshape(b, n, -1, HEAD_DIM)
    yf = yf * lax.rsqrt(jnp.mean(yf * yf, axis=-1, keepdims=True) + NORM_EPS)
    return (yf.reshape(b, n, -1) * w.astype(F32)).astype(y.dtype)


def _heads(t):
    b, n, _ = t.shape
    return t.reshape(b, n, -1, HEAD_DIM).transpose(0, 2, 1, 3)


def _unheads(t):
    b, h, n, dh = t.shape
    return t.transpose(0, 2, 1, 3).reshape(b, n, h * dh)


def _gate_heads(t):
    return jnp.swapaxes(t, 1, 2).astype(F32)


def _split_cols(u):
    parts, start = {}, 0
    for name, size in IN_LAYOUT:
        parts[name] = u[..., start:start + size]
        start += size
    return parts


def _axial_rope_tables(rows):
    row = jnp.repeat(jnp.arange(rows), GRID_W)
    col = jnp.tile(jnp.arange(GRID_W), rows)
    n_freq = HEAD_DIM // 4
    inv_freq = ROPE_BASE ** (-jnp.arange(n_freq, dtype=F32) / n_freq)
    ang = jnp.stack([row[:, None] * inv_freq, col[:, None] * inv_freq], axis=1)
    return jnp.cos(ang), jnp.sin(ang)


def _apply_rope(t, cos, sin):
    b, h, n, dh = t.shape
    tr = t.astype(F32).reshape(b, h, n, 2, 2, dh // 4)
    t1, t2 = tr[..., 0, :], tr[..., 1, :]
    out = jnp.stack([t1 * cos - t2 * sin, t2 * cos + t1 * sin], axis=-2)
    return out.reshape(b, h, n, dh).astype(t.dtype)


def _centred_dwconv(t, w):
    pad = CONV_K // 2
    n = t.shape[1]
    tp = jnp.pad(t, ((0, 0), (pad, pad), (0, 0)))
    out = tp[:, 0:n] * w[0]
    for j in range(1, CONV_K):
        out = out + tp[:, j:j + n] * w[j]
    return out


def _to_chunks(t):
    b, h, n = t.shape[:3]
    return t.astype(F32).reshape(b, h, n // CHUNK, CHUNK, *t.shape[3:])


def _scan_major(t):
    return jnp.moveaxis(t, 2, 0)


def _from_chunks(oc):
    nc, b, h, cl, d = oc.shape
    return jnp.moveaxis(oc, 0, 2).reshape(b, h, nc * cl, d)


def _mlstm_scan(q, k, v, log_i, log_f, state):
    tril = jnp.tril(jnp.ones((CHUNK, CHUNK), dtype=bool))
    xs = tuple(_scan_major(_to_chunks(t)) for t in (q, k, v, log_i, log_f))

    def step(carry, inp):
        c_mat, n_vec, m = carry
        qj, kj, vj, li, lf = inp
        b_cum = jnp.cumsum(lf, axis=-1)
        d_mat = jnp.where(tril, b_cum[..., :, None] - b_cum[..., None, :] + li[..., None, :], -jnp.inf)
        inter = m[..., None] + b_cum
        m_t = jnp.maximum(inter, jnp.max(d_mat, axis=-1))
        w_inter = jnp.exp(inter - m_t)
        s = jnp.einsum('bhtd,bhsd->bhts', qj, kj) * jnp.exp(d_mat - m_t[..., None])
        num = w_inter[..., None] * jnp.einsum('bhtd,bhde->bhte', qj, c_mat) + jnp.einsum('bhts,bhse->bhte', s, vj)
        den = w_inter * jnp.einsum('bhtd,bhd->bht', qj, n_vec) + jnp.sum(s, axis=-1)
        h_out = num / jnp.maximum(jnp.abs(den), jnp.exp(-m_t))[..., None]
        b_end = b_cum[..., -1]
        dec = b_end[..., None] - b_cum + li
        m_new = jnp.maximum(m + b_end, jnp.max(dec, axis=-1))
        w_state = jnp.exp(dec - m_new[..., None])
        carry_decay = jnp.exp(m + b_end - m_new)
        c_new = carry_decay[..., None, None] * c_mat + jnp.einsum('bhs,bhsd,bhse->bhde', w_state, kj, vj)
        n_new = carry_decay[..., None] * n_vec + jnp.einsum('bhs,bhsd->bhd', w_state, kj)
        return (c_new, n_new, m_new), h_out

    state, hc = lax.scan(step, state, xs)
    return _from_chunks(hc), state


def _retention_scan(q, k, v, state, log_gamma):
    lg = log_gamma.astype(F32)[:, None]
    idx = jnp.arange(CHUNK, dtype=F32)
    rel = idx[:, None] - idx[None, :]
    decay_mask = jnp.where(rel >= 0, jnp.exp(lg[..., None] * jnp.maximum(rel, 0.0)), 0.0)
    q_decay = jnp.exp(lg * (idx + 1.0))
    k_decay = jnp.exp(lg * (CHUNK - 1.0 - idx))
    chunk_decay = jnp.exp(lg[:, 0] * CHUNK)
    xs = tuple(_scan_major(_to_chunks(t)) for t in (q, k, v))

    def step(r_mat, inp):
        qj, kj, vj = inp
        s = jnp.einsum('bhtd,bhsd->bhts', qj, kj) * decay_mask
        o = jnp.einsum('bhts,bhse->bhte', s, vj) + q_decay[..., None] * jnp.einsum('bhtd,bhde->bhte', qj, r_mat)
        r_new = chunk_decay[:, None, None] * r_mat + jnp.einsum('bhsd,hs,bhse->bhde', kj, k_decay, vj)
        return r_new, o

    state, oc = lax.scan(step, state, xs)
    return _from_chunks(oc), state


def _gated_delta_scan(q, k, v, log_a, beta, state):
    qc, kc, vc = _to_chunks(q), _to_chunks(k), _to_chunks(v)
    g = jnp.cumsum(_to_chunks(log_a), axis=-1)
    bc = _to_chunks(beta)
    incl = jnp.tril(jnp.ones((CHUNK, CHUNK), dtype=bool))
    strict = jnp.tril(jnp.ones((CHUNK, CHUNK), dtype=bool), -1)
    decay = jnp.where(incl, jnp.exp(jnp.where(incl, g[..., :, None] - g[..., None, :], 0.0)), 0.0)
    kk = jnp.einsum('bhntd,bhnsd->bhnts', kc, kc)
    lower = jnp.where(strict, bc[..., :, None] * decay * kk, 0.0) + jnp.eye(CHUNK, dtype=F32)
    eg = jnp.exp(g)
    w_v = lax.linalg.triangular_solve(lower, bc[..., None] * vc, left_side=True, lower=True, unit_diagonal=True)
    w_k = lax.linalg.triangular_solve(lower, (bc * eg)[..., None] * kc, left_side=True, lower=True, unit_diagonal=True)
    qk = jnp.einsum('bhntd,bhnsd->bhnts', qc, kc) * decay
    k_end = jnp.exp(g[..., -1:] - g)
    g_end = eg[..., -1]
    xs = tuple(_scan_major(t) for t in (qc, kc, w_v, w_k, qk, eg, k_end, g_end))

    def step(s_mat, inp):
        qj, kj, wvj, wkj, qkj, egj, kej, gej = inp
        u = wvj - jnp.einsum('bhtd,bhde->bhte', wkj, s_mat)
        o = egj[..., None] * jnp.einsum('bhtd,bhde->bhte', qj, s_mat) + jnp.einsum('bhts,bhse->bhte', qkj, u)
        s_new = gej[..., None, None] * s_mat + jnp.einsum('bhs,bhsd,bhse->bhde', kej, kj, u)
        return s_new, o

    state, oc = lax.scan(step, state, xs)
    return _from_chunks(oc), state


def _flip_time(ts):
    return tuple(jnp.flip(t, axis=2) for t in ts)


def _bidir(scan_fns, ctx_dirs, lat_dirs, init):
    y_lat, y_ctx = None, None
    for d in range(2):
        ci, li = ctx_dirs[d], lat_dirs[d]
        if d == 1:
            ci, li = _flip_time(ci), _flip_time(li)
        h_c, state = scan_fns[d](*ci, init)
        h_l, _ = scan_fns[d](*li, state)
        if d == 1:
            h_c, h_l = jnp.flip(h_c, axis=2), jnp.flip(h_l, axis=2)
        y_lat = h_l if y_lat is None else y_lat + h_l
        y_ctx = h_c if y_ctx is None else y_ctx + h_c
    return y_lat, y_ctx


def _mlstm_branch(uc, ul, i_bias, f_bias):
    def prep(u):
        return (_heads(u['m_q']) * (HEAD_DIM ** -0.5), _heads(u['m_k']), _heads(u['m_v']))

    def gates(u, d):
        sl = slice(d * N_HEADS, (d + 1) * N_HEADS)
        log_i = _gate_heads(u['m_i'][..., sl]) + i_bias[d].astype(F32)[:, None]
        log_f = jax.nn.log_sigmoid(_gate_heads(u['m_f'][..., sl]) + f_bias[d].astype(F32)[:, None])
        return (log_i, log_f)

    b = ul['m_q'].shape[0]
    init = (jnp.zeros((b, N_HEADS, HEAD_DIM, HEAD_DIM), F32), jnp.zeros((b, N_HEADS, HEAD_DIM), F32),
            jnp.zeros((b, N_HEADS), F32))
    pc, pl = prep(uc), prep(ul)
    h_l, h_c = _bidir([_mlstm_scan, _mlstm_scan], [pc + gates(uc, d) for d in range(2)],
                      [pl + gates(ul, d) for d in range(2)], init)
    dt = ul['m_q'].dtype
    y_l = jax.nn.sigmoid(ul['m_o']) * _unheads(h_l).astype(dt)
    y_c = jax.nn.sigmoid(uc['m_o']) * _unheads(h_c).astype(dt)
    return y_l, y_c


def _retention_branch(uc, ul, log_gamma, rope):
    cos, sin = rope
    scale = HEAD_DIM ** -0.5
    pc = (_heads(uc['r_q']), _heads(uc['r_k']) * scale, _heads(uc['r_v']))
    pl = (_apply_rope(_heads(ul['r_q']), cos, sin), _apply_rope(_heads(ul['r_k']), cos, sin) * scale, _heads(ul['r_v']))
    b = ul['r_q'].shape[0]
    init = jnp.zeros((b, N_HEADS, HEAD_DIM, HEAD_DIM), F32)
    fns = [functools.partial(_retention_scan, log_gamma=log_gamma[d]) for d in range(2)]
    h_l, h_c = _bidir(fns, [pc, pc], [pl, pl], init)
    dt = ul['r_q'].dtype
    return _unheads(h_l).astype(dt), _unheads(h_c).astype(dt)


def _attention_branch(uc, ul, q_norm_w, k_norm_w, rope, need_ctx):
    cos, sin = rope
    scale = HEAD_DIM ** -0.5

    def prep(u):
        return (_rms_norm(_heads(u['a_q']), q_norm_w), _rms_norm(_heads(u['a_k']), k_norm_w), _heads(u['a_v']))

    q_l, k_l, v_l = prep(ul)
    q_l, k_l = _apply_rope(q_l, cos, sin), _apply_rope(k_l, cos, sin)
    q_c, k_c, v_c = prep(uc)
    b, _, n, dh = q_l.shape
    groups = N_HEADS // KV_HEADS
    keys = jnp.concatenate([k_c, k_l], axis=2)
    vals = jnp.concatenate([v_c, v_l], axis=2)

    def attend(q_blk, k_all, v_all):
        s = jnp.einsum('bkgqd,bksd->bkgqs', q_blk, k_all).astype(F32) * scale
        p = jax.nn.softmax(s, axis=-1).astype(v_all.dtype)
        return jnp.einsum('bkgqs,bksd->bkgqd', p, v_all)

    n_blk = n // Q_BLOCK
    q_blocks = q_l.reshape(b, KV_HEADS, groups, n_blk, Q_BLOCK, dh).transpose(3, 0, 1, 2, 4, 5)
    o = lax.map(lambda qb: attend(qb, keys, vals), q_blocks)
    y_l = _unheads(o.transpose(1, 2, 3, 0, 4, 5).reshape(b, N_HEADS, n, dh))
    y_c = None
    if need_ctx:
        n_c = q_c.shape[2]
        o_c = attend(q_c.reshape(b, KV_HEADS, groups, n_c, dh), k_c, v_c)
        y_c = _unheads(o_c.reshape(b, N_HEADS, n_c, dh))
    return y_l, y_c


def _deltanet_branch(uc, ul, conv_w, a_log, dt_bias):
    def prep(u):
        qkv = jax.nn.silu(_centred_dwconv(u['d_qkv'], conv_w))
        q, k, v = jnp.split(qkv, 3, axis=-1)
        return (_l2_norm(_heads(q)) * (HEAD_DIM ** -0.5), _l2_norm(_heads(k)), _heads(v))

    def gates(u, d):
        sl = slice(d * N_HEADS, (d + 1) * N_HEADS)
        dt_soft = jax.nn.softplus(_gate_heads(u['d_a'][..., sl]) + dt_bias[d].astype(F32)[:, None])
        log_a = -jnp.exp(a_log[d].astype(F32))[:, None] * dt_soft
        beta = jax.nn.sigmoid(_gate_heads(u['d_b'][..., sl]))
        return (log_a, beta)

    b = ul['d_qkv'].shape[0]
    init = jnp.zeros((b, N_HEADS, HEAD_DIM, HEAD_DIM), F32)
    pc, pl = prep(uc), prep(ul)
    h_l, h_c = _bidir([_gated_delta_scan, _gated_delta_scan], [pc + gates(uc, d) for d in range(2)],
                      [pl + gates(ul, d) for d in range(2)], init)
    dt = ul['d_qkv'].dtype
    return _unheads(h_l).astype(dt), _unheads(h_c).astype(dt)


def _merge(u, y_m, y_r, y_a, y_d, head_norm_w):
    return jnp.concatenate([
        _head_rms(y_m, head_norm_w[:GROUP_W]) * jax.nn.silu(u['m_z']),
        _head_rms(y_r, head_norm_w[GROUP_W:2 * GROUP_W]) * jax.nn.silu(u['r_z']),
        y_a * jax.nn.silu(u['a_z']),
        _head_rms(y_d, head_norm_w[2 * GROUP_W:]) * jax.nn.silu(u['d_z']),
    ], axis=-1)


def _layer(x, xc, c_silu, cc_silu, ada_w, ada_b, pre_w, post_w, w_in, w_out, m_ib, m_fb, r_lg,
           qn_w, kn_w, conv_w, a_log, dt_bias, hn_w, rope, need_ctx):
    mod_l = (c_silu @ ada_w + ada_b)[:, None, :]
    mod_c = cc_silu @ ada_w + ada_b
    sh_l, sc_l, gt_l = jnp.split(mod_l, 3, axis=-1)
    sh_c, sc_c, gt_c = jnp.split(mod_c, 3, axis=-1)
    h_l = _rms_norm(x, pre_w) * (1.0 + sc_l) + sh_l
    h_c = _rms_norm(xc, pre_w) * (1.0 + sc_c) + sh_c
    ul = _split_cols(h_l @ w_in)
    uc = _split_cols(h_c @ w_in)
    m_l, m_c = _mlstm_branch(uc, ul, m_ib, m_fb)
    r_l, r_c = _retention_branch(uc, ul, r_lg, rope)
    a_l, a_c = _attention_branch(uc, ul, qn_w, kn_w, rope, need_ctx)
    d_l, d_c = _deltanet_branch(uc, ul, conv_w, a_log, dt_bias)
    x = x + gt_l * _rms_norm(_merge(ul, m_l, r_l, a_l, d_l, hn_w) @ w_out, post_w)
    if need_ctx:
        xc = xc + gt_c * _rms_norm(_merge(uc, m_c, r_c, a_c, d_c, hn_w) @ w_out, post_w)
    return x, xc


def setup_inputs(seed: int = 0) -> dict:
    key = jax.random.key(seed)
    ks = jax.random.split(key, 20)

    def nrm(k, shape, s):
        return jax.random.normal(k, shape, F32) * s

    x = nrm(ks[0], (BATCH, SEQ, D_MODEL), 1.0)
    c = nrm(ks[1], (BATCH, D_MODEL), 1.0)
    ctx = nrm(ks[2], (BATCH, CTX_LEN, D_MODEL), 1.0)
    c_ctx = nrm(ks[3], (D_MODEL,), 1.0)
    ada_w = nrm(ks[4], (DEPTH, D_MODEL, 3 * D_MODEL), D_MODEL ** -0.5)
    ada_b = nrm(ks[5], (DEPTH, 3 * D_MODEL), 0.02)
    pre_norm_w = 1.0 + nrm(ks[6], (DEPTH, D_MODEL), 0.05)
    post_norm_w = 1.0 + nrm(ks[7], (DEPTH, D_MODEL), 0.05)
    w_in = nrm(ks[8], (DEPTH, D_MODEL, IN_W), D_MODEL ** -0.5)
    w_out = nrm(ks[9], (DEPTH, MIX_W, D_MODEL), MIX_W ** -0.5)
    mlstm_i_bias = nrm(ks[10], (DEPTH, 2, N_HEADS), 0.1)
    mlstm_f_bias = jnp.linspace(3.0, 6.0, N_HEADS, dtype=F32) + nrm(ks[11], (DEPTH, 2, N_HEADS), 0.1)
    base_lg = jnp.log(1.0 - 2.0 ** (-5.0 - jnp.arange(N_HEADS, dtype=F32)))
    ret_log_gamma = base_lg * jnp.exp(nrm(ks[12], (DEPTH, 2, N_HEADS), 0.1))
    attn_q_norm_w = 1.0 + nrm(ks[13], (DEPTH, HEAD_DIM), 0.05)
    attn_k_norm_w = 1.0 + nrm(ks[14], (DEPTH, HEAD_DIM), 0.05)
    dn_conv_w = nrm(ks[15], (DEPTH, CONV_K, 3 * GROUP_W), CONV_K ** -0.5)
    dn_a_log = jnp.log(jax.random.uniform(ks[16], (DEPTH, 2, N_HEADS), F32, 1.0, 16.0))
    dt0 = jnp.exp(jax.random.uniform(ks[17], (DEPTH, 2, N_HEADS), F32, math.log(1e-3), math.log(1e-1)))
    dn_dt_bias = dt0 + jnp.log(-jnp.expm1(-dt0))
    head_norm_w = 1.0 + nrm(ks[18], (DEPTH, 3 * GROUP_W), 0.05)
    return {'x': x, 'c': c, 'ctx': ctx, 'c_ctx': c_ctx, 'ada_w': ada_w, 'ada_b': ada_b,
            'pre_norm_w': pre_norm_w, 'post_norm_w': post_norm_w, 'w_in': w_in, 'w_out': w_out,
            'mlstm_i_bias': mlstm_i_bias, 'mlstm_f_bias': mlstm_f_bias, 'ret_log_gamma': ret_log_gamma,
            'attn_q_norm_w': attn_q_norm_w, 'attn_k_norm_w': attn_k_norm_w, 'dn_conv_w': dn_conv_w,
            'dn_a_log': dn_a_log, 'dn_dt_bias': dn_dt_bias, 'head_norm_w': head_norm_w}


def reference(x, c, ctx, c_ctx, ada_w, ada_b, pre_norm_w, post_norm_w, w_in, w_out, mlstm_i_bias,
              mlstm_f_bias, ret_log_gamma, attn_q_norm_w, attn_k_norm_w, dn_conv_w, dn_a_log,
              dn_dt_bias, head_norm_w):
    n_tokens = x.shape[1]
    rows = n_tokens // GRID_W
    rope = _axial_rope_tables(rows)
    c_silu = jax.nn.silu(c)
    cc_silu = jax.nn.silu(c_ctx)
    xc = ctx
    for layer in range(DEPTH):
        x, xc = _layer(x, xc, c_silu, cc_silu, ada_w[layer], ada_b[layer], pre_norm_w[layer],
                       post_norm_w[layer], w_in[layer], w_out[layer], mlstm_i_bias[layer],
                       mlstm_f_bias[layer], ret_log_gamma[layer], attn_q_norm_w[layer],
                       attn_k_norm_w[layer], dn_conv_w[layer], dn_a_log[layer], dn_dt_bias[layer],
                       head_norm_w[layer], rope, layer < DEPTH - 1)
    return x
```

```python
import contextlib
import math
import numpy as np
import ml_dtypes
import concourse.bass as bass
import concourse.mybir as mybir
from concourse.bass_utils import run_bass_kernel_spmd

F32 = mybir.dt.float32
BF16 = mybir.dt.bfloat16
AF = mybir.ActivationFunctionType
ALU = mybir.AluOpType
AX = mybir.AxisListType

EPOCH = 30000
DMA_RING = 8
BIG = 30000.0
D = 2048
KC = 16
EPS = 1e-6


class Buf:
    __slots__ = ("name", "w", "r")

    def __init__(self, name=""):
        self.name = name
        self.w = None
        self.r = []


class Sched:
    def __init__(self, nc, same_engine_sync=True):
        self.nc = nc
        self.same = same_engine_sync
        self.streams = {e: [] for e in ("pe", "act", "dve", "pool", "sp")}
        self.cnt = {e: 0 for e in self.streams}
        self.dcnt = {q: 0 for q in self.streams}
        self.known = {e: {} for e in self.streams}
        self.semkeys = set()

    def _tok_sem(self, tok):
        if tok[0] == "c":
            _, e, n = tok
            return ("c", e, (n - 1) // EPOCH), (n - 1) % EPOCH + 1
        _, q, i = tok
        return ("d", q, i % DMA_RING), 16 * (i // DMA_RING + 1)

    def _need(self, eng, tok, waits):
        if tok is None:
            return
        if tok[0] == "c" and tok[1] == eng and (eng == "pe" or not self.same):
            return
        key, val = self._tok_sem(tok)
        if self.known[eng].get(key, 0) >= val:
            return
        if waits.get(key, 0) < val:
            waits[key] = val

    def _deps(self, eng, reads, writes):
        waits = {}
        for b in reads:
            self._need(eng, b.w, waits)
        for b in writes:
            self._need(eng, b.w, waits)
            for t in b.r:
                if t[0] == "c" and t[1] == eng:
                    continue
                self._need(eng, t, waits)
        for k, v in waits.items():
            self.known[eng][k] = v
            self.semkeys.add(k)
        return waits

    def _commit(self, tok, reads, writes):
        for b in reads:
            b.r.append(tok)
        for b in writes:
            b.w = tok
            b.r = []

    def op(self, eng, fn, reads=(), writes=()):
        waits = self._deps(eng, reads, writes)
        self.cnt[eng] += 1
        tok = ("c", eng, self.cnt[eng])
        key, _ = self._tok_sem(tok)
        self.semkeys.add(key)
        self.streams[eng].append((waits, fn, key, 1))
        self._commit(tok, reads, writes)
        return tok

    def dma(self, q, fn, reads=(), writes=()):
        waits = self._deps(q, reads, writes)
        i = self.dcnt[q]
        self.dcnt[q] += 1
        tok = ("d", q, i)
        key, val = self._tok_sem(tok)
        self.semkeys.add(key)
        if i >= DMA_RING:
            pkey, pval = self._tok_sem(("d", q, i - DMA_RING))
            if self.known[q].get(pkey, 0) < pval:
                waits[pkey] = max(waits.get(pkey, 0), pval)
                self.known[q][pkey] = pval
        self.streams[q].append((waits, fn, key, 16))
        self._commit(tok, reads, writes)
        return tok

    def barrier(self):
        toks = []
        for e in self.streams:
            if self.cnt[e] > 0:
                toks.append(("c", e, self.cnt[e]))
            for i in range(max(0, self.dcnt[e] - DMA_RING), self.dcnt[e]):
                toks.append(("d", e, i))
        for e in self.streams:
            waits = {}
            for t in toks:
                if t[0] == "c" and t[1] == e:
                    continue
                self._need(e, t, waits)
            for k, v in waits.items():
                self.known[e][k] = v
                self.semkeys.add(k)
            self.streams[e].append((waits, None, None, 0))

    def emit(self):
        nc = self.nc
        keys = sorted(self.semkeys)
        with contextlib.ExitStack() as es:
            sems = {}
            for k in keys:
                sems[k] = es.enter_context(nc.semaphore("s_%s_%s_%d" % k))
            block = es.enter_context(nc.Block())

            def run(engname):
                def body(eng):
                    for waits, fn, key, inc in self.streams[engname]:
                        for k, v in waits.items():
                            eng.wait_ge(sems[k], v)
                        if fn is not None:
                            fn(eng).then_inc(sems[key], inc)
                return body
            block.tensor(run("pe"))
            block.scalar(run("act"))
            block.vector(run("dve"))
            block.gpsimd(run("pool"))
            block.sync(run("sp"))


_UID = [0]


def _uname(name):
    _UID[0] += 1
    return "%s_u%d" % (name, _UID[0])


class TPool:
    def __init__(self, es, nc, name, shape, dtype, n, psum=False):
        self.tiles = []
        for i in range(n):
            mk = nc.psum_tensor if psum else nc.sbuf_tensor
            t = es.enter_context(mk(_uname("%s%d" % (name, i)), shape, dtype))
            self.tiles.append((t, Buf("%s%d" % (name, i))))
        self.i = 0

    def next(self):
        t = self.tiles[self.i % len(self.tiles)]
        self.i += 1
        return t


OFF = dict(m_q=0, m_k=512, m_v=1024, m_o=1536, m_z=2048, r_q=2560, r_k=3072, r_v=3584, r_z=4096,
           a_q=4608, a_k=5120, a_v=5376, a_z=5632, d_q=6144, d_k=6656, d_v=7168, d_z=7680)
UW = 8192


def build_program(NCT, NLT, DEPTH, debug=False):
    NT = NCT + NLT
    T = NT * 128
    nc = bass.Bass("TRN2", target_bir_lowering=False)
    S = Sched(nc)

    def din(name, shape, dt=F32):
        return nc.dram_tensor(name, list(shape), dt, kind="ExternalInput").ap()

    def dscr(name, shape, dt=F32):
        return nc.dram_tensor(name, list(shape), dt, kind="Internal").ap()

    x_in = din("x", [NLT * 128, D])
    ctx_in = din("ctx", [NCT * 128, D])
    cvec = din("cvec", [32, 128])
    ada_w = din("ada_w", [DEPTH, D, 3 * D])
    ada_b = din("ada_b", [DEPTH, 3 * D])
    pre_w = din("pre_w", [DEPTH, D])
    post_w = din("post_w", [DEPTH, D])
    w_in = din("w_in", [DEPTH, D, 8224])
    w_out = din("w_out", [DEPTH, D, D])
    mib = din("mib", [DEPTH, 8])
    mfb = din("mfb", [DEPTH, 8])
    rlg = din("rlg", [DEPTH, 8])
    qnw = din("qnw", [DEPTH, 128])
    knw = din("knw", [DEPTH, 128])
    convw = din("convw", [DEPTH, 5, 1536])
    alog = din("alog", [DEPTH, 8])
    dtb = din("dtb", [DEPTH, 8])
    hnw = din("hnw", [DEPTH, 1536])
    c_ident = din("c_ident", [128, 128])
    c_ones = din("c_ones", [128, 128])
    c_tri = din("c_tri", [2, 128, 512])
    c_mbig = din("c_mbig", [2, 128, 512])
    c_blk = din("c_blk", [128, 128])
    c_chm = din("c_chm", [128, 8])
    c_offd = din("c_offd", [128, 512])
    c_sel = din("c_sel", [2, 2, 128])
    ropec = din("ropec", [NLT * 128, 128])
    ropes = din("ropes", [NLT * 128, 128])
    y_out = nc.dram_tensor("y", [NLT * 128, D], F32, kind="ExternalOutput").ap()

    X1 = dscr("X1", [T, D])
    U = dscr("U", [T, UW], BF16)
    UG = dscr("UG", [T, 32])
    OM = [dscr("OM%d" % d, [T, 512]) for d in range(2)]
    OR = [dscr("OR%d" % d, [T, 512]) for d in range(2)]
    OD = [dscr("OD%d" % d, [T, 512]) for d in range(2)]
    OA = dscr("OA", [T, 512])
    DQ = dscr("DQ", [T, 1536], BF16)
    dbg = {}
    if debug:
        dbg["U"] = nc.dram_tensor("dbgU", [T, UW], BF16, kind="ExternalOutput").ap()
        dbg["UG"] = nc.dram_tensor("dbgUG", [T, 32], F32, kind="ExternalOutput").ap()
        for nm in ("OM0", "OM1", "OR0", "OR1", "OD0", "OD1", "OA"):
            dbg[nm] = nc.dram_tensor("dbg" + nm, [T, 512], F32, kind="ExternalOutput").ap()
        dbg["X1"] = nc.dram_tensor("dbgX1", [T, D], F32, kind="ExternalOutput").ap()

    dbufs = {}

    def DB(name, n):
        k = (name, n)
        if k not in dbufs:
            dbufs[k] = Buf("%s_%d" % k)
        return dbufs[k]

    def mm(out, lhsT, rhs, start, stop, R, W):
        S.op("pe", lambda e: e.matmul(out, lhsT=lhsT, rhs=rhs, start=start, stop=stop), R, W)

    def act(out, in_, func, R, W, bias=None, scale=None, accum=None, eng="act"):
        kw = {}
        if bias is not None:
            kw["bias"] = bias
        if scale is not None:
            kw["scale"] = scale
        if accum is not None:
            kw["accum_out"] = accum
        S.op("act", lambda e: e.activation(out=out, in_=in_, func=func, **kw), R, W)

    def tt(eng, out, in0, in1, op, R, W):
        S.op(eng, lambda e: e.tensor_tensor(out=out, in0=in0, in1=in1, op=op), R, W)

    def ts(eng, out, in0, s1, s2, op0, op1, R, W):
        if s2 is None:
            S.op(eng, lambda e: e.tensor_scalar(out=out, in0=in0, scalar1=s1, scalar2=None, op0=op0), R, W)
        else:
            S.op(eng, lambda e: e.tensor_scalar(out=out, in0=in0, scalar1=s1, scalar2=s2, op0=op0, op1=op1), R, W)

    def stt(out, in0, scalar, in1, op0, op1, R, W):
        S.op("dve", lambda e: e.scalar_tensor_tensor(out=out, in0=in0, scalar=scalar, in1=in1, op0=op0, op1=op1), R, W)

    def cp(eng, out, in_, R, W):
        if eng == "act":
            S.op("act", lambda e: e.activation(out=out, in_=in_, func=AF.Copy), R, W)
        else:
            S.op(eng, lambda e: e.tensor_copy(out=out, in_=in_), R, W)

    def red(out, in_, R, W):
        S.op("dve", lambda e: e.tensor_reduce(out=out, in_=in_, axis=AX.X, op=ALU.add), R, W)

    def recip(out, in_, R, W):
        S.op("dve", lambda e: e.reciprocal(out=out, in_=in_), R, W)

    def mset(eng, ap, v, W):
        S.op(eng, lambda e: e.memset(ap, v), (), W)

    dq = [0]

    def dma(out, in_, R, W, q=None):
        if q is None:
            q = "sp"
            dq[0] += 1
        S.dma(q, lambda e: e.dma_start(out=out, in_=in_), R, W)

    def rsqrt_inplace(ap, tmp_scale, add, R):
        ts("dve", ap, ap, tmp_scale, add, ALU.mult, ALU.add, R, R)
        act(ap, ap, AF.Sqrt, R, R)
        recip(ap, ap, R, R)

    with contextlib.ExitStack() as es0:
        def sb(name, shape, dt=F32, es=es0):
            return es.enter_context(nc.sbuf_tensor(_uname(name), list(shape), dt)), Buf(name)

        ident, b_ident = sb("ident", [128, 128])
        ones, b_ones = sb("ones", [128, 128])
        tri = [sb("tri%d" % d, [128, 512]) for d in range(2)]
        mbig = [sb("mbig%d" % d, [128, 512]) for d in range(2)]
        blk, b_blk = sb("blk", [128, 128])
        chm, b_chm = sb("chm", [128, 8])
        offd, b_offd = sb("offd", [128, 512])
        dma(ident[:], c_ident[:, :], [], [b_ident])
        dma(ones[:], c_ones[:, :], [], [b_ones])
        for d in range(2):
            dma(tri[d][0][:], c_tri[d], [], [tri[d][1]])
            dma(mbig[d][0][:], c_mbig[d], [], [mbig[d][1]])
        dma(blk[:], c_blk[:, :], [], [b_blk])
        dma(chm[:], c_chm[:, :], [], [b_chm])
        dma(offd[:], c_offd[:, :], [], [b_offd])

        scT, b_scT = sb("scT", [128, DEPTH * 2 * 16])
        shT, b_shT = sb("shT", [128, DEPTH * 2 * 16])
        GPW = dscr("GPW", [DEPTH * 2, D])

        def scT_ap(l, which, k):
            i = (l * 2 + which) * 16 + k
            return scT[:, i:i + 1]

        def shT_ap(l, which, k):
            i = (l * 2 + which) * 16 + k
            return shT[:, i:i + 1]

        with contextlib.ExitStack() as es:
            pa, b_pa = sb("p0a", [64, 128], es=es)
            pb, b_pb = sb("p0b", [DEPTH * 48, 128], es=es)
            paT, b_paT = sb("p0aT", [128, 64], es=es)
            pbT, b_pbT = sb("p0bT", [128, DEPTH * 48], es=es)
            cs2, b_cs2 = sb("p0cs2", [128, 16, 2], es=es)
            modT, b_modT = sb("p0modT", [128, 48, 2], es=es)
            grow, b_grow = sb("p0grow", [2, D], es=es)
            brow, b_brow = sb("p0brow", [2, D], es=es)
            prow, b_prow = sb("p0prow", [2, D], es=es)
            slabs = TPool(es, nc, "p0slab", [128, 16, 512], F32, 2)
            ps_t = es.enter_context(nc.psum_tensor(_uname("p0ps_t"), [128, 512], F32)); b_ps_t = Buf()
            ps_m = es.enter_context(nc.psum_tensor(_uname("p0ps_m"), [128, 512], F32)); b_ps_m = Buf()
            ps_g = TPool(es, nc, "p0ps_g", [128, 512], F32, 2, psum=True)

            dma(pa[0:32, :], cvec[:, :], [], [b_pa])
            dma(pa[32:32 + DEPTH * 16, :], pre_w.rearrange("l (k p) -> (l k) p", p=128), [], [b_pa])
            dma(pb[:], ada_b.rearrange("l (k p) -> (l k) p", p=128), [], [b_pb])
            act(pa[0:32, :], pa[0:32, :], AF.Silu, [b_pa], [b_pa])
            S.op("pe", lambda e: e.transpose(out=ps_t[:, 0:64], in_=pa[:, :], identity=ident[0:64, 0:64]), [b_pa, b_ident], [b_ps_t])
            cp("dve", paT[:], ps_t[:, 0:64], [b_ps_t], [b_paT])
            S.op("pe", lambda e: e.transpose(out=ps_t[:, 128:128 + DEPTH * 48], in_=pb[:, :], identity=ident[0:DEPTH * 48, 0:DEPTH * 48]), [b_pb, b_ident], [b_ps_t])
            cp("dve", pbT[:], ps_t[:, 128:128 + DEPTH * 48], [b_ps_t], [b_pbT])
            cp("dve", cs2[:, :, 0], paT[:, 0:16], [b_paT], [b_cs2])
            cp("dve", cs2[:, :, 1], paT[:, 16:32], [b_paT], [b_cs2])
            for l in range(DEPTH):
                dma(brow[0:1, :], ada_b[l:l + 1, 2 * D:3 * D], [], [b_brow])
                dma(brow[1:2, :], ada_b[l:l + 1, 2 * D:3 * D], [], [b_brow])
                dma(prow[0:1, :], post_w[l:l + 1, :], [], [b_prow])
                dma(prow[1:2, :], post_w[l:l + 1, :], [], [b_prow])
                awv = ada_w[l].rearrange("(k p) n -> p k n", p=128)
                for sl in range(12):
                    slab, b_slab = slabs.next()
                    for kk in range(0, 16, 4):
                        dma(slab[:, kk:kk + 4, :], awv[:, kk:kk + 4, sl * 512:(sl + 1) * 512], [], [b_slab])
                    for j in range(4):
                        cb = sl * 4 + j
                        for k in range(KC):
                            mm(ps_m[:, cb * 2:cb * 2 + 2], slab[:, k, j * 128:(j + 1) * 128], cs2[:, k, :],
                               k == 0, k == KC - 1, [b_slab, b_cs2], [b_ps_m])
                    if sl >= 8:
                        pg, b_pg = ps_g.next()
                        for k in range(KC):
                            mm(pg[0:2, :], cs2[:, k, :], slab[:, k, :], k == 0, k == KC - 1, [b_slab, b_cs2], [b_pg])
                        cc = (sl - 8) * 512
                        tt("dve", grow[:, cc:cc + 512], pg[0:2, :], brow[:, cc:cc + 512], ALU.add, [b_pg, b_brow], [b_grow])
                cp("dve", modT[:].rearrange("p a b -> p (a b)"), ps_m[:, 0:96], [b_ps_m], [b_modT])
                tt("dve", grow[:], grow[:], prow[:], ALU.mult, [b_grow, b_prow], [b_grow])
                dma(GPW[l * 2:l * 2 + 2, :], grow[:], [b_grow], [DB("GPW", l)])
                for which in range(2):
                    i0 = (l * 2 + which) * 16
                    tt("dve", shT[:, i0:i0 + 16], modT[:, 0:16, which], pbT[:, l * 48:l * 48 + 16], ALU.add, [b_modT, b_pbT], [b_shT])
                    tt("dve", scT[:, i0:i0 + 16], modT[:, 16:32, which], pbT[:, l * 48 + 16:l * 48 + 32], ALU.add, [b_modT, b_pbT], [b_scT])
                    stt(scT[:, i0:i0 + 16], scT[:, i0:i0 + 16], 1.0, paT[:, 32 + l * 16:48 + l * 16], ALU.add, ALU.mult, [b_scT, b_paT], [b_scT])
            S.barrier()

        for l in range(DEPTH):
            last = (l == DEPTH - 1)

            def xsrc(n):
                if l == 0:
                    return (ctx_in[n * 128:(n + 1) * 128, :] if n < NCT else x_in[(n - NCT) * 128:(n - NCT + 1) * 128, :]), []
                return X1[n * 128:(n + 1) * 128, :], [DB("X1", n)]

            with contextlib.ExitStack() as es:
                TBMAX = (NT + 1) // 2
                hT = [sb("a_hT%d" % i, [128, KC, 128], BF16, es=es) for i in range(TBMAX)]
                xt_p = TPool(es, nc, "a_x", [128, D], F32, 2)
                st_p = TPool(es, nc, "a_st", [128, 4], F32, 2)
                wst_p = TPool(es, nc, "a_wst", [128, KC, 512], F32, 2)
                wbf_p = TPool(es, nc, "a_wbf", [128, KC, 512], BF16, 2)
                ub_p = TPool(es, nc, "a_ub", [128, 512], BF16, 3)
                ug_p = TPool(es, nc, "a_ug", [128, 32], F32, 2)
                ps_p = TPool(es, nc, "a_ps", [128, 512], F32, 6, psum=True)
                wv = w_in[l].rearrange("(k p) n -> p k n", p=128)
                blocks = [list(range(0, TBMAX)), list(range(TBMAX, NT))]
                for tb in blocks:
                    if not tb:
                        continue
                    for i, n in enumerate(tb):
                        which = 1 if n < NCT else 0
                        xt, b_xt = xt_p.next()
                        st, b_st = st_p.next()
                        src, sr = xsrc(n)
                        dma(xt[:], src, sr, [b_xt])
                        ps, b_ps = ps_p.next()
                        for c4 in range(4):
                            act(ps[:, :], xt[:, c4 * 512:(c4 + 1) * 512], AF.Square, [b_xt], [b_ps, b_st], accum=st[:, c4:c4 + 1])
                        red(st[:, 0:1], st[:, 0:4], [b_st], [b_st])
                        rsqrt_inplace(st[:, 0:1], 1.0 / D, EPS, [b_st])
                        ts("dve", xt[:], xt[:], st[:, 0:1], None, ALU.mult, None, [b_xt, b_st], [b_xt])
                        ht, b_ht = hT[i]
                        for k4 in range(4):
                            ps, b_ps = ps_p.next()
                            for j in range(4):
                                k = k4 * 4 + j
                                S.op("pe", (lambda k=k, j=j, ps=ps, xt=xt: lambda e: e.transpose(out=ps[:, j * 128:(j + 1) * 128], in_=xt[:, k * 128:(k + 1) * 128], identity=ident[:]))(),
                                     [b_xt, b_ident], [b_ps])
                            for j in range(4):
                                k = k4 * 4 + j
                                act(ht[:, k, :], ps[:, j * 128:(j + 1) * 128], AF.Identity, [b_ps, b_scT, b_shT], [b_ht],
                                    bias=shT_ap(l, which, k), scale=scT_ap(l, which, k))
                    def load_w(cb):
                        wst, b_wst = wst_p.next()
                        wbf, b_wbf = wbf_p.next()
                        if cb < 16:
                            o0 = cb * 512 + (16 if cb >= 5 else 0)
                            wdt = 512
                            for kk in range(0, 16, 4):
                                dma(wst[:, kk:kk + 4, :], wv[:, kk:kk + 4, o0:o0 + 512], [], [b_wst])
                        else:
                            wdt = 32
                            dma(wst[:, :, 0:16], wv[:, :, 2560:2576], [], [b_wst])
                            dma(wst[:, :, 16:32], wv[:, :, 8208:8224], [], [b_wst])
                        cp("dve", wbf[:, 0:8, 0:wdt], wst[:, 0:8, 0:wdt], [b_wst], [b_wbf])
                        cp("dve", wbf[:, 8:16, 0:wdt], wst[:, 8:16, 0:wdt], [b_wst], [b_wbf])
                        return wbf, b_wbf, wdt
                    nxt_w = load_w(0)
                    for cb in range(17):
                        wbf, b_wbf, wdt = nxt_w
                        if cb + 1 < 17:
                            nxt_w = load_w(cb + 1)
                        for i, n in enumerate(tb):
                            ht, b_ht = hT[i]
                            ps, b_ps = ps_p.next()
                            for k in range(KC):
                                mm(ps[:, 0:wdt], ht[:, k, :], wbf[:, k, 0:wdt], k == 0, k == KC - 1, [b_ht, b_wbf], [b_ps])
                            if cb < 16:
                                ub, b_ub = ub_p.next()
                                cp("act", ub[:, :], ps[:, 0:512], [b_ps], [b_ub])
                                dma(U[n * 128:(n + 1) * 128, cb * 512:(cb + 1) * 512], ub[:, :], [b_ub], [DB("U", n)], q="act")
                            else:
                                ug, b_ug = ug_p.next()
                                cp("act", ug[:, :], ps[:, 0:32], [b_ps], [b_ug])
                                dma(UG[n * 128:(n + 1) * 128, :], ug[:, :], [b_ug], [DB("UG", n)], q="act")
                S.barrier()

            if debug and l == debug - 1:
                pass

            ctx_needed = not last
            fwd_order = list(range(NT))
            bwd_order = list(range(NCT - 1, -1, -1)) + list(range(NT - 1, NCT - 1, -1))

            with contextlib.ExitStack() as es:
                QT, b_QT = sb("at_QT", [128, 4, T], BF16, es=es)
                KT, b_KT = sb("at_KT", [128, 2, T], BF16, es=es)
                V1, b_V1 = sb("at_V1", [128, NT, 2, 132], BF16, es=es)
                nwq, b_nwq = sb("at_nwq", [128, 128], es=es)
                nwk, b_nwk = sb("at_nwk", [128, 128], es=es)
                st_p = TPool(es, nc, "at_st", [128, 8], F32, 4)
                pt_p = TPool(es, nc, "at_pt", [128, 512], BF16, 3)
                ob_p = TPool(es, nc, "at_ob", [128, 128], F32, 3)
                ps_p = TPool(es, nc, "at_ps", [128, 512], F32, 4, psum=True)
                acc_p = TPool(es, nc, "at_acc", [128, 512], F32, 4, psum=True)
                dma(nwq[:], qnw[l:l + 1, :].partition_broadcast(128), [], [b_nwq])
                dma(nwk[:], knw[l:l + 1, :].partition_broadcast(128), [], [b_nwk])
                mset("dve", V1[:, :, :, 128:129], 1.0, [b_V1])
                ts("dve", nwq[:], nwq[:], 128.0 ** -0.5, None, ALU.mult, None, [b_nwq], [b_nwq])

                with contextlib.ExitStack() as es2:
                    raw_p = TPool(es2, nc, "at_raw", [128, 1024], BF16, 3)
                    q_p = TPool(es2, nc, "at_q", [128, 768], F32, 3)
                    q2_p = TPool(es2, nc, "at_q2", [128, 768], F32, 3)
                    sw_p = TPool(es2, nc, "at_sw", [128, 768], F32, 3)
                    rc_p = TPool(es2, nc, "at_rc", [128, 128], F32, 3)
                    rs_p = TPool(es2, nc, "at_rs", [128, 128], F32, 3)

                    def a1_tile(n):
                        raw, b_raw = raw_p.next()
                        dma(raw[:, 0:1024], U[n * 128:(n + 1) * 128, OFF["a_q"]:OFF["a_q"] + 1024], [DB("U", n)], [b_raw])
                        q, b_q = q_p.next()
                        q2, b_q2 = q2_p.next()
                        st, b_st = st_p.next()
                        cp("pool", q[:, :], raw[:, 0:768], [b_raw], [b_q])
                        cp("pool", V1[:, n, :, 0:128], raw[:, 768:1024].rearrange("p (h d) -> p h d", h=2), [b_raw], [b_V1])
                        tt("dve", q2[:, :], q[:, :], q[:, :], ALU.mult, [b_q], [b_q2])
                        red(st[:, 0:6], q2[:, :].rearrange("p (h d) -> p h d", h=6), [b_q2], [b_st])
                        yield
                        rsqrt_inplace(st[:, 0:6], 1.0 / 128, EPS, [b_st])
                        yield
                        q3 = q[:, :].rearrange("p (h d) -> p h d", h=6)
                        tt("dve", q3, q3, st[:, 0:6].unsqueeze(2).to_broadcast([128, 6, 128]), ALU.mult, [b_q, b_st], [b_q])
                        tt("pool", q3[:, 0:4, :], q3[:, 0:4, :], nwq[:, :].unsqueeze(1).to_broadcast([128, 4, 128]), ALU.mult, [b_q, b_nwq], [b_q])
                        tt("pool", q3[:, 4:6, :], q3[:, 4:6, :], nwk[:, :].unsqueeze(1).to_broadcast([128, 2, 128]), ALU.mult, [b_q, b_nwk], [b_q])
                        yield
                        if n >= NCT:
                            rc, b_rc = rc_p.next()
                            rs, b_rs = rs_p.next()
                            sw, b_sw = sw_p.next()
                            r0 = (n - NCT) * 128
                            dma(rc[:], ropec[r0:r0 + 128, :], [], [b_rc])
                            dma(rs[:], ropes[r0:r0 + 128, :], [], [b_rs])
                            q4 = q[:, :].rearrange("p (g two f) -> p g two f", two=2, f=32)
                            s4 = sw[:, :].rearrange("p (g two f) -> p g two f", two=2, f=32)
                            cp("pool", s4[:, :, 0, :], q4[:, :, 1, :], [b_q], [b_sw])
                            cp("pool", s4[:, :, 1, :], q4[:, :, 0, :], [b_q], [b_sw])
                            s3 = sw[:, :].rearrange("p (h d) -> p h d", h=6)
                            tt("dve", q3, q3, rc[:, :].unsqueeze(1).to_broadcast([128, 6, 128]), ALU.mult, [b_q, b_rc], [b_q])
                            tt("pool", s3, s3, rs[:, :].unsqueeze(1).to_broadcast([128, 6, 128]), ALU.mult, [b_sw, b_rs], [b_sw])
                            yield
                            tt("dve", q3, q3, s3, ALU.add, [b_q, b_sw], [b_q])
                            yield
                        for half in range(2):
                            ps, b_ps = ps_p.next()
                            nh = 4 if half == 0 else 2
                            for j in range(nh):
                                hh = half * 4 + j
                                S.op("pe", (lambda ps=ps, j=j, q=q, hh=hh: lambda e: e.transpose(out=ps[:, j * 128:(j + 1) * 128], in_=q[:, hh * 128:(hh + 1) * 128], identity=ident[:]))(),
                                     [b_q, b_ident], [b_ps])
                            if half == 0:
                                cp("act", QT[:, :, n * 128:(n + 1) * 128], ps[:, 0:512].rearrange("p (h t) -> p h t", h=4), [b_ps], [b_QT])
                            else:
                                cp("act", KT[:, :, n * 128:(n + 1) * 128], ps[:, 0:256].rearrange("p (h t) -> p h t", h=2), [b_ps], [b_KT])

                    pend = list(range(NT))
                    act_g = []
                    while pend or act_g:
                        while pend and len(act_g) < 3:
                            act_g.append(a1_tile(pend.pop(0)))
                        alive = []
                        for g_ in act_g:
                            try:
                                next(g_)
                                alive.append(g_)
                            except StopIteration:
                                pass
                        act_g = alive
                    S.barrier()

                def attend(qtiles, ktiles):
                    for h in range(4):
                        kvh = h // 2
                        for qb0 in range(0, len(qtiles), 4):
                            qts = qtiles[qb0:qb0 + 4]
                            nq = len(qts)
                            q0 = qts[0] * 128
                            accs = [acc_p.next() for _ in range(nq)]
                            for si, s in enumerate(ktiles):
                                ps, b_ps = ps_p.next()
                                mm(ps[:, 0:nq * 128], KT[:, kvh, s * 128:(s + 1) * 128], QT[:, h, q0:q0 + nq * 128], True, True, [b_KT, b_QT], [b_ps])
                                pt, b_pt = pt_p.next()
                                act(pt[:, 0:nq * 128], ps[:, 0:nq * 128], AF.Exp, [b_ps], [b_pt])
                                for j in range(nq):
                                    mm(accs[j][0][:, 0:129], pt[:, j * 128:(j + 1) * 128], V1[:, s, kvh, 0:129], si == 0, si == len(ktiles) - 1,
                                       [b_pt, b_V1], [accs[j][1]])
                                yield
                            for j in range(nq):
                                ac, b_ac = accs[j]
                                ob, b_ob = ob_p.next()
                                st, b_st = st_p.next()
                                act(st[:, 0:1], ac[:, 128:129], AF.Ln, [b_ac], [b_st])
                                act(st[:, 0:1], st[:, 0:1], AF.Exp, [b_st], [b_st], scale=-1.0)
                                act(ob[:, :], ac[:, 0:128], AF.Identity, [b_ac, b_st], [b_ob], scale=st[:, 0:1])
                                n = qts[j]
                                dma(OA[n * 128:(n + 1) * 128, h * 128:(h + 1) * 128], ob[:, :], [b_ob], [DB("OA", n)], q="act")

                def gen_attn():
                    yield from attend(list(range(NCT, NT)), list(range(NT)))
                    if ctx_needed:
                        yield from attend(list(range(NCT)), list(range(NCT)))

                with contextlib.ExitStack() as es3:
                    cw = [sb("p_cw%d" % j, [128, 1536], es=es3) for j in range(5)]
                    for j in range(5):
                        dma(cw[j][0][:], convw[l, j:j + 1, :].partition_broadcast(128), [], [cw[j][1]])

                    def gen_prep(tiles, gi):
                        sh_p = TPool(es3, nc, "p%d_sh" % gi, [128, 1536], BF16, 2)
                        acc_p2 = TPool(es3, nc, "p%d_acc" % gi, [128, 1536], F32, 1)
                        tmp_p = TPool(es3, nc, "p%d_tmp" % gi, [128, 1536], F32, 2)
                        pst_p = TPool(es3, nc, "p%d_st" % gi, [128, 16], F32, 2)
                        pob_p = TPool(es3, nc, "p%d_ob" % gi, [128, 1536], BF16, 1)
                        rraw_p = TPool(es3, nc, "p%d_rraw" % gi, [128, 1024], BF16, 2)
                        rq_p = TPool(es3, nc, "p%d_rq" % gi, [128, 1024], F32, 1)
                        rsw_p = TPool(es3, nc, "p%d_rsw" % gi, [128, 1024], F32, 1)
                        prc_p = TPool(es3, nc, "p%d_rc" % gi, [128, 128], F32, 1)
                        prs_p = TPool(es3, nc, "p%d_rs" % gi, [128, 128], F32, 1)
                        yield
                        for n in tiles:
                            rows = slice(n * 128, (n + 1) * 128)
                            seg0, seg1 = (0, NCT * 128) if n < NCT else (NCT * 128, T)
                            acc, b_acc = acc_p2.next()
                            for j in range(5):
                                sh, b_sh = sh_p.next()
                                lo = n * 128 + j - 2
                                hi = lo + 128
                                clo, chi = max(lo, seg0), min(hi, seg1)
                                if clo > lo or chi < hi:
                                    mset("pool", sh[:, :], 0.0, [b_sh])
                                dma(sh[clo - lo:chi - lo, :], U[clo:chi, OFF["d_q"]:OFF["d_q"] + 1536],
                                    [DB("U", m) for m in range(max(0, n - 1), min(NT, n + 2))], [b_sh])
                                if j == 0:
                                    tt("pool", acc[:, :], sh[:, :], cw[0][0][:, :], ALU.mult, [b_sh, cw[0][1]], [b_acc])
                                else:
                                    tmp, b_tmp = tmp_p.next()
                                    tt("pool" if j % 2 else "dve", tmp[:, :], sh[:, :], cw[j][0][:, :], ALU.mult, [b_sh, cw[j][1]], [b_tmp])
                                    yield
                                    tt("dve" if j % 2 else "pool", acc[:, :], acc[:, :], tmp[:, :], ALU.add, [b_acc, b_tmp], [b_acc])
                                yield
                            for _w in range(8):
                                yield
                            act(acc[:, :], acc[:, :], AF.Silu, [b_acc], [b_acc])
                            yield
                            tmp, b_tmp = tmp_p.next()
                            st, b_st = pst_p.next()
                            tt("pool", tmp[:, 0:1024], acc[:, 0:1024], acc[:, 0:1024], ALU.mult, [b_acc], [b_tmp])
                            yield
                            red(st[:, 0:8], tmp[:, 0:1024].rearrange("p (h d) -> p h d", h=8), [b_tmp], [b_st])
                            yield
                            ts("dve", st[:, 0:8], st[:, 0:8], 1.0, EPS, ALU.mult, ALU.add, [b_st], [b_st])
                            for _w in range(8):
                                yield
                            act(st[:, 0:8], st[:, 0:8], AF.Sqrt, [b_st], [b_st])
                            yield
                            recip(st[:, 0:8], st[:, 0:8], [b_st], [b_st])
                            yield
                            ts("dve", st[:, 0:4], st[:, 0:4], 128.0 ** -0.5, None, ALU.mult, None, [b_st], [b_st])
                            yield
                            qk3 = acc[:, 0:1024].rearrange("p (h d) -> p h d", h=8)
                            tt("dve", qk3, qk3, st[:, 0:8].unsqueeze(2).to_broadcast([128, 8, 128]), ALU.mult, [b_acc, b_st], [b_acc])
                            yield
                            ob, b_ob = pob_p.next()
                            cp("dve", ob[:, :], acc[:, :], [b_acc], [b_ob])
                            yield
                            dma(DQ[rows, :], ob[:, :], [b_ob], [DB("DQ", n)], q="pool")
                            yield
                            rr, b_rr = rraw_p.next()
                            rq, b_rq = rq_p.next()
                            dma(rr[:, :], U[rows, OFF["r_q"]:OFF["r_q"] + 1024], [DB("U", n)], [b_rr])
                            cp("pool", rq[:, :], rr[:, :], [b_rr], [b_rq])
                            yield
                            if n >= NCT:
                                rc, b_rc = prc_p.next()
                                rs, b_rs = prs_p.next()
                                sw, b_sw = rsw_p.next()
                                r0 = (n - NCT) * 128
                                dma(rc[:], ropec[r0:r0 + 128, :], [], [b_rc])
                                dma(rs[:], ropes[r0:r0 + 128, :], [], [b_rs])
                                q4 = rq[:, :].rearrange("p (g two f) -> p g two f", two=2, f=32)
                                s4 = sw[:, :].rearrange("p (g two f) -> p g two f", two=2, f=32)
                                cp("pool", s4[:, :, 0, :], q4[:, :, 1, :], [b_rq], [b_sw])
                                cp("pool", s4[:, :, 1, :], q4[:, :, 0, :], [b_rq], [b_sw])
                                q3 = rq[:, :].rearrange("p (h d) -> p h d", h=8)
                                s3 = sw[:, :].rearrange("p (h d) -> p h d", h=8)
                                tt("dve", q3, q3, rc[:, :].unsqueeze(1).to_broadcast([128, 8, 128]), ALU.mult, [b_rq, b_rc], [b_rq])
                                tt("pool", s3, s3, rs[:, :].unsqueeze(1).to_broadcast([128, 8, 128]), ALU.mult, [b_sw, b_rs], [b_sw])
                                yield
                                tt("dve", q3, q3, s3, ALU.add, [b_rq, b_sw], [b_rq])
                                yield
                            ts("dve", rq[:, 512:1024], rq[:, 512:1024], 128.0 ** -0.5, None, ALU.mult, None, [b_rq], [b_rq])
                            yield
                            rr2, b_rr2 = rraw_p.next()
                            cp("dve", rr2[:, :], rq[:, :], [b_rq], [b_rr2])
                            yield
                            dma(U[rows, OFF["r_q"]:OFF["r_q"] + 1024], rr2[:, :], [b_rr2], [DB("Ur", n)], q="pool")
                            yield

                    gens = [gen_attn(), gen_prep(list(range(0, NT, 2)), 0), gen_prep(list(range(1, NT, 2)), 1)]
                    while gens:
                        alive = []
                        for g_ in gens:
                            try:
                                next(g_)
                                alive.append(g_)
                            except StopIteration:
                                pass
                        gens = alive
                    S.barrier()

            for mixer in ("m", "r", "d"):
                with contextlib.ExitStack() as es:
                    VW = 129 if mixer == "m" else 128
                    OUT = dict(m=OM, r=OR, d=OD)[mixer]
                    ugt, b_ugt = sb("g_ug", [128, NT, 32], es=es)
                    for n in range(NT):
                        dma(ugt[:, n, :], UG[n * 128:(n + 1) * 128, :], [DB("UG", n)], [b_ugt])
                    prm, b_prm = sb("g_prm", [128, 32], es=es)
                    G = {}
                    NG = NT * 4
                    for d in range(2):
                        for nm in ("lf", "li", "g", "gend", "eg", "kend", "rowp", "beta", "beg"):
                            G[(nm, d)] = sb("g_%s%d" % (nm, d), [128, NT, 4], es=es)
                        G[("gam", d)] = sb("g_gam%d" % d, [128, NT, 8], es=es)
                    gtmp, b_gtmp = sb("g_tmp", [128, NT, 8], es=es)
                    psA = TPool(es, nc, "r_psA", [128, 512], F32, 4, psum=True)
                    psB = TPool(es, nc, "r_psB", [128, 512], F32, 4, psum=True)
                    gps = psA
                    if mixer == "m":
                        dma(prm[:, 0:8], mib[l:l + 1, :].partition_broadcast(128), [], [b_prm])
                        dma(prm[:, 8:16], mfb[l:l + 1, :].partition_broadcast(128), [], [b_prm])
                    elif mixer == "r":
                        dma(prm[:, 0:8], rlg[l:l + 1, :].partition_broadcast(128), [], [b_prm])
                    else:
                        dma(prm[:, 0:8], alog[l:l + 1, :].partition_broadcast(128), [], [b_prm])
                        dma(prm[:, 8:16], dtb[l:l + 1, :].partition_broadcast(128), [], [b_prm])
                        act(prm[:, 0:8], prm[:, 0:8], AF.Exp, [b_prm], [b_prm])
                    for d in range(2):
                        lf, b_lf = G[("lf", d)]
                        li, b_li = G[("li", d)]
                        g, b_g = G[("g", d)]
                        gend, b_gend = G[("gend", d)]
                        eg, b_eg = G[("eg", d)]
                        kend, b_kend = G[("kend", d)]
                        rowp, b_rowp = G[("rowp", d)]
                        beta, b_beta = G[("beta", d)]
                        beg, b_beg = G[("beg", d)]
                        gam, b_gam = G[("gam", d)]

                        def prmb(c0):
                            return prm[:, c0 + d * 4:c0 + d * 4 + 4].unsqueeze(1).to_broadcast([128, NT, 4])
                        if mixer == "m":
                            tt("dve", li[:], ugt[:, :, d * 4:d * 4 + 4], prmb(0), ALU.add, [b_ugt, b_prm], [b_li])
                            tt("dve", lf[:], ugt[:, :, 8 + d * 4:12 + d * 4], prmb(8), ALU.add, [b_ugt, b_prm], [b_lf])
                            act(lf[:], lf[:], AF.Exp, [b_lf], [b_lf], scale=-1.0)
                            act(lf[:], lf[:], AF.Ln, [b_lf], [b_lf], bias=1.0)
                            ts("dve", lf[:], lf[:], -1.0, None, ALU.mult, None, [b_lf], [b_lf])
                        elif mixer == "r":
                            mset("dve", li[:], 0.0, [b_li])
                            mset("dve", lf[:], 0.0, [b_lf])
                            tt("dve", lf[:], lf[:], prmb(0), ALU.add, [b_lf, b_prm], [b_lf])
                        else:
                            mset("dve", li[:], 0.0, [b_li])
                            tt("dve", lf[:], ugt[:, :, 16 + d * 4:20 + d * 4], prmb(8), ALU.add, [b_ugt, b_prm], [b_lf])
                            act(lf[:], lf[:], AF.Exp, [b_lf], [b_lf])
                            act(lf[:], lf[:], AF.Ln, [b_lf], [b_lf], bias=1.0)
                            tt("dve", lf[:], lf[:], prmb(0), ALU.mult, [b_lf, b_prm], [b_lf])
                            ts("dve", lf[:], lf[:], -1.0, None, ALU.mult, None, [b_lf], [b_lf])
                            act(beta[:], ugt[:, :, 24 + d * 4:28 + d * 4], AF.Exp, [b_ugt], [b_beta], scale=-1.0)
                            ts("dve", beta[:], beta[:], 1.0, None, ALU.add, None, [b_beta], [b_beta])
                            recip(beta[:], beta[:], [b_beta], [b_beta])
                        lf2 = lf[:].rearrange("p n h -> p (n h)")
                        ps, b_ps = gps.next()
                        mm(ps[:, 0:NG], tri[d][0][:, 0:128], lf2, True, True, [tri[d][1], b_lf], [b_ps])
                        cp("dve", g[:].rearrange("p n h -> p (n h)"), ps[:, 0:NG], [b_ps], [b_g])
                        ps, b_ps = gps.next()
                        mm(ps[:, 0:NG], blk[:, :], lf2, True, True, [b_blk, b_lf], [b_ps])
                        cp("dve", gend[:].rearrange("p n h -> p (n h)"), ps[:, 0:NG], [b_ps], [b_gend])
                        act(eg[:], g[:], AF.Exp, [b_g], [b_eg])
                        tt("dve", rowp[:], li[:], g[:], ALU.subtract, [b_li, b_g], [b_rowp])
                        tt("dve", kend[:], gend[:], rowp[:], ALU.add, [b_gend, b_rowp], [b_kend])
                        act(kend[:], kend[:], AF.Exp, [b_kend], [b_kend])
                        if mixer == "d":
                            tt("dve", beg[:], beta[:], eg[:], ALU.mult, [b_beta, b_eg], [b_beg])
                        g4 = gtmp[:].rearrange("p n (c h) -> p n c h", c=2)
                        for c in range(2):
                            tt("dve", g4[:, :, c, :], lf[:], chm[:, c * 4:c * 4 + 4].unsqueeze(1).to_broadcast([128, NT, 4]), ALU.mult, [b_lf, b_chm], [b_gtmp])
                        ps, b_ps = gps.next()
                        mm(ps[:, 0:NT * 8], ones[:, :], gtmp[:].rearrange("p n c -> p (n c)"), True, True, [b_ones, b_gtmp], [b_ps])
                        act(gam[:].rearrange("p n c -> p (n c)"), ps[:, 0:NT * 8], AF.Exp, [b_ps], [b_gam])

                    KSLOT = 3 if mixer == "d" else 4
                    st_p = TPool(es, nc, "r_st", [128, 16], F32, 12)
                    slots = []
                    for si in range(KSLOT):
                        Bd = {}
                        Bd["raw"] = sb("s%d_raw" % si, [128, 1536], BF16, es=es)
                        Bd["qkv"] = sb("s%d_qkv" % si, [128, 1536], F32, es=es)
                        Bd["qs"] = sb("s%d_qs" % si, [128, 512], F32, es=es)
                        Bd["ke"] = sb("s%d_ke" % si, [128, 512], BF16, es=es)
                        Bd["v1"] = sb("s%d_v1" % si, [128, 4, 132], BF16, es=es)
                        for nm in ("QT", "QtT", "KT", "QKT"):
                            Bd[nm] = sb("s%d_%s" % (si, nm), [128, 512], BF16, es=es)
                        Bd["ep"] = sb("s%d_ep" % si, [128, 512], F32, es=es)
                        Bd["DT"] = sb("s%d_DT" % si, [128, 512], F32, es=es)
                        Bd["ob"] = sb("s%d_ob" % si, [128, 512], F32, es=es)
                        if mixer == "d":
                            Bd["kb"] = sb("s%d_kb" % si, [128, 512], F32, es=es)
                            Bd["wk"] = sb("s%d_wk" % si, [128, 512], F32, es=es)
                            Bd["KbT"] = sb("s%d_KbT" % si, [128, 512], BF16, es=es)
                            Bd["WkT"] = sb("s%d_WkT" % si, [128, 512], BF16, es=es)
                            Bd["AT"] = [sb("s%d_AT%d" % (si, j), [128, 512], F32, es=es) for j in range(3)]
                            Bd["A"] = [sb("s%d_A%d" % (si, j), [128, 512], F32, es=es) for j in range(3)]
                            Bd["R"] = [sb("s%d_R%d" % (si, j), [128, 4, 256], F32, es=es) for j in range(3)]
                            Bd["U"] = [sb("s%d_U%d" % (si, j), [128, 512], BF16, es=es) for j in range(2)]
                        slots.append(Bd)
                    state = [sb("r_S%d" % d, [128, 4, 132], es=es) for d in range(2)]
                    stateb = [sb("r_Sb%d" % d, [128, 4, 132], BF16, es=es) for d in range(2)]
                    for d in range(2):
                        mset("dve", state[d][0][:], 0.0, [state[d][1]])
                        mset("dve", stateb[d][0][:], 0.0, [stateb[d][1]])

                    def transpose4(src, b_src, dst, b_dst):
                        ps, b_ps = psA.next()
                        for h in range(4):
                            S.op("pe", (lambda ps=ps, h=h, src=src: lambda e: e.transpose(out=ps[:, h * 128:(h + 1) * 128], in_=src[:, h * 128:(h + 1) * 128], identity=ident[:]))(),
                                 [b_src, b_ident], [b_ps])
                        cp("act", dst[:, :], ps[:, :], [b_ps], [b_dst])

                    def unit(n, d, Bd, kseq):
                        eg, b_eg = G[("eg", d)]
                        kend, b_kend = G[("kend", d)]
                        rowp, b_rowp = G[("rowp", d)]
                        lf, b_lf = G[("lf", d)]
                        gam, b_gam = G[("gam", d)]
                        beta, b_beta = G[("beta", d)]
                        beg, b_beg = G[("beg", d)]
                        raw, b_raw = Bd["raw"]
                        qkv, b_qkv = Bd["qkv"]
                        rows = slice(n * 128, (n + 1) * 128)
                        if mixer == "d":
                            dma(raw[:, :], DQ[rows, :], [DB("DQ", n)], [b_raw])
                        else:
                            c0 = OFF["m_q"] if mixer == "m" else OFF["r_q"]
                            dma(raw[:, :], U[rows, c0:c0 + 1536], [DB("U", n), DB("Ur", n)], [b_raw])
                        cp("pool", qkv[:, :], raw[:, :], [b_raw], [b_qkv])
                        if mixer == "m":
                            ts("dve", qkv[:, 0:512], qkv[:, 0:512], 128.0 ** -0.5, None, ALU.mult, None, [b_qkv], [b_qkv])
                        yield
                        qv = qkv[:, 0:512]
                        kv = qkv[:, 512:1024]
                        vv = qkv[:, 1024:1536]
                        h4 = lambda ap: ap.rearrange("p (h d) -> p h d", h=4)
                        bc = lambda t_: t_[:, n, :].unsqueeze(2).to_broadcast([128, 4, 128])
                        qs, b_qs = Bd["qs"]
                        ke, b_ke = Bd["ke"]
                        tt("dve", h4(qs[:, :]), h4(qv), bc(eg), ALU.mult, [b_qkv, b_eg], [b_qs])
                        tt("pool", h4(ke[:, :]), h4(kv), bc(kend), ALU.mult, [b_qkv, b_kend], [b_ke])
                        if mixer == "d":
                            kb, b_kb = Bd["kb"]
                            tt("dve", h4(kb[:, :]), h4(kv), bc(beta), ALU.mult, [b_qkv, b_beta], [b_kb])
                            R, b_R = Bd["R"][0]
                            tt("dve", R[:, :, 0:128], h4(vv), bc(beta), ALU.mult, [b_qkv, b_beta], [b_R])
                            tt("pool", R[:, :, 128:256], h4(kv), bc(beg), ALU.mult, [b_qkv, b_beg], [b_R])
                        else:
                            v1, b_v1 = Bd["v1"]
                            cp("pool", v1[:, :, 0:128], h4(vv), [b_qkv], [b_v1])
                            if mixer == "m":
                                mset("pool", v1[:, :, 128:129], 1.0, [b_v1])
                        yield
                        QTt, b_QTt = Bd["QT"]
                        QtT, b_QtT = Bd["QtT"]
                        KTt, b_KTt = Bd["KT"]
                        transpose4(qv, b_qkv, QTt, b_QTt)
                        yield
                        transpose4(kv, b_qkv, KTt, b_KTt)
                        yield
                        transpose4(qs, b_qs, QtT, b_QtT)
                        yield
                        if mixer == "d":
                            KbT, b_KbT = Bd["KbT"]
                            transpose4(kb, b_kb, KbT, b_KbT)
                            yield
                        ep, b_ep = Bd["ep"]
                        tt("dve", h4(ep[:, :]), h4(tri[d][0][:, :]), bc(lf), ALU.mult, [tri[d][1], b_lf], [b_ep])
                        psE, b_psE = psB.next()
                        mm(psE[:, :], ones[:, :], ep[:, :], True, False, [b_ones, b_ep], [b_psE])
                        mm(psE[:, :], ident[:, :], mbig[d][0][:, :], False, True, [b_ident, mbig[d][1]], [b_psE])
                        DT, b_DT = Bd["DT"]
                        for h in range(4):
                            act(DT[:, h * 128:(h + 1) * 128], psE[:, h * 128:(h + 1) * 128], AF.Exp, [b_psE, b_rowp], [b_DT], bias=rowp[:, n, h:h + 1])
                        yield
                        psQ, b_psQ = psB.next()
                        for h in range(4):
                            mm(psQ[:, h * 128:(h + 1) * 128], KTt[:, h * 128:(h + 1) * 128], QTt[:, h * 128:(h + 1) * 128], True, True, [b_KTt, b_QTt], [b_psQ])
                        QKT, b_QKT = Bd["QKT"]
                        tt("dve", QKT[:, :], psQ[:, :], DT[:, :], ALU.mult, [b_psQ, b_DT], [b_QKT])
                        yield
                        ob, b_ob = Bd["ob"]
                        St, b_St = state[d]
                        Sb, b_Sb = stateb[d]
                        if mixer == "d":
                            psK, b_psK = psB.next()
                            for h in range(4):
                                mm(psK[:, h * 128:(h + 1) * 128], KTt[:, h * 128:(h + 1) * 128], KbT[:, h * 128:(h + 1) * 128], True, True, [b_KTt, b_KbT], [b_psK])
                            ai = 0
                            AT, b_AT = Bd["AT"][0]
                            A, b_A = Bd["A"][0]
                            tt("dve", AT[:, :], psK[:, :], DT[:, :], ALU.mult, [b_psK, b_DT], [b_AT])
                            tt("pool", AT[:, :], AT[:, :], offd[:, :], ALU.mult, [b_AT, b_offd], [b_AT])
                            yield
                            psT, b_psT = psA.next()
                            for h in range(4):
                                S.op("pe", (lambda psT=psT, h=h, AT=AT: lambda e: e.transpose(out=psT[:, h * 128:(h + 1) * 128], in_=AT[:, h * 128:(h + 1) * 128], identity=ident[:]))(),
                                     [b_AT, b_ident], [b_psT])
                            cp("act", A[:, :], psT[:, :], [b_psT], [b_A])
                            yield
                            ri = 0
                            for lev in range(6):
                                p1, b_p1 = psB.next()
                                p2, b_p2 = psB.next()
                                for h in range(4):
                                    pp, b_pp = (p1, b_p1) if h < 2 else (p2, b_p2)
                                    mm(pp[:, (h % 2) * 256:(h % 2) * 256 + 256], AT[:, h * 128:(h + 1) * 128], R[:, h, :], True, True, [b_AT, b_R], [b_pp])
                                ri = (ri + 1) % 3
                                Rn, b_Rn = Bd["R"][ri]
                                op = ALU.subtract if lev == 0 else ALU.add
                                tt("dve", Rn[:, 0:2, :], R[:, 0:2, :], p1[:, :].rearrange("p (h e) -> p h e", h=2), op, [b_R, b_p1], [b_Rn])
                                tt("dve", Rn[:, 2:4, :], R[:, 2:4, :], p2[:, :].rearrange("p (h e) -> p h e", h=2), op, [b_R, b_p2], [b_Rn])
                                R, b_R = Rn, b_Rn
                                yield
                                if lev < 5:
                                    pA, b_pA = psA.next()
                                    pT, b_pT = psA.next()
                                    for h in range(4):
                                        hs = slice(h * 128, (h + 1) * 128)
                                        mm(pA[:, hs], AT[:, hs], A[:, hs], True, True, [b_AT, b_A], [b_pA])
                                        mm(pT[:, hs], A[:, hs], AT[:, hs], True, True, [b_AT, b_A], [b_pT])
                                    ai = (ai + 1) % 3
                                    A2, b_A2 = Bd["A"][ai]
                                    AT2, b_AT2 = Bd["AT"][ai]
                                    cp("act", A2[:, :], pA[:, :], [b_pA], [b_A2])
                                    cp("dve", AT2[:, :], pT[:, :], [b_pT], [b_AT2])
                                    A, b_A, AT, b_AT = A2, b_A2, AT2, b_AT2
                                    yield
                            wk, b_wk = Bd["wk"]
                            cp("pool", h4(wk[:, :]), R[:, :, 128:256], [b_R], [b_wk])
                            WkT, b_WkT = Bd["WkT"]
                            transpose4(wk, b_wk, WkT, b_WkT)
                            yield
                        while done_seq[d] < kseq:
                            yield
                        for ci, c in enumerate((0, 1) if d == 0 else (1, 0)):
                            P = slice(c * 64, c * 64 + 64)
                            if mixer == "d":
                                psU, b_psU = psB.next()
                                for h in range(4):
                                    mm(psU[:, h * 128:(h + 1) * 128], WkT[:, h * 128:(h + 1) * 128], Sb[:, h, 0:128], True, True, [b_WkT, b_Sb], [b_psU])
                                Ut, b_Ut = Bd["U"][ci]
                                tt("dve", Ut[P, :].rearrange("p (h e) -> p h e", h=4), R[P, :, 0:128], psU[P, :].rearrange("p (h e) -> p h e", h=4),
                                   ALU.subtract, [b_R, b_psU], [b_Ut])
                                rhsU = (lambda Ut=Ut, P=P: lambda h: Ut[P, h * 128:(h + 1) * 128])()
                                b_rhs = b_Ut
                                yield
                            else:
                                rhsU = (lambda v1=v1, P=P: lambda h: v1[P, h, 0:VW])()
                                b_rhs = b_v1
                            po1, b_po1 = psB.next()
                            po2, b_po2 = psB.next()
                            ps1, b_ps1 = psA.next()
                            ps2, b_ps2 = psA.next()
                            for h in range(4):
                                po, b_po = (po1, b_po1) if h < 2 else (po2, b_po2)
                                o_ap = po[:, (h % 2) * 256:(h % 2) * 256 + VW]
                                mm(o_ap, QtT[:, h * 128:(h + 1) * 128], Sb[:, h, 0:VW], True, False, [b_QtT, b_Sb], [b_po])
                                mm(o_ap, QKT[P, h * 128:(h + 1) * 128], rhsU(h), False, True, [b_QKT, b_rhs], [b_po])
                            for h in range(4):
                                pss, b_pss = (ps1, b_ps1) if h < 2 else (ps2, b_ps2)
                                mm(pss[:, (h % 2) * 256:(h % 2) * 256 + VW], ke[P, h * 128:(h + 1) * 128], rhsU(h), True, True, [b_ke, b_rhs], [b_pss])
                            for h in range(4):
                                pss, b_pss = (ps1, b_ps1) if h < 2 else (ps2, b_ps2)
                                stt(St[:, h, 0:VW], St[:, h, 0:VW], gam[:, n, c * 4 + h:c * 4 + h + 1], pss[:, (h % 2) * 256:(h % 2) * 256 + VW],
                                    ALU.mult, ALU.add, [b_St, b_gam, b_pss], [b_St])
                            cp("act", Sb[:, :, 0:VW], St[:, :, 0:VW], [b_St], [b_Sb])
                            for hp in range(2):
                                po, b_po = (po1, b_po1) if hp == 0 else (po2, b_po2)
                                po3 = po[:, :].rearrange("p (h e) -> p h e", h=2)
                                o3 = ob[:, hp * 256:(hp + 1) * 256].rearrange("p (h e) -> p h e", h=2)
                                if mixer == "m":
                                    st2, b_st2 = st_p.next()
                                    act(st2[P, 0:2], po3[P, :, 128], AF.Abs, [b_po], [b_st2])
                                    ts("dve", st2[P, 0:2], st2[P, 0:2], 1.0, None, ALU.max, None, [b_st2], [b_st2])
                                    recip(st2[P, 0:2], st2[P, 0:2], [b_st2], [b_st2])
                                    tt("dve", o3[P], po3[P, :, 0:128], st2[P, 0:2].unsqueeze(2).to_broadcast([64, 2, 128]), ALU.mult, [b_po, b_st2], [b_ob])
                                else:
                                    cp("act", o3[P], po3[P, :, 0:128], [b_po], [b_ob])
                            yield
                        done_seq[d] += 1
                        dma(OUT[d][rows, :], ob[:, :], [b_ob], [DB("O%s%d" % (mixer, d), n)], q="act")

                    done_seq = [0, 0]
                    order = []
                    for i in range(NT):
                        order.append((fwd_order[i], 0, i))
                        order.append((bwd_order[i], 1, i))
                    active = []
                    free = list(range(KSLOT))
                    nxt = 0
                    while nxt < len(order) or active:
                        while free and nxt < len(order):
                            si = free.pop(0)
                            n_, d_, k_ = order[nxt]
                            nxt += 1
                            active.append((si, unit(n_, d_, slots[si], k_)))
                        still = []
                        for si, g_ in active:
                            try:
                                next(g_)
                                still.append((si, g_))
                            except StopIteration:
                                free.append(si)
                        active = still
                    S.barrier()

            with contextlib.ExitStack() as es:
                wo, b_wo = sb("c_wo", [128, KC, D], BF16, es=es)
                hw, b_hw = sb("c_hw", [128, 1536], es=es)
                gp = [sb("c_gp%d" % w, [128, D], es=es) for w in range(2)]
                wov = w_out[l].rearrange("(k p) n -> p k n", p=128)
                with contextlib.ExitStack() as es2:
                    wst_p = TPool(es2, nc, "c_wst", [128, 4, 512], F32, 2)
                    for k4 in range(0, KC, 4):
                        for cbk in range(4):
                            wst, b_wst = wst_p.next()
                            dma(wst[:, :, :], wov[:, k4:k4 + 4, cbk * 512:(cbk + 1) * 512], [], [b_wst])
                            cp("pool" if cbk % 2 else "dve", wo[:, k4:k4 + 4, cbk * 512:(cbk + 1) * 512], wst[:, :, :], [b_wst], [b_wo])
                    dma(hw[:], hnw[l:l + 1, :].partition_broadcast(128), [], [b_hw])
                    for w in range(2):
                        dma(gp[w][0][:], GPW[l * 2 + w:l * 2 + w + 1, :].partition_broadcast(128), [DB("GPW", l)], [gp[w][1]])
                    S.barrier()
                sq_p = TPool(es, nc, "c_sq", [128, 512], F32, 3)
                st_p = TPool(es, nc, "c_st", [128, 8], F32, 6)
                ps_p = TPool(es, nc, "c_ps", [128, 512], F32, 4, psum=True)
                po_p = TPool(es, nc, "c_po", [128, 512], F32, 4, psum=True)
                cslots = []
                for si in range(2):
                    Bc = {}
                    Bc["z"] = sb("c%d_z" % si, [128, 2560], BF16, es=es)
                    Bc["zf"] = sb("c%d_zf" % si, [128, 2560], F32, es=es)
                    Bc["o"] = [sb("c%d_o%d" % (si, j), [128, 512], F32, es=es) for j in range(7)]
                    Bc["y"] = sb("c%d_y" % si, [128, D], F32, es=es)
                    Bc["yT"] = sb("c%d_yT" % si, [128, KC, 128], BF16, es=es)
                    Bc["x"] = sb("c%d_x" % si, [128, D], F32, es=es)
                    cslots.append(Bc)

                def ctile(n, Bc):
                    which = 1 if n < NCT else 0
                    rows = slice(n * 128, (n + 1) * 128)
                    z, b_z = Bc["z"]
                    zf, b_zf = Bc["zf"]
                    y, b_y = Bc["y"]
                    yT, b_yT = Bc["yT"]
                    xt, b_xt = Bc["x"]
                    dma(z[:, 0:1024], U[rows, OFF["m_o"]:OFF["m_o"] + 1024], [DB("U", n)], [b_z])
                    dma(z[:, 1024:1536], U[rows, OFF["r_z"]:OFF["r_z"] + 512], [DB("U", n)], [b_z])
                    dma(z[:, 1536:2048], U[rows, OFF["a_z"]:OFF["a_z"] + 512], [DB("U", n)], [b_z])
                    dma(z[:, 2048:2560], U[rows, OFF["d_z"]:OFF["d_z"] + 512], [DB("U", n)], [b_z])
                    obufs = {}
                    oi = 0
                    for mx, OUTS in (("m", OM), ("r", OR), ("d", OD)):
                        for d_ in range(2):
                            o_, b_o = Bc["o"][oi]
                            oi += 1
                            dma(o_[:, :], OUTS[d_][rows, :], [DB("O%s%d" % (mx, d_), n)], [b_o])
                            obufs[(mx, d_)] = (o_, b_o)
                    o_, b_o = Bc["o"][6]
                    dma(o_[:, :], OA[rows, :], [DB("OA", n)], [b_o])
                    obufs[("a", 0)] = (o_, b_o)
                    src, sr = xsrc(n)
                    dma(xt[:], src, sr, [b_xt])
                    yield
                    act(zf[:, 0:512], z[:, 0:512], AF.Sigmoid, [b_z], [b_zf])
                    act(zf[:, 512:2560], z[:, 512:2560], AF.Silu, [b_z], [b_zf])
                    yield
                    for gi, mx in enumerate(("m", "r", "a", "d")):
                        ycol = y[:, gi * 512:(gi + 1) * 512]
                        if mx == "a":
                            o0, b_o0 = obufs[("a", 0)]
                            tt("dve", ycol, o0[:, :], zf[:, 1536:2048], ALU.mult, [b_o0, b_zf], [b_y])
                            continue
                        o0, b_o0 = obufs[(mx, 0)]
                        o1, b_o1 = obufs[(mx, 1)]
                        st, b_st = st_p.next()
                        tt("pool", o0[:, :], o0[:, :], o1[:, :], ALU.add, [b_o0, b_o1], [b_o0])
                        if mx == "m":
                            tt("pool", o0[:, :], o0[:, :], zf[:, 0:512], ALU.mult, [b_o0, b_zf], [b_o0])
                        sq, b_sq = sq_p.next()
                        tt("dve", sq[:, :], o0[:, :], o0[:, :], ALU.mult, [b_o0], [b_sq])
                        red(st[:, 0:4], sq[:, :].rearrange("p (h d) -> p h d", h=4), [b_sq], [b_st])
                        yield
                        rsqrt_inplace(st[:, 0:4], 1.0 / 128, EPS, [b_st])
                        yield
                        hoff = dict(m=0, r=512, d=1024)[mx]
                        zoff = dict(m=512, r=1024, d=2048)[mx]
                        tt("dve", o0[:, :].rearrange("p (h d) -> p h d", h=4), o0[:, :].rearrange("p (h d) -> p h d", h=4),
                           st[:, 0:4].unsqueeze(2).to_broadcast([128, 4, 128]), ALU.mult, [b_o0, b_st], [b_o0])
                        tt("pool", o0[:, :], o0[:, :], hw[:, hoff:hoff + 512], ALU.mult, [b_o0, b_hw], [b_o0])
                        tt("dve", ycol, o0[:, :], zf[:, zoff:zoff + 512], ALU.mult, [b_o0, b_zf], [b_y])
                        yield
                    for k4 in range(4):
                        ps, b_ps = ps_p.next()
                        for j in range(4):
                            k = k4 * 4 + j
                            S.op("pe", (lambda ps=ps, j=j, k=k, y=y: lambda e: e.transpose(out=ps[:, j * 128:(j + 1) * 128], in_=y[:, k * 128:(k + 1) * 128], identity=ident[:]))(),
                                 [b_y, b_ident], [b_ps])
                        cp("act", yT[:, k4 * 4:k4 * 4 + 4, :], ps[:, :].rearrange("p (k t) -> p k t", k=4), [b_ps], [b_yT])
                    yield
                    pos = [po_p.next() for _ in range(4)]
                    for cbk in range(4):
                        po, b_po = pos[cbk]
                        for k in range(KC):
                            mm(po[:, :], yT[:, k, :], wo[:, k, cbk * 512:(cbk + 1) * 512], k == 0, k == KC - 1, [b_yT, b_wo], [b_po])
                    st2, b_st2 = st_p.next()
                    for cbk in range(4):
                        po, b_po = pos[cbk]
                        sq, b_sq = sq_p.next()
                        act(sq[:, :], po[:, :], AF.Square, [b_po], [b_sq, b_st2], accum=st2[:, cbk:cbk + 1])
                    red(st2[:, 4:5], st2[:, 0:4], [b_st2], [b_st2])
                    rsqrt_inplace(st2[:, 4:5], 1.0 / D, EPS, [b_st2])
                    for cbk in range(4):
                        po, b_po = pos[cbk]
                        cs = slice(cbk * 512, (cbk + 1) * 512)
                        sq, b_sq = sq_p.next()
                        stt(sq[:, :], po[:, :], st2[:, 4:5], gp[which][0][:, cs], ALU.mult, ALU.mult, [b_po, b_st2, gp[which][1]], [b_sq])
                        tt("pool", xt[:, cs], xt[:, cs], sq[:, :], ALU.add, [b_xt, b_sq], [b_xt])
                    if last:
                        dma(y_out[(n - NCT) * 128:(n - NCT + 1) * 128, :], xt[:], [b_xt], [DB("Y", n)], q="pool")
                    else:
                        dma(X1[rows, :], xt[:], [b_xt], [DB("X1", n)], q="pool")

                tiles = list(range(NCT, NT)) if last else list(range(NT))
                pend = list(tiles)
                cact = []
                cfree = [0, 1]
                tick = 0
                while pend or cact:
                    if pend and cfree and (not cact or tick >= 5):
                        si = cfree.pop(0)
                        cact.append((si, ctile(pend.pop(0), cslots[si])))
                    still = []
                    for si, g_ in cact:
                        try:
                            next(g_)
                            still.append((si, g_))
                        except StopIteration:
                            cfree.append(si)
                    cact = still
                    tick += 1
                S.barrier()

            if debug and l == 0:
                with contextlib.ExitStack() as es:
                    bt, b_bt = sb("dbg_t", [128, 2048], es=es)
                    btb, b_btb = sb("dbg_tb", [128, 2048], BF16, es=es)
                    for n in range(NT):
                        rows = slice(n * 128, (n + 1) * 128)
                        for c in range(4):
                            dma(btb[:, :], U[rows, c * 2048:(c + 1) * 2048], [], [b_btb], q="sp")
                            dma(dbg["U"][rows, c * 2048:(c + 1) * 2048], btb[:, :], [b_btb], [DB("dbgU", n)], q="sp")
                        for nm, src in (("UG", UG), ("OM0", OM[0]), ("OM1", OM[1]), ("OR0", OR[0]), ("OR1", OR[1]), ("OD0", OD[0]), ("OD1", OD[1]), ("OA", OA), ("X1", X1)):
                            w_ = src.shape[1]
                            dma(bt[:, 0:w_], src[rows, :], [], [b_bt], q="sp")
                            dma(dbg[nm][rows, :], bt[:, 0:w_], [b_bt], [DB("dbg" + nm, n)], q="sp")
                    S.barrier()

        S.barrier()
        S.emit()
    return nc


def _consts(NLT):
    idx = np.arange(128)
    same = (idx[:, None] // 64) == (idx[None, :] // 64)
    tri0 = (same & (idx[:, None] <= idx[None, :])).astype(np.float32)
    tri1 = (same & (idx[:, None] >= idx[None, :])).astype(np.float32)
    tri = np.stack([np.tile(tri0, (1, 4)), np.tile(tri1, (1, 4))]).astype(np.float32)
    mbig = ((tri - 1.0) * BIG).astype(np.float32)
    blk = same.astype(np.float32)
    chm = np.zeros((128, 8), np.float32)
    chm[:64, 0:4] = 1.0
    chm[64:, 4:8] = 1.0
    offd = np.tile(1.0 - np.eye(128, dtype=np.float32), (1, 4)).astype(np.float32)
    sel = np.zeros((2, 2, 128), np.float32)
    sel[0, 0, :] = 1.0
    sel[1, 1, :] = 1.0
    L = NLT * 128
    t = np.arange(L)
    row, col = t // 64, t % 64
    inv = (10000.0 ** (-np.arange(32, dtype=np.float32) / 32)).astype(np.float32)
    ang = np.stack([row[:, None].astype(np.float32) * inv, col[:, None].astype(np.float32) * inv], axis=1)
    cos, sin = np.cos(ang).astype(np.float32), np.sin(ang).astype(np.float32)
    cf = np.zeros((L, 2, 2, 32), np.float32)
    sf = np.zeros((L, 2, 2, 32), np.float32)
    cf[:, :, 0, :] = cos
    cf[:, :, 1, :] = cos
    sf[:, :, 0, :] = -sin
    sf[:, :, 1, :] = sin
    return dict(c_ident=np.eye(128, dtype=np.float32), c_ones=np.ones((128, 128), np.float32), c_tri=tri, c_mbig=mbig,
                c_blk=blk, c_chm=chm, c_offd=offd, c_sel=sel, ropec=cf.reshape(L, 128), ropes=sf.reshape(L, 128))


_PROG = {}


def run(inputs, NCT, NLT, DEPTH, debug=False, n_cores=8):
    key = (NCT, NLT, DEPTH, debug)
    if key not in _PROG:
        _PROG[key] = build_program(NCT, NLT, DEPTH, debug)
    nc = _PROG[key]
    f = lambda a: np.ascontiguousarray(np.asarray(a, dtype=np.float32))
    B = inputs["x"].shape[0]
    shared = dict(
        ada_w=f(inputs["ada_w"]), ada_b=f(inputs["ada_b"]), pre_w=f(inputs["pre_norm_w"]), post_w=f(inputs["post_norm_w"]),
        w_in=f(inputs["w_in"]), w_out=f(inputs["w_out"]),
        mib=f(inputs["mlstm_i_bias"]).reshape(DEPTH, 8), mfb=f(inputs["mlstm_f_bias"]).reshape(DEPTH, 8),
        rlg=f(inputs["ret_log_gamma"]).reshape(DEPTH, 8), qnw=f(inputs["attn_q_norm_w"]), knw=f(inputs["attn_k_norm_w"]),
        convw=f(inputs["dn_conv_w"]), alog=f(inputs["dn_a_log"]).reshape(DEPTH, 8), dtb=f(inputs["dn_dt_bias"]).reshape(DEPTH, 8),
        hnw=f(inputs["head_norm_w"]))
    shared.update(_consts(NLT))
    cc = f(inputs["c_ctx"]).reshape(16, 128)
    in_maps = []
    for i in range(n_cores):
        b = i % B
        m = dict(shared)
        m["x"] = f(inputs["x"][b])
        m["ctx"] = f(inputs["ctx"][b])
        m["cvec"] = np.ascontiguousarray(np.concatenate([f(inputs["c"][b]).reshape(16, 128), cc], axis=0))
        in_maps.append(m)
    res = run_bass_kernel_spmd(nc, in_maps, core_ids=list(range(n_cores)))
    return res


def kernel(**inputs):
    res = run(inputs, 2, 32, 2)
    B = inputs["x"].shape[0]
    out = np.stack([np.asarray(res.results[b]["y"], dtype=np.float32) for b in range(B)], axis=0)
    return out
```

```python
import contextlib
import math
import numpy as np
import ml_dtypes
import concourse.bass as bass
import concourse.mybir as mybir
from concourse.bass_utils import run_bass_kernel_spmd

F32 = mybir.dt.float32
BF16 = mybir.dt.bfloat16
AF = mybir.ActivationFunctionType
ALU = mybir.AluOpType
AX = mybir.AxisListType

EPOCH = 30000
DMA_RING = 8
BIG = 30000.0
D = 2048
KC = 16
EPS = 1e-6


class Buf:
    __slots__ = ("name", "w", "r")

    def __init__(self, name=""):
        self.name = name
        self.w = None
        self.r = []


class Sched:
    def __init__(self, nc, same_engine_sync=True):
        self.nc = nc
        self.same = same_engine_sync
        self.streams = {e: [] for e in ("pe", "act", "dve", "pool", "sp")}
        self.cnt = {e: 0 for e in self.streams}
        self.dcnt = {q: 0 for q in self.streams}
        self.known = {e: {} for e in self.streams}
        self.semkeys = set()

    def _tok_sem(self, tok):
        if tok[0] == "c":
            _, e, n = tok
            return ("c", e, (n - 1) // EPOCH), (n - 1) % EPOCH + 1
        _, q, i = tok
        return ("d", q, i % DMA_RING), 16 * (i // DMA_RING + 1)

    def _need(self, eng, tok, waits):
        if tok is None:
            return
        if tok[0] == "c" and tok[1] == eng and (eng == "pe" or not self.same):
            return
        key, val = self._tok_sem(tok)
        if self.known[eng].get(key, 0) >= val:
            return
        if waits.get(key, 0) < val:
            waits[key] = val

    def _deps(self, eng, reads, writes):
        waits = {}
        for b in reads:
            self._need(eng, b.w, waits)
        for b in writes:
            self._need(eng, b.w, waits)
            for t in b.r:
                if t[0] == "c" and t[1] == eng:
                    continue
                self._need(eng, t, waits)
        for k, v in waits.items():
            self.known[eng][k] = v
            self.semkeys.add(k)
        return waits

    def _commit(self, tok, reads, writes):
        for b in reads:
            b.r.append(tok)
        for b in writes:
            b.w = tok
            b.r = []

    def op(self, eng, fn, reads=(), writes=()):
        waits = self._deps(eng, reads, writes)
        self.cnt[eng] += 1
        tok = ("c", eng, self.cnt[eng])
        key, _ = self._tok_sem(tok)
        self.semkeys.add(key)
        self.streams[eng].append((waits, fn, key, 1))
        self._commit(tok, reads, writes)
        return tok

    def dma(self, q, fn, reads=(), writes=()):
        waits = self._deps(q, reads, writes)
        i = self.dcnt[q]
        self.dcnt[q] += 1
        tok = ("d", q, i)
        key, val = self._tok_sem(tok)
        self.semkeys.add(key)
        if i >= DMA_RING:
            pkey, pval = self._tok_sem(("d", q, i - DMA_RING))
            if self.known[q].get(pkey, 0) < pval:
                waits[pkey] = max(waits.get(pkey, 0), pval)
                self.known[q][pkey] = pval
        self.streams[q].append((waits, fn, key, 16))
        self._commit(tok, reads, writes)
        return tok

    def barrier(self):
        toks = []
        for e in self.streams:
            if self.cnt[e] > 0:
                toks.append(("c", e, self.cnt[e]))
            for i in range(max(0, self.dcnt[e] - DMA_RING), self.dcnt[e]):
                toks.append(("d", e, i))
        for e in self.streams:
            waits = {}
            for t in toks:
                if t[0] == "c" and t[1] == e:
                    continue
                self._need(e, t, waits)
            for k, v in waits.items():
                self.known[e][k] = v
                self.semkeys.add(k)
            self.streams[e].append((waits, None, None, 0))

    def emit(self):
        nc = self.nc
        keys = sorted(self.semkeys)
        with contextlib.ExitStack() as es:
            sems = {}
            for k in keys:
                sems[k] = es.enter_context(nc.semaphore("s_%s_%s_%d" % k))
            block = es.enter_context(nc.Block())

            def run(engname):
                def body(eng):
                    for waits, fn, key, inc in self.streams[engname]:
                        for k, v in waits.items():
                            eng.wait_ge(sems[k], v)
                        if fn is not None:
                            fn(eng).then_inc(sems[key], inc)
                return body
            block.tensor(run("pe"))
            block.scalar(run("act"))
            block.vector(run("dve"))
            block.gpsimd(run("pool"))
            block.sync(run("sp"))


_UID = [0]


def _uname(name):
    _UID[0] += 1
    return "%s_u%d" % (name, _UID[0])


class TPool:
    def __init__(self, es, nc, name, shape, dtype, n, psum=False):
        self.tiles = []
        for i in range(n):
            mk = nc.psum_tensor if psum else nc.sbuf_tensor
            t = es.enter_context(mk(_uname("%s%d" % (name, i)), shape, dtype))
            self.tiles.append((t, Buf("%s%d" % (name, i))))
        self.i = 0

    def next(self):
        t = self.tiles[self.i % len(self.tiles)]
        self.i += 1
        return t


OFF = dict(m_q=0, m_k=512, m_v=1024, m_o=1536, m_z=2048, r_q=2560, r_k=3072, r_v=3584, r_z=4096,
           a_q=4608, a_k=5120, a_v=5376, a_z=5632, d_q=6144, d_k=6656, d_v=7168, d_z=7680)
UW = 8192


def build_program(NCT, NLT, DEPTH, debug=False):
    NT = NCT + NLT
    T = NT * 128
    nc = bass.Bass("TRN2", target_bir_lowering=False)
    S = Sched(nc)

    def din(name, shape, dt=F32):
        return nc.dram_tensor(name, list(shape), dt, kind="ExternalInput").ap()

    def dscr(name, shape, dt=F32):
        return nc.dram_tensor(name, list(shape), dt, kind="Internal").ap()

    x_in = din("x", [NLT * 128, D])
    ctx_in = din("ctx", [NCT * 128, D])
    cvec = din("cvec", [32, 128])
    ada_w = din("ada_w", [DEPTH, D, 3 * D])
    ada_b = din("ada_b", [DEPTH, 3 * D])
    pre_w = din("pre_w", [DEPTH, D])
    post_w = din("post_w", [DEPTH, D])
    w_in = din("w_in", [DEPTH, D, 8224])
    w_out = din("w_out", [DEPTH, D, D])
    mib = din("mib", [DEPTH, 8])
    mfb = din("mfb", [DEPTH, 8])
    rlg = din("rlg", [DEPTH, 8])
    qnw = din("qnw", [DEPTH, 128])
    knw = din("knw", [DEPTH, 128])
    convw = din("convw", [DEPTH, 5, 1536])
    alog = din("alog", [DEPTH, 8])
    dtb = din("dtb", [DEPTH, 8])
    hnw = din("hnw", [DEPTH, 1536])
    c_ident = din("c_ident", [128, 128])
    c_ones = din("c_ones", [128, 128])
    c_tri = din("c_tri", [2, 128, 512])
    c_mbig = din("c_mbig", [2, 128, 512])
    c_blk = din("c_blk", [128, 128])
    c_chm = din("c_chm", [128, 8])
    c_offd = din("c_offd", [128, 512])
    c_sel = din("c_sel", [2, 2, 128])
    ropec = din("ropec", [NLT * 128, 128])
    ropes = din("ropes", [NLT * 128, 128])
    y_out = nc.dram_tensor("y", [NLT * 128, D], F32, kind="ExternalOutput").ap()

    X1 = dscr("X1", [T, D])
    U = dscr("U", [T, UW], BF16)
    UG = dscr("UG", [T, 32])
    OM = [dscr("OM%d" % d, [T, 512]) for d in range(2)]
    OR = [dscr("OR%d" % d, [T, 512]) for d in range(2)]
    OD = [dscr("OD%d" % d, [T, 512]) for d in range(2)]
    OA = dscr("OA", [T, 512])
    DQ = dscr("DQ", [T, 1536], BF16)
    dbg = {}
    if debug:
        dbg["U"] = nc.dram_tensor("dbgU", [T, UW], BF16, kind="ExternalOutput").ap()
        dbg["UG"] = nc.dram_tensor("dbgUG", [T, 32], F32, kind="ExternalOutput").ap()
        for nm in ("OM0", "OM1", "OR0", "OR1", "OD0", "OD1", "OA"):
            dbg[nm] = nc.dram_tensor("dbg" + nm, [T, 512], F32, kind="ExternalOutput").ap()
        dbg["X1"] = nc.dram_tensor("dbgX1", [T, D], F32, kind="ExternalOutput").ap()

    dbufs = {}

    def DB(name, n):
        k = (name, n)
        if k not in dbufs:
            dbufs[k] = Buf("%s_%d" % k)
        return dbufs[k]

    def mm(out, lhsT, rhs, start, stop, R, W):
        S.op("pe", lambda e: e.matmul(out, lhsT=lhsT, rhs=rhs, start=start, stop=stop), R, W)

    def act(out, in_, func, R, W, bias=None, scale=None, accum=None, eng="act"):
        kw = {}
        if bias is not None:
            kw["bias"] = bias
        if scale is not None:
            kw["scale"] = scale
        if accum is not None:
            kw["accum_out"] = accum
        S.op("act", lambda e: e.activation(out=out, in_=in_, func=func, **kw), R, W)

    def tt(eng, out, in0, in1, op, R, W):
        S.op(eng, lambda e: e.tensor_tensor(out=out, in0=in0, in1=in1, op=op), R, W)

    def ts(eng, out, in0, s1, s2, op0, op1, R, W):
        if s2 is None:
            S.op(eng, lambda e: e.tensor_scalar(out=out, in0=in0, scalar1=s1, scalar2=None, op0=op0), R, W)
        else:
            S.op(eng, lambda e: e.tensor_scalar(out=out, in0=in0, scalar1=s1, scalar2=s2, op0=op0, op1=op1), R, W)

    def stt(out, in0, scalar, in1, op0, op1, R, W):
        S.op("dve", lambda e: e.scalar_tensor_tensor(out=out, in0=in0, scalar=scalar, in1=in1, op0=op0, op1=op1), R, W)

    def cp(eng, out, in_, R, W):
        if eng == "act":
            S.op("act", lambda e: e.activation(out=out, in_=in_, func=AF.Copy), R, W)
        else:
            S.op(eng, lambda e: e.tensor_copy(out=out, in_=in_), R, W)

    def red(out, in_, R, W):
        S.op("dve", lambda e: e.tensor_reduce(out=out, in_=in_, axis=AX.X, op=ALU.add), R, W)

    def recip(out, in_, R, W):
        S.op("dve", lambda e: e.reciprocal(out=out, in_=in_), R, W)

    def mset(eng, ap, v, W):
        S.op(eng, lambda e: e.memset(ap, v), (), W)

    dq = [0]

    def dma(out, in_, R, W, q=None):
        if q is None:
            q = "sp"
            dq[0] += 1
        S.dma(q, lambda e: e.dma_start(out=out, in_=in_), R, W)

    def rsqrt_inplace(ap, tmp_scale, add, R):
        ts("dve", ap, ap, tmp_scale, add, ALU.mult, ALU.add, R, R)
        act(ap, ap, AF.Sqrt, R, R)
        recip(ap, ap, R, R)

    with contextlib.ExitStack() as es0:
        def sb(name, shape, dt=F32, es=es0):
            return es.enter_context(nc.sbuf_tensor(_uname(name), list(shape), dt)), Buf(name)

        ident, b_ident = sb("ident", [128, 128])
        ones, b_ones = sb("ones", [128, 128])
        tri = [sb("tri%d" % d, [128, 512]) for d in range(2)]
        mbig = [sb("mbig%d" % d, [128, 512]) for d in range(2)]
        blk, b_blk = sb("blk", [128, 128])
        chm, b_chm = sb("chm", [128, 8])
        offd, b_offd = sb("offd", [128, 512])
        dma(ident[:], c_ident[:, :], [], [b_ident])
        dma(ones[:], c_ones[:, :], [], [b_ones])
        for d in range(2):
            dma(tri[d][0][:], c_tri[d], [], [tri[d][1]])
            dma(mbig[d][0][:], c_mbig[d], [], [mbig[d][1]])
        dma(blk[:], c_blk[:, :], [], [b_blk])
        dma(chm[:], c_chm[:, :], [], [b_chm])
        dma(offd[:], c_offd[:, :], [], [b_offd])

        scT, b_scT = sb("scT", [128, DEPTH * 2 * 16])
        shT, b_shT = sb("shT", [128, DEPTH * 2 * 16])
        GPW = dscr("GPW", [DEPTH * 2, D])

        def scT_ap(l, which, k):
            i = (l * 2 + which) * 16 + k
            return scT[:, i:i + 1]

        def shT_ap(l, which, k):
            i = (l * 2 + which) * 16 + k
            return shT[:, i:i + 1]

        with contextlib.ExitStack() as es:
            pa, b_pa = sb("p0a", [64, 128], es=es)
            pb, b_pb = sb("p0b", [DEPTH * 48, 128], es=es)
            paT, b_paT = sb("p0aT", [128, 64], es=es)
            pbT, b_pbT = sb("p0bT", [128, DEPTH * 48], es=es)
            cs2, b_cs2 = sb("p0cs2", [128, 16, 2], es=es)
            modT, b_modT = sb("p0modT", [128, 48, 2], es=es)
            grow, b_grow = sb("p0grow", [2, D], es=es)
            brow, b_brow = sb("p0brow", [2, D], es=es)
            prow, b_prow = sb("p0prow", [2, D], es=es)
            slabs = TPool(es, nc, "p0slab", [128, 16, 512], F32, 2)
            ps_t = es.enter_context(nc.psum_tensor(_uname("p0ps_t"), [128, 512], F32)); b_ps_t = Buf()
            ps_m = es.enter_context(nc.psum_tensor(_uname("p0ps_m"), [128, 512], F32)); b_ps_m = Buf()
            ps_g = TPool(es, nc, "p0ps_g", [128, 512], F32, 2, psum=True)

            dma(pa[0:32, :], cvec[:, :], [], [b_pa])
            dma(pa[32:32 + DEPTH * 16, :], pre_w.rearrange("l (k p) -> (l k) p", p=128), [], [b_pa])
            dma(pb[:], ada_b.rearrange("l (k p) -> (l k) p", p=128), [], [b_pb])
            act(pa[0:32, :], pa[0:32, :], AF.Silu, [b_pa], [b_pa])
            S.op("pe", lambda e: e.transpose(out=ps_t[:, 0:64], in_=pa[:, :], identity=ident[0:64, 0:64]), [b_pa, b_ident], [b_ps_t])
            cp("dve", paT[:], ps_t[:, 0:64], [b_ps_t], [b_paT])
            S.op("pe", lambda e: e.transpose(out=ps_t[:, 128:128 + DEPTH * 48], in_=pb[:, :], identity=ident[0:DEPTH * 48, 0:DEPTH * 48]), [b_pb, b_ident], [b_ps_t])
            cp("dve", pbT[:], ps_t[:, 128:128 + DEPTH * 48], [b_ps_t], [b_pbT])
            cp("dve", cs2[:, :, 0], paT[:, 0:16], [b_paT], [b_cs2])
            cp("dve", cs2[:, :, 1], paT[:, 16:32], [b_paT], [b_cs2])
            for l in range(DEPTH):
                dma(brow[0:1, :], ada_b[l:l + 1, 2 * D:3 * D], [], [b_brow])
                dma(brow[1:2, :], ada_b[l:l + 1, 2 * D:3 * D], [], [b_brow])
                dma(prow[0:1, :], post_w[l:l + 1, :], [], [b_prow])
                dma(prow[1:2, :], post_w[l:l + 1, :], [], [b_prow])
                awv = ada_w[l].rearrange("(k p) n -> p k n", p=128)
                for sl in range(12):
                    slab, b_slab = slabs.next()
                    for kk in range(0, 16, 4):
                        dma(slab[:, kk:kk + 4, :], awv[:, kk:kk + 4, sl * 512:(sl + 1) * 512], [], [b_slab])
                    for j in range(4):
                        cb = sl * 4 + j
                        for k in range(KC):
                            mm(ps_m[:, cb * 2:cb * 2 + 2], slab[:, k, j * 128:(j + 1) * 128], cs2[:, k, :],
                               k == 0, k == KC - 1, [b_slab, b_cs2], [b_ps_m])
                    if sl >= 8:
                        pg, b_pg = ps_g.next()
                        for k in range(KC):
                            mm(pg[0:2, :], cs2[:, k, :], slab[:, k, :], k == 0, k == KC - 1, [b_slab, b_cs2], [b_pg])
                        cc = (sl - 8) * 512
                        tt("dve", grow[:, cc:cc + 512], pg[0:2, :], brow[:, cc:cc + 512], ALU.add, [b_pg, b_brow], [b_grow])
                cp("dve", modT[:].rearrange("p a b -> p (a b)"), ps_m[:, 0:96], [b_ps_m], [b_modT])
                tt("dve", grow[:], grow[:], prow[:], ALU.mult, [b_grow, b_prow], [b_grow])
                dma(GPW[l * 2:l * 2 + 2, :], grow[:], [b_grow], [DB("GPW", l)])
                for which in range(2):
                    i0 = (l * 2 + which) * 16
                    tt("dve", shT[:, i0:i0 + 16], modT[:, 0:16, which], pbT[:, l * 48:l * 48 + 16], ALU.add, [b_modT, b_pbT], [b_shT])
                    tt("dve", scT[:, i0:i0 + 16], modT[:, 16:32, which], pbT[:, l * 48 + 16:l * 48 + 32], ALU.add, [b_modT, b_pbT], [b_scT])
                    stt(scT[:, i0:i0 + 16], scT[:, i0:i0 + 16], 1.0, paT[:, 32 + l * 16:48 + l * 16], ALU.add, ALU.mult, [b_scT, b_paT], [b_scT])
            S.barrier()

        for l in range(DEPTH):
            last = (l == DEPTH - 1)

            def xsrc(n):
                if l == 0:
                    return (ctx_in[n * 128:(n + 1) * 128, :] if n < NCT else x_in[(n - NCT) * 128:(n - NCT + 1) * 128, :]), []
                return X1[n * 128:(n + 1) * 128, :], [DB("X1", n)]

            with contextlib.ExitStack() as es:
                TBMAX = (NT + 1) // 2
                hT = [sb("a_hT%d" % i, [128, KC, 128], BF16, es=es) for i in range(TBMAX)]
                xt_p = TPool(es, nc, "a_x", [128, D], F32, 2)
                st_p = TPool(es, nc, "a_st", [128, 4], F32, 2)
                wst_p = TPool(es, nc, "a_wst", [128, KC, 512], F32, 2)
                wbf_p = TPool(es, nc, "a_wbf", [128, KC, 512], BF16, 2)
                ub_p = TPool(es, nc, "a_ub", [128, 512], BF16, 3)
                ug_p = TPool(es, nc, "a_ug", [128, 32], F32, 2)
                ps_p = TPool(es, nc, "a_ps", [128, 512], F32, 6, psum=True)
                wv = w_in[l].rearrange("(k p) n -> p k n", p=128)
                blocks = [list(range(0, TBMAX)), list(range(TBMAX, NT))]

                def prep_tile(i, n):
                    which = 1 if n < NCT else 0
                    xt, b_xt = xt_p.next()
                    st, b_st = st_p.next()
                    src, sr = xsrc(n)
                    dma(xt[:], src, sr, [b_xt])
                    ps, b_ps = ps_p.next()
                    for c4 in range(4):
                        act(ps[:, :], xt[:, c4 * 512:(c4 + 1) * 512], AF.Square, [b_xt], [b_ps, b_st], accum=st[:, c4:c4 + 1])
                    red(st[:, 0:1], st[:, 0:4], [b_st], [b_st])
                    rsqrt_inplace(st[:, 0:1], 1.0 / D, EPS, [b_st])
                    ts("dve", xt[:], xt[:], st[:, 0:1], None, ALU.mult, None, [b_xt, b_st], [b_xt])
                    ht, b_ht = hT[i]
                    for k4 in range(4):
                        ps, b_ps = ps_p.next()
                        for j in range(4):
                            k = k4 * 4 + j
                            S.op("pe", (lambda k=k, j=j, ps=ps, xt=xt: lambda e: e.transpose(out=ps[:, j * 128:(j + 1) * 128], in_=xt[:, k * 128:(k + 1) * 128], identity=ident[:]))(),
                                 [b_xt, b_ident], [b_ps])
                        for j in range(4):
                            k = k4 * 4 + j
                            act(ht[:, k, :], ps[:, j * 128:(j + 1) * 128], AF.Identity, [b_ps, b_scT, b_shT], [b_ht],
                                bias=shT_ap(l, which, k), scale=scT_ap(l, which, k))

                for bi, tb in enumerate(blocks):
                    if not tb:
                        continue
                    nxt_tb = blocks[bi + 1] if bi + 1 < len(blocks) else []
                    if bi == 0:
                        for i, n in enumerate(tb):
                            prep_tile(i, n)
                    def load_w(cb):
                        wst, b_wst = wst_p.next()
                        wbf, b_wbf = wbf_p.next()
                        if cb < 16:
                            o0 = cb * 512 + (16 if cb >= 5 else 0)
                            wdt = 512
                            for kk in range(0, 16, 4):
                                dma(wst[:, kk:kk + 4, :], wv[:, kk:kk + 4, o0:o0 + 512], [], [b_wst])
                        else:
                            wdt = 32
                            dma(wst[:, :, 0:16], wv[:, :, 2560:2576], [], [b_wst])
                            dma(wst[:, :, 16:32], wv[:, :, 8208:8224], [], [b_wst])
                        cp("dve", wbf[:, 0:8, 0:wdt], wst[:, 0:8, 0:wdt], [b_wst], [b_wbf])
                        cp("dve", wbf[:, 8:16, 0:wdt], wst[:, 8:16, 0:wdt], [b_wst], [b_wbf])
                        return wbf, b_wbf, wdt
                    cb_order = [16] + list(range(16))
                    nxt_w = load_w(cb_order[0])
                    for ci, cb in enumerate(cb_order):
                        wbf, b_wbf, wdt = nxt_w
                        if ci + 1 < 17:
                            nxt_w = load_w(cb_order[ci + 1])
                        for i, n in enumerate(tb):
                            ht, b_ht = hT[i]
                            ps, b_ps = ps_p.next()
                            for k in range(KC):
                                mm(ps[:, 0:wdt], ht[:, k, :], wbf[:, k, 0:wdt], k == 0, k == KC - 1, [b_ht, b_wbf], [b_ps])
                            if cb < 16:
                                ub, b_ub = ub_p.next()
                                cp("act", ub[:, :], ps[:, 0:512], [b_ps], [b_ub])
                                dma(U[n * 128:(n + 1) * 128, cb * 512:(cb + 1) * 512], ub[:, :], [b_ub], [DB("U", n)], q="act")
                            else:
                                ug, b_ug = ug_p.next()
                                cp("act", ug[:, :], ps[:, 0:32], [b_ps], [b_ug])
                                dma(UG[n * 128:(n + 1) * 128, :], ug[:, :], [b_ug], [DB("UG", n)], q="act")
                            if ci == 16 and i < len(nxt_tb):
                                prep_tile(i, nxt_tb[i])
                S.barrier()

            if debug and l == debug - 1:
                pass

            ctx_needed = not last
            fwd_order = list(range(NT))
            bwd_order = list(range(NCT - 1, -1, -1)) + list(range(NT - 1, NCT - 1, -1))

            with contextlib.ExitStack() as es:
                QT, b_QT = sb("at_QT", [128, 4, T], BF16, es=es)
                KT, b_KT = sb("at_KT", [128, 2, T], BF16, es=es)
                V1, b_V1 = sb("at_V1", [128, NT, 2, 132], BF16, es=es)
                nwq, b_nwq = sb("at_nwq", [128, 128], es=es)
                nwk, b_nwk = sb("at_nwk", [128, 128], es=es)
                st_p = TPool(es, nc, "at_st", [128, 8], F32, 4)
                pt_p = TPool(es, nc, "at_pt", [128, 512], BF16, 3)
                ob_p = TPool(es, nc, "at_ob", [128, 128], F32, 3)
                ps_p = TPool(es, nc, "at_ps", [128, 512], F32, 4, psum=True)
                acc_p = TPool(es, nc, "at_acc", [128, 512], F32, 4, psum=True)
                dma(nwq[:], qnw[l:l + 1, :].partition_broadcast(128), [], [b_nwq])
                dma(nwk[:], knw[l:l + 1, :].partition_broadcast(128), [], [b_nwk])
                mset("dve", V1[:, :, :, 128:129], 1.0, [b_V1])
                ts("dve", nwq[:], nwq[:], 128.0 ** -0.5, None, ALU.mult, None, [b_nwq], [b_nwq])

                with contextlib.ExitStack() as es2:
                    raw_p = TPool(es2, nc, "at_raw", [128, 1024], BF16, 3)
                    q_p = TPool(es2, nc, "at_q", [128, 768], F32, 3)
                    q2_p = TPool(es2, nc, "at_q2", [128, 768], F32, 3)
                    sw_p = TPool(es2, nc, "at_sw", [128, 768], F32, 3)
                    rc_p = TPool(es2, nc, "at_rc", [128, 128], F32, 3)
                    rs_p = TPool(es2, nc, "at_rs", [128, 128], F32, 3)

                    def a1_tile(n):
                        raw, b_raw = raw_p.next()
                        dma(raw[:, 0:1024], U[n * 128:(n + 1) * 128, OFF["a_q"]:OFF["a_q"] + 1024], [DB("U", n)], [b_raw])
                        q, b_q = q_p.next()
                        q2, b_q2 = q2_p.next()
                        st, b_st = st_p.next()
                        cp("dve", q[:, :], raw[:, 0:768], [b_raw], [b_q])
                        cp("pool", V1[:, n, :, 0:128], raw[:, 768:1024].rearrange("p (h d) -> p h d", h=2), [b_raw], [b_V1])
                        tt("dve", q2[:, :], q[:, :], q[:, :], ALU.mult, [b_q], [b_q2])
                        red(st[:, 0:6], q2[:, :].rearrange("p (h d) -> p h d", h=6), [b_q2], [b_st])
                        yield
                        rsqrt_inplace(st[:, 0:6], 1.0 / 128, EPS, [b_st])
                        yield
                        q3 = q[:, :].rearrange("p (h d) -> p h d", h=6)
                        tt("dve", q3, q3, st[:, 0:6].unsqueeze(2).to_broadcast([128, 6, 128]), ALU.mult, [b_q, b_st], [b_q])
                        tt("pool", q3[:, 0:4, :], q3[:, 0:4, :], nwq[:, :].unsqueeze(1).to_broadcast([128, 4, 128]), ALU.mult, [b_q, b_nwq], [b_q])
                        tt("pool", q3[:, 4:6, :], q3[:, 4:6, :], nwk[:, :].unsqueeze(1).to_broadcast([128, 2, 128]), ALU.mult, [b_q, b_nwk], [b_q])
                        yield
                        if n >= NCT:
                            rc, b_rc = rc_p.next()
                            rs, b_rs = rs_p.next()
                            sw, b_sw = sw_p.next()
                            r0 = (n - NCT) * 128
                            dma(rc[:], ropec[r0:r0 + 128, :], [], [b_rc])
                            dma(rs[:], ropes[r0:r0 + 128, :], [], [b_rs])
                            q4 = q[:, :].rearrange("p (g two f) -> p g two f", two=2, f=32)
                            s4 = sw[:, :].rearrange("p (g two f) -> p g two f", two=2, f=32)
                            cp("pool", s4[:, :, 0, :], q4[:, :, 1, :], [b_q], [b_sw])
                            cp("pool", s4[:, :, 1, :], q4[:, :, 0, :], [b_q], [b_sw])
                            s3 = sw[:, :].rearrange("p (h d) -> p h d", h=6)
                            tt("dve", q3, q3, rc[:, :].unsqueeze(1).to_broadcast([128, 6, 128]), ALU.mult, [b_q, b_rc], [b_q])
                            tt("pool", s3, s3, rs[:, :].unsqueeze(1).to_broadcast([128, 6, 128]), ALU.mult, [b_sw, b_rs], [b_sw])
                            yield
                            tt("dve", q3, q3, s3, ALU.add, [b_q, b_sw], [b_q])
                            yield
                        for half in range(2):
                            ps, b_ps = ps_p.next()
                            nh = 4 if half == 0 else 2
                            for j in range(nh):
                                hh = half * 4 + j
                                S.op("pe", (lambda ps=ps, j=j, q=q, hh=hh: lambda e: e.transpose(out=ps[:, j * 128:(j + 1) * 128], in_=q[:, hh * 128:(hh + 1) * 128], identity=ident[:]))(),
                                     [b_q, b_ident], [b_ps])
                            if half == 0:
                                cp("act", QT[:, :, n * 128:(n + 1) * 128], ps[:, 0:512].rearrange("p (h t) -> p h t", h=4), [b_ps], [b_QT])
                            else:
                                cp("act", KT[:, :, n * 128:(n + 1) * 128], ps[:, 0:256].rearrange("p (h t) -> p h t", h=2), [b_ps], [b_KT])

                    pend = list(range(NT))
                    act_g = []
                    while pend or act_g:
                        while pend and len(act_g) < 3:
                            act_g.append(a1_tile(pend.pop(0)))
                        alive = []
                        for g_ in act_g:
                            try:
                                next(g_)
                                alive.append(g_)
                            except StopIteration:
                                pass
                        act_g = alive
                    S.barrier()

                def attend(qtiles, ktiles):
                    for h in range(4):
                        kvh = h // 2
                        for qb0 in range(0, len(qtiles), 4):
                            qts = qtiles[qb0:qb0 + 4]
                            nq = len(qts)
                            q0 = qts[0] * 128
                            accs = [acc_p.next() for _ in range(nq)]
                            for si, s in enumerate(ktiles):
                                ps, b_ps = ps_p.next()
                                mm(ps[:, 0:nq * 128], KT[:, kvh, s * 128:(s + 1) * 128], QT[:, h, q0:q0 + nq * 128], True, True, [b_KT, b_QT], [b_ps])
                                pt, b_pt = pt_p.next()
                                act(pt[:, 0:nq * 128], ps[:, 0:nq * 128], AF.Exp, [b_ps], [b_pt])
                                for j in range(nq):
                                    mm(accs[j][0][:, 0:129], pt[:, j * 128:(j + 1) * 128], V1[:, s, kvh, 0:129], si == 0, si == len(ktiles) - 1,
                                       [b_pt, b_V1], [accs[j][1]])
                                yield
                            for j in range(nq):
                                ac, b_ac = accs[j]
                                ob, b_ob = ob_p.next()
                                st, b_st = st_p.next()
                                act(st[:, 0:1], ac[:, 128:129], AF.Ln, [b_ac], [b_st])
                                act(st[:, 0:1], st[:, 0:1], AF.Exp, [b_st], [b_st], scale=-1.0)
                                act(ob[:, :], ac[:, 0:128], AF.Identity, [b_ac, b_st], [b_ob], scale=st[:, 0:1])
                                n = qts[j]
                                dma(OA[n * 128:(n + 1) * 128, h * 128:(h + 1) * 128], ob[:, :], [b_ob], [DB("OA", n)], q="act")

                def gen_attn():
                    yield from attend(list(range(NCT, NT)), list(range(NT)))
                    if ctx_needed:
                        yield from attend(list(range(NCT)), list(range(NCT)))

                with contextlib.ExitStack() as es3:
                    cw = [sb("p_cw%d" % j, [128, 1536], es=es3) for j in range(5)]
                    for j in range(5):
                        dma(cw[j][0][:], convw[l, j:j + 1, :].partition_broadcast(128), [], [cw[j][1]])

                    def gen_prep(tiles, gi):
                        sh_p = TPool(es3, nc, "p%d_sh" % gi, [128, 1536], BF16, 2)
                        acc_p2 = TPool(es3, nc, "p%d_acc" % gi, [128, 1536], F32, 1)
                        tmp_p = TPool(es3, nc, "p%d_tmp" % gi, [128, 1536], F32, 2)
                        pst_p = TPool(es3, nc, "p%d_st" % gi, [128, 16], F32, 2)
                        pob_p = TPool(es3, nc, "p%d_ob" % gi, [128, 1536], BF16, 1)
                        rraw_p = TPool(es3, nc, "p%d_rraw" % gi, [128, 1024], BF16, 2)
                        rq_p = TPool(es3, nc, "p%d_rq" % gi, [128, 1024], F32, 1)
                        rsw_p = TPool(es3, nc, "p%d_rsw" % gi, [128, 1024], F32, 1)
                        prc_p = TPool(es3, nc, "p%d_rc" % gi, [128, 128], F32, 1)
                        prs_p = TPool(es3, nc, "p%d_rs" % gi, [128, 128], F32, 1)
                        yield
                        for n in tiles:
                            rows = slice(n * 128, (n + 1) * 128)
                            seg0, seg1 = (0, NCT * 128) if n < NCT else (NCT * 128, T)
                            acc, b_acc = acc_p2.next()
                            for j in range(5):
                                sh, b_sh = sh_p.next()
                                lo = n * 128 + j - 2
                                hi = lo + 128
                                clo, chi = max(lo, seg0), min(hi, seg1)
                                if clo > lo or chi < hi:
                                    mset("pool", sh[:, :], 0.0, [b_sh])
                                dma(sh[clo - lo:chi - lo, :], U[clo:chi, OFF["d_q"]:OFF["d_q"] + 1536],
                                    [DB("U", m) for m in range(max(0, n - 1), min(NT, n + 2))], [b_sh])
                                if j == 0:
                                    tt("pool", acc[:, :], sh[:, :], cw[0][0][:, :], ALU.mult, [b_sh, cw[0][1]], [b_acc])
                                else:
                                    tmp, b_tmp = tmp_p.next()
                                    tt("pool" if j == 1 else "dve", tmp[:, :], sh[:, :], cw[j][0][:, :], ALU.mult, [b_sh, cw[j][1]], [b_tmp])
                                    yield
                                    tt("dve" if j % 2 else "pool", acc[:, :], acc[:, :], tmp[:, :], ALU.add, [b_acc, b_tmp], [b_acc])
                                yield
                            for _w in range(8):
                                yield
                            act(acc[:, :], acc[:, :], AF.Silu, [b_acc], [b_acc])
                            yield
                            tmp, b_tmp = tmp_p.next()
                            st, b_st = pst_p.next()
                            tt("dve", tmp[:, 0:1024], acc[:, 0:1024], acc[:, 0:1024], ALU.mult, [b_acc], [b_tmp])
                            yield
                            red(st[:, 0:8], tmp[:, 0:1024].rearrange("p (h d) -> p h d", h=8), [b_tmp], [b_st])
                            yield
                            ts("dve", st[:, 0:8], st[:, 0:8], 1.0, EPS, ALU.mult, ALU.add, [b_st], [b_st])
                            for _w in range(8):
                                yield
                            act(st[:, 0:8], st[:, 0:8], AF.Sqrt, [b_st], [b_st])
                            yield
                            recip(st[:, 0:8], st[:, 0:8], [b_st], [b_st])
                            yield
                            ts("dve", st[:, 0:4], st[:, 0:4], 128.0 ** -0.5, None, ALU.mult, None, [b_st], [b_st])
                            yield
                            qk3 = acc[:, 0:1024].rearrange("p (h d) -> p h d", h=8)
                            tt("dve", qk3, qk3, st[:, 0:8].unsqueeze(2).to_broadcast([128, 8, 128]), ALU.mult, [b_acc, b_st], [b_acc])
                            yield
                            ob, b_ob = pob_p.next()
                            for _w in range(8):
                                yield
                            cp("act", ob[:, :], acc[:, :], [b_acc], [b_ob])
                            dma(DQ[rows, :], ob[:, :], [b_ob], [DB("DQ", n)], q="act")
                            yield
                            rr, b_rr = rraw_p.next()
                            rq, b_rq = rq_p.next()
                            dma(rr[:, :], U[rows, OFF["r_q"]:OFF["r_q"] + 1024], [DB("U", n)], [b_rr])
                            cp("pool", rq[:, :], rr[:, :], [b_rr], [b_rq])
                            yield
                            if n >= NCT:
                                rc, b_rc = prc_p.next()
                                rs, b_rs = prs_p.next()
                                sw, b_sw = rsw_p.next()
                                r0 = (n - NCT) * 128
                                dma(rc[:], ropec[r0:r0 + 128, :], [], [b_rc])
                                dma(rs[:], ropes[r0:r0 + 128, :], [], [b_rs])
                                q4 = rq[:, :].rearrange("p (g two f) -> p g two f", two=2, f=32)
                                s4 = sw[:, :].rearrange("p (g two f) -> p g two f", two=2, f=32)
                                cp("pool", s4[:, :, 0, :], q4[:, :, 1, :], [b_rq], [b_sw])
                                cp("pool", s4[:, :, 1, :], q4[:, :, 0, :], [b_rq], [b_sw])
                                q3 = rq[:, :].rearrange("p (h d) -> p h d", h=8)
                                s3 = sw[:, :].rearrange("p (h d) -> p h d", h=8)
                                tt("dve", q3, q3, rc[:, :].unsqueeze(1).to_broadcast([128, 8, 128]), ALU.mult, [b_rq, b_rc], [b_rq])
                                tt("pool", s3, s3, rs[:, :].unsqueeze(1).to_broadcast([128, 8, 128]), ALU.mult, [b_sw, b_rs], [b_sw])
                                yield
                                tt("dve", q3, q3, s3, ALU.add, [b_rq, b_sw], [b_rq])
                                yield
                            ts("dve", rq[:, 512:1024], rq[:, 512:1024], 128.0 ** -0.5, None, ALU.mult, None, [b_rq], [b_rq])
                            yield
                            rr2, b_rr2 = rraw_p.next()
                            for _w in range(8):
                                yield
                            cp("act", rr2[:, :], rq[:, :], [b_rq], [b_rr2])
                            dma(U[rows, OFF["r_q"]:OFF["r_q"] + 1024], rr2[:, :], [b_rr2], [DB("Ur", n)], q="act")
                            yield

                    gens = [gen_attn(), gen_prep(list(range(0, NT, 2)), 0), gen_prep(list(range(1, NT, 2)), 1)]
                    while gens:
                        alive = []
                        for g_ in gens:
                            try:
                                next(g_)
                                alive.append(g_)
                            except StopIteration:
                                pass
                        gens = alive
                    S.barrier()

            for mixer in ("m", "r", "d"):
                with contextlib.ExitStack() as es:
                    VW = 129 if mixer == "m" else 128
                    OUT = dict(m=OM, r=OR, d=OD)[mixer]
                    ugt, b_ugt = sb("g_ug", [128, NT, 32], es=es)
                    for n in range(NT):
                        dma(ugt[:, n, :], UG[n * 128:(n + 1) * 128, :], [DB("UG", n)], [b_ugt])
                    prm, b_prm = sb("g_prm", [128, 32], es=es)
                    G = {}
                    NG = NT * 4
                    for d in range(2):
                        for nm in ("lf", "li", "g", "gend", "eg", "kend", "rowp", "beta", "beg"):
                            G[(nm, d)] = sb("g_%s%d" % (nm, d), [128, NT, 4], es=es)
                        G[("gam", d)] = sb("g_gam%d" % d, [128, NT, 8], es=es)
                    gtmp, b_gtmp = sb("g_tmp", [128, NT, 8], es=es)
                    psA = TPool(es, nc, "r_psA", [128, 512], F32, 4, psum=True)
                    psB = TPool(es, nc, "r_psB", [128, 512], F32, 4, psum=True)
                    gps = psA
                    if mixer == "m":
                        dma(prm[:, 0:8], mib[l:l + 1, :].partition_broadcast(128), [], [b_prm])
                        dma(prm[:, 8:16], mfb[l:l + 1, :].partition_broadcast(128), [], [b_prm])
                    elif mixer == "r":
                        dma(prm[:, 0:8], rlg[l:l + 1, :].partition_broadcast(128), [], [b_prm])
                    else:
                        dma(prm[:, 0:8], alog[l:l + 1, :].partition_broadcast(128), [], [b_prm])
                        dma(prm[:, 8:16], dtb[l:l + 1, :].partition_broadcast(128), [], [b_prm])
                        act(prm[:, 0:8], prm[:, 0:8], AF.Exp, [b_prm], [b_prm])
                    for d in range(2):
                        lf, b_lf = G[("lf", d)]
                        li, b_li = G[("li", d)]
                        g, b_g = G[("g", d)]
                        gend, b_gend = G[("gend", d)]
                        eg, b_eg = G[("eg", d)]
                        kend, b_kend = G[("kend", d)]
                        rowp, b_rowp = G[("rowp", d)]
                        beta, b_beta = G[("beta", d)]
                        beg, b_beg = G[("beg", d)]
                        gam, b_gam = G[("gam", d)]

                        def prmb(c0):
                            return prm[:, c0 + d * 4:c0 + d * 4 + 4].unsqueeze(1).to_broadcast([128, NT, 4])
                        if mixer == "m":
                            tt("dve", li[:], ugt[:, :, d * 4:d * 4 + 4], prmb(0), ALU.add, [b_ugt, b_prm], [b_li])
                            tt("dve", lf[:], ugt[:, :, 8 + d * 4:12 + d * 4], prmb(8), ALU.add, [b_ugt, b_prm], [b_lf])
                            act(lf[:], lf[:], AF.Exp, [b_lf], [b_lf], scale=-1.0)
                            act(lf[:], lf[:], AF.Ln, [b_lf], [b_lf], bias=1.0)
                            ts("dve", lf[:], lf[:], -1.0, None, ALU.mult, None, [b_lf], [b_lf])
                        elif mixer == "r":
                            mset("dve", li[:], 0.0, [b_li])
                            mset("dve", lf[:], 0.0, [b_lf])
                            tt("dve", lf[:], lf[:], prmb(0), ALU.add, [b_lf, b_prm], [b_lf])
                        else:
                            mset("dve", li[:], 0.0, [b_li])
                            tt("dve", lf[:], ugt[:, :, 16 + d * 4:20 + d * 4], prmb(8), ALU.add, [b_ugt, b_prm], [b_lf])
                            act(lf[:], lf[:], AF.Exp, [b_lf], [b_lf])
                            act(lf[:], lf[:], AF.Ln, [b_lf], [b_lf], bias=1.0)
                            tt("dve", lf[:], lf[:], prmb(0), ALU.mult, [b_lf, b_prm], [b_lf])
                            ts("dve", lf[:], lf[:], -1.0, None, ALU.mult, None, [b_lf], [b_lf])
                            act(beta[:], ugt[:, :, 24 + d * 4:28 + d * 4], AF.Exp, [b_ugt], [b_beta], scale=-1.0)
                            ts("dve", beta[:], beta[:], 1.0, None, ALU.add, None, [b_beta], [b_beta])
                            recip(beta[:], beta[:], [b_beta], [b_beta])
                        lf2 = lf[:].rearrange("p n h -> p (n h)")
                        ps, b_ps = gps.next()
                        mm(ps[:, 0:NG], tri[d][0][:, 0:128], lf2, True, True, [tri[d][1], b_lf], [b_ps])
                        cp("dve", g[:].rearrange("p n h -> p (n h)"), ps[:, 0:NG], [b_ps], [b_g])
                        ps, b_ps = gps.next()
                        mm(ps[:, 0:NG], blk[:, :], lf2, True, True, [b_blk, b_lf], [b_ps])
                        cp("dve", gend[:].rearrange("p n h -> p (n h)"), ps[:, 0:NG], [b_ps], [b_gend])
                        act(eg[:], g[:], AF.Exp, [b_g], [b_eg])
                        tt("dve", rowp[:], li[:], g[:], ALU.subtract, [b_li, b_g], [b_rowp])
                        tt("dve", kend[:], gend[:], rowp[:], ALU.add, [b_gend, b_rowp], [b_kend])
                        act(kend[:], kend[:], AF.Exp, [b_kend], [b_kend])
                        if mixer == "d":
                            tt("dve", beg[:], beta[:], eg[:], ALU.mult, [b_beta, b_eg], [b_beg])
                        g4 = gtmp[:].rearrange("p n (c h) -> p n c h", c=2)
                        for c in range(2):
                            tt("dve", g4[:, :, c, :], lf[:], chm[:, c * 4:c * 4 + 4].unsqueeze(1).to_broadcast([128, NT, 4]), ALU.mult, [b_lf, b_chm], [b_gtmp])
                        ps, b_ps = gps.next()
                        mm(ps[:, 0:NT * 8], ones[:, :], gtmp[:].rearrange("p n c -> p (n c)"), True, True, [b_ones, b_gtmp], [b_ps])
                        act(gam[:].rearrange("p n c -> p (n c)"), ps[:, 0:NT * 8], AF.Exp, [b_ps], [b_gam])

                    KSLOT = 3 if mixer == "d" else 4
                    st_p = TPool(es, nc, "r_st", [128, 16], F32, 12)
                    slots = []
                    for si in range(KSLOT):
                        Bd = {}
                        Bd["raw"] = sb("s%d_raw" % si, [128, 1536], BF16, es=es)
                        Bd["qkv"] = sb("s%d_qkv" % si, [128, 1536], F32, es=es)
                        Bd["qs"] = sb("s%d_qs" % si, [128, 512], F32, es=es)
                        Bd["ke"] = sb("s%d_ke" % si, [128, 512], BF16, es=es)
                        Bd["v1"] = sb("s%d_v1" % si, [128, 4, 132], BF16, es=es)
                        for nm in ("QT", "QtT", "KT", "QKT"):
                            Bd[nm] = sb("s%d_%s" % (si, nm), [128, 512], BF16, es=es)
                        Bd["ep"] = sb("s%d_ep" % si, [128, 512], F32, es=es)
                        Bd["DT"] = sb("s%d_DT" % si, [128, 512], F32, es=es)
                        Bd["ob"] = sb("s%d_ob" % si, [128, 512], F32, es=es)
                        if mixer == "d":
                            Bd["kb"] = sb("s%d_kb" % si, [128, 512], F32, es=es)
                            Bd["wk"] = sb("s%d_wk" % si, [128, 512], F32, es=es)
                            Bd["KbT"] = sb("s%d_KbT" % si, [128, 512], BF16, es=es)
                            Bd["WkT"] = sb("s%d_WkT" % si, [128, 512], BF16, es=es)
                            Bd["AT"] = [sb("s%d_AT%d" % (si, j), [128, 512], F32, es=es) for j in range(3)]
                            Bd["A"] = [sb("s%d_A%d" % (si, j), [128, 512], F32, es=es) for j in range(3)]
                            Bd["R"] = [sb("s%d_R%d" % (si, j), [128, 4, 256], F32, es=es) for j in range(3)]
                            Bd["U"] = [sb("s%d_U%d" % (si, j), [128, 512], BF16, es=es) for j in range(2)]
                        slots.append(Bd)
                    state = [sb("r_S%d" % d, [128, 4, 132], es=es) for d in range(2)]
                    stateb = [sb("r_Sb%d" % d, [128, 4, 132], BF16, es=es) for d in range(2)]
                    for d in range(2):
                        mset("dve", state[d][0][:], 0.0, [state[d][1]])
                        mset("dve", stateb[d][0][:], 0.0, [stateb[d][1]])

                    def transpose4(src, b_src, dst, b_dst):
                        ps, b_ps = psA.next()
                        for h in range(4):
                            S.op("pe", (lambda ps=ps, h=h, src=src: lambda e: e.transpose(out=ps[:, h * 128:(h + 1) * 128], in_=src[:, h * 128:(h + 1) * 128], identity=ident[:]))(),
                                 [b_src, b_ident], [b_ps])
                        cp("act", dst[:, :], ps[:, :], [b_ps], [b_dst])

                    def unit(n, d, Bd, kseq):
                        eg, b_eg = G[("eg", d)]
                        kend, b_kend = G[("kend", d)]
                        rowp, b_rowp = G[("rowp", d)]
                        lf, b_lf = G[("lf", d)]
                        gam, b_gam = G[("gam", d)]
                        beta, b_beta = G[("beta", d)]
                        beg, b_beg = G[("beg", d)]
                        raw, b_raw = Bd["raw"]
                        qkv, b_qkv = Bd["qkv"]
                        rows = slice(n * 128, (n + 1) * 128)
                        if mixer == "d":
                            dma(raw[:, :], DQ[rows, :], [DB("DQ", n)], [b_raw])
                        else:
                            c0 = OFF["m_q"] if mixer == "m" else OFF["r_q"]
                            dma(raw[:, :], U[rows, c0:c0 + 1536], [DB("U", n), DB("Ur", n)], [b_raw])
                        cp("dve", qkv[:, :], raw[:, :], [b_raw], [b_qkv])
                        if mixer == "m":
                            ts("dve", qkv[:, 0:512], qkv[:, 0:512], 128.0 ** -0.5, None, ALU.mult, None, [b_qkv], [b_qkv])
                        yield
                        qv = qkv[:, 0:512]
                        kv = qkv[:, 512:1024]
                        vv = qkv[:, 1024:1536]
                        h4 = lambda ap: ap.rearrange("p (h d) -> p h d", h=4)
                        bc = lambda t_: t_[:, n, :].unsqueeze(2).to_broadcast([128, 4, 128])
                        qs, b_qs = Bd["qs"]
                        ke, b_ke = Bd["ke"]
                        tt("dve", h4(qs[:, :]), h4(qv), bc(eg), ALU.mult, [b_qkv, b_eg], [b_qs])
                        tt("pool", h4(ke[:, :]), h4(kv), bc(kend), ALU.mult, [b_qkv, b_kend], [b_ke])
                        if mixer == "d":
                            kb, b_kb = Bd["kb"]
                            tt("dve", h4(kb[:, :]), h4(kv), bc(beta), ALU.mult, [b_qkv, b_beta], [b_kb])
                            R, b_R = Bd["R"][0]
                            tt("dve", R[:, :, 0:128], h4(vv), bc(beta), ALU.mult, [b_qkv, b_beta], [b_R])
                            tt("pool", R[:, :, 128:256], h4(kv), bc(beg), ALU.mult, [b_qkv, b_beg], [b_R])
                        else:
                            v1, b_v1 = Bd["v1"]
                            cp("pool", v1[:, :, 0:128], h4(vv), [b_qkv], [b_v1])
                            if mixer == "m":
                                mset("pool", v1[:, :, 128:129], 1.0, [b_v1])
                        yield
                        QTt, b_QTt = Bd["QT"]
                        QtT, b_QtT = Bd["QtT"]
                        KTt, b_KTt = Bd["KT"]
                        transpose4(qv, b_qkv, QTt, b_QTt)
                        yield
                        transpose4(kv, b_qkv, KTt, b_KTt)
                        yield
                        transpose4(qs, b_qs, QtT, b_QtT)
                        yield
                        if mixer == "d":
                            KbT, b_KbT = Bd["KbT"]
                            transpose4(kb, b_kb, KbT, b_KbT)
                            yield
                        ep, b_ep = Bd["ep"]
                        tt("dve", h4(ep[:, :]), h4(tri[d][0][:, :]), bc(lf), ALU.mult, [tri[d][1], b_lf], [b_ep])
                        psE, b_psE = psB.next()
                        mm(psE[:, :], ones[:, :], ep[:, :], True, False, [b_ones, b_ep], [b_psE])
                        mm(psE[:, :], ident[:, :], mbig[d][0][:, :], False, True, [b_ident, mbig[d][1]], [b_psE])
                        DT, b_DT = Bd["DT"]
                        for h in range(4):
                            act(DT[:, h * 128:(h + 1) * 128], psE[:, h * 128:(h + 1) * 128], AF.Exp, [b_psE, b_rowp], [b_DT], bias=rowp[:, n, h:h + 1])
                        yield
                        psQ, b_psQ = psB.next()
                        for h in range(4):
                            mm(psQ[:, h * 128:(h + 1) * 128], KTt[:, h * 128:(h + 1) * 128], QTt[:, h * 128:(h + 1) * 128], True, True, [b_KTt, b_QTt], [b_psQ])
                        QKT, b_QKT = Bd["QKT"]
                        tt("dve", QKT[:, :], psQ[:, :], DT[:, :], ALU.mult, [b_psQ, b_DT], [b_QKT])
                        yield
                        ob, b_ob = Bd["ob"]
                        St, b_St = state[d]
                        Sb, b_Sb = stateb[d]
                        if mixer == "d":
                            psK, b_psK = psB.next()
                            for h in range(4):
                                mm(psK[:, h * 128:(h + 1) * 128], KTt[:, h * 128:(h + 1) * 128], KbT[:, h * 128:(h + 1) * 128], True, True, [b_KTt, b_KbT], [b_psK])
                            ai = 0
                            AT, b_AT = Bd["AT"][0]
                            A, b_A = Bd["A"][0]
                            tt("dve", AT[:, :], psK[:, :], DT[:, :], ALU.mult, [b_psK, b_DT], [b_AT])
                            tt("pool", AT[:, :], AT[:, :], offd[:, :], ALU.mult, [b_AT, b_offd], [b_AT])
                            yield
                            psT, b_psT = psA.next()
                            for h in range(4):
                                S.op("pe", (lambda psT=psT, h=h, AT=AT: lambda e: e.transpose(out=psT[:, h * 128:(h + 1) * 128], in_=AT[:, h * 128:(h + 1) * 128], identity=ident[:]))(),
                                     [b_AT, b_ident], [b_psT])
                            cp("act", A[:, :], psT[:, :], [b_psT], [b_A])
                            yield
                            ri = 0
                            for lev in range(6):
                                p1, b_p1 = psB.next()
                                p2, b_p2 = psB.next()
                                for h in range(4):
                                    pp, b_pp = (p1, b_p1) if h < 2 else (p2, b_p2)
                                    mm(pp[:, (h % 2) * 256:(h % 2) * 256 + 256], AT[:, h * 128:(h + 1) * 128], R[:, h, :], True, True, [b_AT, b_R], [b_pp])
                                ri = (ri + 1) % 3
                                Rn, b_Rn = Bd["R"][ri]
                                op = ALU.subtract if lev == 0 else ALU.add
                                tt("dve", Rn[:, 0:2, :], R[:, 0:2, :], p1[:, :].rearrange("p (h e) -> p h e", h=2), op, [b_R, b_p1], [b_Rn])
                                tt("dve", Rn[:, 2:4, :], R[:, 2:4, :], p2[:, :].rearrange("p (h e) -> p h e", h=2), op, [b_R, b_p2], [b_Rn])
                                R, b_R = Rn, b_Rn
                                yield
                                if lev < 5:
                                    pA, b_pA = psA.next()
                                    pT, b_pT = psA.next()
                                    for h in range(4):
                                        hs = slice(h * 128, (h + 1) * 128)
                                        mm(pA[:, hs], AT[:, hs], A[:, hs], True, True, [b_AT, b_A], [b_pA])
                                        mm(pT[:, hs], A[:, hs], AT[:, hs], True, True, [b_AT, b_A], [b_pT])
                                    ai = (ai + 1) % 3
                                    A2, b_A2 = Bd["A"][ai]
                                    AT2, b_AT2 = Bd["AT"][ai]
                                    cp("act", A2[:, :], pA[:, :], [b_pA], [b_A2])
                                    cp("dve", AT2[:, :], pT[:, :], [b_pT], [b_AT2])
                                    A, b_A, AT, b_AT = A2, b_A2, AT2, b_AT2
                                    yield
                            wk, b_wk = Bd["wk"]
                            cp("pool", h4(wk[:, :]), R[:, :, 128:256], [b_R], [b_wk])
                            WkT, b_WkT = Bd["WkT"]
                            transpose4(wk, b_wk, WkT, b_WkT)
                            yield
                        while done_seq[d] < kseq:
                            yield
                        for ci, c in enumerate((0, 1) if d == 0 else (1, 0)):
                            P = slice(c * 64, c * 64 + 64)
                            if mixer == "d":
                                psU, b_psU = psB.next()
                                for h in range(4):
                                    mm(psU[:, h * 128:(h + 1) * 128], WkT[:, h * 128:(h + 1) * 128], Sb[:, h, 0:128], True, True, [b_WkT, b_Sb], [b_psU])
                                Ut, b_Ut = Bd["U"][ci]
                                tt("dve", Ut[P, :].rearrange("p (h e) -> p h e", h=4), R[P, :, 0:128], psU[P, :].rearrange("p (h e) -> p h e", h=4),
                                   ALU.subtract, [b_R, b_psU], [b_Ut])
                                rhsU = (lambda Ut=Ut, P=P: lambda h: Ut[P, h * 128:(h + 1) * 128])()
                                b_rhs = b_Ut
                                yield
                            else:
                                rhsU = (lambda v1=v1, P=P: lambda h: v1[P, h, 0:VW])()
                                b_rhs = b_v1
                            po1, b_po1 = psB.next()
                            po2, b_po2 = psB.next()
                            ps1, b_ps1 = psA.next()
                            ps2, b_ps2 = psA.next()
                            for h in range(4):
                                po, b_po = (po1, b_po1) if h < 2 else (po2, b_po2)
                                o_ap = po[:, (h % 2) * 256:(h % 2) * 256 + VW]
                                mm(o_ap, QtT[:, h * 128:(h + 1) * 128], Sb[:, h, 0:VW], True, False, [b_QtT, b_Sb], [b_po])
                                mm(o_ap, QKT[P, h * 128:(h + 1) * 128], rhsU(h), False, True, [b_QKT, b_rhs], [b_po])
                            for h in range(4):
                                pss, b_pss = (ps1, b_ps1) if h < 2 else (ps2, b_ps2)
                                mm(pss[:, (h % 2) * 256:(h % 2) * 256 + VW], ke[P, h * 128:(h + 1) * 128], rhsU(h), True, True, [b_ke, b_rhs], [b_pss])
                            for h in range(4):
                                pss, b_pss = (ps1, b_ps1) if h < 2 else (ps2, b_ps2)
                                stt(St[:, h, 0:VW], St[:, h, 0:VW], gam[:, n, c * 4 + h:c * 4 + h + 1], pss[:, (h % 2) * 256:(h % 2) * 256 + VW],
                                    ALU.mult, ALU.add, [b_St, b_gam, b_pss], [b_St])
                            cp("act", Sb[:, :, 0:VW], St[:, :, 0:VW], [b_St], [b_Sb])
                            for hp in range(2):
                                po, b_po = (po1, b_po1) if hp == 0 else (po2, b_po2)
                                po3 = po[:, :].rearrange("p (h e) -> p h e", h=2)
                                o3 = ob[:, hp * 256:(hp + 1) * 256].rearrange("p (h e) -> p h e", h=2)
                                if mixer == "m":
                                    st2, b_st2 = st_p.next()
                                    act(st2[P, 0:2], po3[P, :, 128], AF.Abs, [b_po], [b_st2])
                                    ts("dve", st2[P, 0:2], st2[P, 0:2], 1.0, None, ALU.max, None, [b_st2], [b_st2])
                                    recip(st2[P, 0:2], st2[P, 0:2], [b_st2], [b_st2])
                                    tt("dve", o3[P], po3[P, :, 0:128], st2[P, 0:2].unsqueeze(2).to_broadcast([64, 2, 128]), ALU.mult, [b_po, b_st2], [b_ob])
                                else:
                                    cp("act", o3[P], po3[P, :, 0:128], [b_po], [b_ob])
                            yield
                        done_seq[d] += 1
                        dma(OUT[d][rows, :], ob[:, :], [b_ob], [DB("O%s%d" % (mixer, d), n)], q="act")

                    done_seq = [0, 0]
                    order = []
                    for i in range(NT):
                        order.append((fwd_order[i], 0, i))
                        order.append((bwd_order[i], 1, i))
                    active = []
                    free = list(range(KSLOT))
                    nxt = 0
                    while nxt < len(order) or active:
                        while free and nxt < len(order):
                            si = free.pop(0)
                            n_, d_, k_ = order[nxt]
                            nxt += 1
                            active.append((si, unit(n_, d_, slots[si], k_)))
                        still = []
                        for si, g_ in active:
                            try:
                                next(g_)
                                still.append((si, g_))
                            except StopIteration:
                                free.append(si)
                        active = still
                    S.barrier()

            with contextlib.ExitStack() as es:
                wo, b_wo = sb("c_wo", [128, KC, D], BF16, es=es)
                hw, b_hw = sb("c_hw", [128, 1536], es=es)
                gp = [sb("c_gp%d" % w, [128, D], es=es) for w in range(2)]
                wov = w_out[l].rearrange("(k p) n -> p k n", p=128)
                with contextlib.ExitStack() as es2:
                    wst_p = TPool(es2, nc, "c_wst", [128, 4, 512], F32, 2)
                    for k4 in range(0, KC, 4):
                        for cbk in range(4):
                            wst, b_wst = wst_p.next()
                            dma(wst[:, :, :], wov[:, k4:k4 + 4, cbk * 512:(cbk + 1) * 512], [], [b_wst])
                            cp("pool" if cbk % 2 else "dve", wo[:, k4:k4 + 4, cbk * 512:(cbk + 1) * 512], wst[:, :, :], [b_wst], [b_wo])
                    dma(hw[:], hnw[l:l + 1, :].partition_broadcast(128), [], [b_hw])
                    for w in range(2):
                        dma(gp[w][0][:], GPW[l * 2 + w:l * 2 + w + 1, :].partition_broadcast(128), [DB("GPW", l)], [gp[w][1]])
                    S.barrier()
                sq_p = TPool(es, nc, "c_sq", [128, 512], F32, 3)
                st_p = TPool(es, nc, "c_st", [128, 8], F32, 6)
                ps_p = TPool(es, nc, "c_ps", [128, 512], F32, 4, psum=True)
                po_p = TPool(es, nc, "c_po", [128, 512], F32, 4, psum=True)
                cslots = []
                for si in range(2):
                    Bc = {}
                    Bc["z"] = sb("c%d_z" % si, [128, 2560], BF16, es=es)
                    Bc["zf"] = sb("c%d_zf" % si, [128, 2560], F32, es=es)
                    Bc["o"] = [sb("c%d_o%d" % (si, j), [128, 512], F32, es=es) for j in range(7)]
                    Bc["y"] = sb("c%d_y" % si, [128, D], F32, es=es)
                    Bc["yT"] = sb("c%d_yT" % si, [128, KC, 128], BF16, es=es)
                    Bc["x"] = sb("c%d_x" % si, [128, D], F32, es=es)
                    cslots.append(Bc)

                def ctile(n, Bc):
                    which = 1 if n < NCT else 0
                    rows = slice(n * 128, (n + 1) * 128)
                    z, b_z = Bc["z"]
                    zf, b_zf = Bc["zf"]
                    y, b_y = Bc["y"]
                    yT, b_yT = Bc["yT"]
                    xt, b_xt = Bc["x"]
                    dma(z[:, 0:1024], U[rows, OFF["m_o"]:OFF["m_o"] + 1024], [DB("U", n)], [b_z])
                    dma(z[:, 1024:1536], U[rows, OFF["r_z"]:OFF["r_z"] + 512], [DB("U", n)], [b_z])
                    dma(z[:, 1536:2048], U[rows, OFF["a_z"]:OFF["a_z"] + 512], [DB("U", n)], [b_z])
                    dma(z[:, 2048:2560], U[rows, OFF["d_z"]:OFF["d_z"] + 512], [DB("U", n)], [b_z])
                    obufs = {}
                    oi = 0
                    for mx, OUTS in (("m", OM), ("r", OR), ("d", OD)):
                        for d_ in range(2):
                            o_, b_o = Bc["o"][oi]
                            oi += 1
                            dma(o_[:, :], OUTS[d_][rows, :], [DB("O%s%d" % (mx, d_), n)], [b_o])
                            obufs[(mx, d_)] = (o_, b_o)
                    o_, b_o = Bc["o"][6]
                    dma(o_[:, :], OA[rows, :], [DB("OA", n)], [b_o])
                    obufs[("a", 0)] = (o_, b_o)
                    src, sr = xsrc(n)
                    dma(xt[:], src, sr, [b_xt])
                    yield
                    act(zf[:, 0:512], z[:, 0:512], AF.Sigmoid, [b_z], [b_zf])
                    act(zf[:, 512:2560], z[:, 512:2560], AF.Silu, [b_z], [b_zf])
                    yield
                    for gi, mx in enumerate(("m", "r", "a", "d")):
                        ycol = y[:, gi * 512:(gi + 1) * 512]
                        if mx == "a":
                            o0, b_o0 = obufs[("a", 0)]
                            tt("dve", ycol, o0[:, :], zf[:, 1536:2048], ALU.mult, [b_o0, b_zf], [b_y])
                            continue
                        o0, b_o0 = obufs[(mx, 0)]
                        o1, b_o1 = obufs[(mx, 1)]
                        st, b_st = st_p.next()
                        tt("pool", o0[:, :], o0[:, :], o1[:, :], ALU.add, [b_o0, b_o1], [b_o0])
                        if mx == "m":
                            tt("pool", o0[:, :], o0[:, :], zf[:, 0:512], ALU.mult, [b_o0, b_zf], [b_o0])
                        sq, b_sq = sq_p.next()
                        tt("dve", sq[:, :], o0[:, :], o0[:, :], ALU.mult, [b_o0], [b_sq])
                        red(st[:, 0:4], sq[:, :].rearrange("p (h d) -> p h d", h=4), [b_sq], [b_st])
                        yield
                        rsqrt_inplace(st[:, 0:4], 1.0 / 128, EPS, [b_st])
                        yield
                        hoff = dict(m=0, r=512, d=1024)[mx]
                        zoff = dict(m=512, r=1024, d=2048)[mx]
                        tt("dve", o0[:, :].rearrange("p (h d) -> p h d", h=4), o0[:, :].rearrange("p (h d) -> p h d", h=4),
                           st[:, 0:4].unsqueeze(2).to_broadcast([128, 4, 128]), ALU.mult, [b_o0, b_st], [b_o0])
                        tt("pool", o0[:, :], o0[:, :], hw[:, hoff:hoff + 512], ALU.mult, [b_o0, b_hw], [b_o0])
                        tt("dve", ycol, o0[:, :], zf[:, zoff:zoff + 512], ALU.mult, [b_o0, b_zf], [b_y])
                        yield
                    for k4 in range(4):
                        ps, b_ps = ps_p.next()
                        for j in range(4):
                            k = k4 * 4 + j
                            S.op("pe", (lambda ps=ps, j=j, k=k, y=y: lambda e: e.transpose(out=ps[:, j * 128:(j + 1) * 128], in_=y[:, k * 128:(k + 1) * 128], identity=ident[:]))(),
                                 [b_y, b_ident], [b_ps])
                        cp("act", yT[:, k4 * 4:k4 * 4 + 4, :], ps[:, :].rearrange("p (k t) -> p k t", k=4), [b_ps], [b_yT])
                    yield
                    pos = [po_p.next() for _ in range(4)]
                    for cbk in range(4):
                        po, b_po = pos[cbk]
                        for k in range(KC):
                            mm(po[:, :], yT[:, k, :], wo[:, k, cbk * 512:(cbk + 1) * 512], k == 0, k == KC - 1, [b_yT, b_wo], [b_po])
                    st2, b_st2 = st_p.next()
                    for cbk in range(4):
                        po, b_po = pos[cbk]
                        sq, b_sq = sq_p.next()
                        act(sq[:, :], po[:, :], AF.Square, [b_po], [b_sq, b_st2], accum=st2[:, cbk:cbk + 1])
                    red(st2[:, 4:5], st2[:, 0:4], [b_st2], [b_st2])
                    rsqrt_inplace(st2[:, 4:5], 1.0 / D, EPS, [b_st2])
                    for cbk in range(4):
                        po, b_po = pos[cbk]
                        cs = slice(cbk * 512, (cbk + 1) * 512)
                        sq, b_sq = sq_p.next()
                        stt(sq[:, :], po[:, :], st2[:, 4:5], gp[which][0][:, cs], ALU.mult, ALU.mult, [b_po, b_st2, gp[which][1]], [b_sq])
                        tt("pool", xt[:, cs], xt[:, cs], sq[:, :], ALU.add, [b_xt, b_sq], [b_xt])
                    if last:
                        dma(y_out[(n - NCT) * 128:(n - NCT + 1) * 128, :], xt[:], [b_xt], [DB("Y", n)], q="pool")
                    else:
                        dma(X1[rows, :], xt[:], [b_xt], [DB("X1", n)], q="pool")

                tiles = list(range(NCT, NT)) if last else list(range(NT))
                pend = list(tiles)
                cact = []
                cfree = [0, 1]
                tick = 0
                while pend or cact:
                    if pend and cfree and (not cact or tick >= 5):
                        si = cfree.pop(0)
                        cact.append((si, ctile(pend.pop(0), cslots[si])))
                    still = []
                    for si, g_ in cact:
                        try:
                            next(g_)
                            still.append((si, g_))
                        except StopIteration:
                            cfree.append(si)
                    cact = still
                    tick += 1
                S.barrier()

            if debug and l == 0:
                with contextlib.ExitStack() as es:
                    bt, b_bt = sb("dbg_t", [128, 2048], es=es)
                    btb, b_btb = sb("dbg_tb", [128, 2048], BF16, es=es)
                    for n in range(NT):
                        rows = slice(n * 128, (n + 1) * 128)
                        for c in range(4):
                            dma(btb[:, :], U[rows, c * 2048:(c + 1) * 2048], [], [b_btb], q="sp")
                            dma(dbg["U"][rows, c * 2048:(c + 1) * 2048], btb[:, :], [b_btb], [DB("dbgU", n)], q="sp")
                        for nm, src in (("UG", UG), ("OM0", OM[0]), ("OM1", OM[1]), ("OR0", OR[0]), ("OR1", OR[1]), ("OD0", OD[0]), ("OD1", OD[1]), ("OA", OA), ("X1", X1)):
                            w_ = src.shape[1]
                            dma(bt[:, 0:w_], src[rows, :], [], [b_bt], q="sp")
                            dma(dbg[nm][rows, :], bt[:, 0:w_], [b_bt], [DB("dbg" + nm, n)], q="sp")
                    S.barrier()

        S.barrier()
        S.emit()
    return nc


def _consts(NLT):
    idx = np.arange(128)
    same = (idx[:, None] // 64) == (idx[None, :] // 64)
    tri0 = (same & (idx[:, None] <= idx[None, :])).astype(np.float32)
    tri1 = (same & (idx[:, None] >= idx[None, :])).astype(np.float32)
    tri = np.stack([np.tile(tri0, (1, 4)), np.tile(tri1, (1, 4))]).astype(np.float32)
    mbig = ((tri - 1.0) * BIG).astype(np.float32)
    blk = same.astype(np.float32)
    chm = np.zeros((128, 8), np.float32)
    chm[:64, 0:4] = 1.0
    chm[64:, 4:8] = 1.0
    offd = np.tile(1.0 - np.eye(128, dtype=np.float32), (1, 4)).astype(np.float32)
    sel = np.zeros((2, 2, 128), np.float32)
    sel[0, 0, :] = 1.0
    sel[1, 1, :] = 1.0
    L = NLT * 128
    t = np.arange(L)
    row, col = t // 64, t % 64
    inv = (10000.0 ** (-np.arange(32, dtype=np.float32) / 32)).astype(np.float32)
    ang = np.stack([row[:, None].astype(np.float32) * inv, col[:, None].astype(np.float32) * inv], axis=1)
    cos, sin = np.cos(ang).astype(np.float32), np.sin(ang).astype(np.float32)
    cf = np.zeros((L, 2, 2, 32), np.float32)
    sf = np.zeros((L, 2, 2, 32), np.float32)
    cf[:, :, 0, :] = cos
    cf[:, :, 1, :] = cos
    sf[:, :, 0, :] = -sin
    sf[:, :, 1, :] = sin
    return dict(c_ident=np.eye(128, dtype=np.float32), c_ones=np.ones((128, 128), np.float32), c_tri=tri, c_mbig=mbig,
                c_blk=blk, c_chm=chm, c_offd=offd, c_sel=sel, ropec=cf.reshape(L, 128), ropes=sf.reshape(L, 128))


_PROG = {}


def run(inputs, NCT, NLT, DEPTH, debug=False, n_cores=8):
    key = (NCT, NLT, DEPTH, debug)
    if key not in _PROG:
        _PROG[key] = build_program(NCT, NLT, DEPTH, debug)
    nc = _PROG[key]
    f = lambda a: np.ascontiguousarray(np.asarray(a, dtype=np.float32))
    B = inputs["x"].shape[0]
    shared = dict(
        ada_w=f(inputs["ada_w"]), ada_b=f(inputs["ada_b"]), pre_w=f(inputs["pre_norm_w"]), post_w=f(inputs["post_norm_w"]),
        w_in=f(inputs["w_in"]), w_out=f(inputs["w_out"]),
        mib=f(inputs["mlstm_i_bias"]).reshape(DEPTH, 8), mfb=f(inputs["mlstm_f_bias"]).reshape(DEPTH, 8),
        rlg=f(inputs["ret_log_gamma"]).reshape(DEPTH, 8), qnw=f(inputs["attn_q_norm_w"]), knw=f(inputs["attn_k_norm_w"]),
        convw=f(inputs["dn_conv_w"]), alog=f(inputs["dn_a_log"]).reshape(DEPTH, 8), dtb=f(inputs["dn_dt_bias"]).reshape(DEPTH, 8),
        hnw=f(inputs["head_norm_w"]))
    shared.update(_consts(NLT))
    cc = f(inputs["c_ctx"]).reshape(16, 128)
    in_maps = []
    for i in range(n_cores):
        b = i % B
        m = dict(shared)
        m["x"] = f(inputs["x"][b])
        m["ctx"] = f(inputs["ctx"][b])
        m["cvec"] = np.ascontiguousarray(np.concatenate([f(inputs["c"][b]).reshape(16, 128), cc], axis=0))
        in_maps.append(m)
    res = run_bass_kernel_spmd(nc, in_maps, core_ids=list(range(n_cores)))
    return res


def kernel(**inputs):
    res = run(inputs, 2, 32, 2)
    B = inputs["x"].shape[0]
    out = np.stack([np.asarray(res.results[b]["y"], dtype=np.float32) for b in range(B)], axis=0)
    return out
```

```python
import contextlib
import math
import numpy as np
import ml_dtypes
import concourse.bass as bass
import concourse.mybir as mybir
from concourse.bass_utils import run_bass_kernel_spmd

F32 = mybir.dt.float32
BF16 = mybir.dt.bfloat16
AF = mybir.ActivationFunctionType
ALU = mybir.AluOpType
AX = mybir.AxisListType

EPOCH = 30000
DMA_RING = 8
BIG = 30000.0
D = 2048
KC = 16
EPS = 1e-6


class Buf:
    __slots__ = ("name", "w", "r")

    def __init__(self, name=""):
        self.name = name
        self.w = None
        self.r = []


class Sched:
    def __init__(self, nc, same_engine_sync=True):
        self.nc = nc
        self.same = same_engine_sync
        self.streams = {e: [] for e in ("pe", "act", "dve", "pool", "sp")}
        self.cnt = {e: 0 for e in self.streams}
        self.dcnt = {q: 0 for q in self.streams}
        self.known = {e: {} for e in self.streams}
        self.semkeys = set()

    def _tok_sem(self, tok):
        if tok[0] == "c":
            _, e, n = tok
            return ("c", e, (n - 1) // EPOCH), (n - 1) % EPOCH + 1
        _, q, i = tok
        return ("d", q, i % DMA_RING), 16 * (i // DMA_RING + 1)

    def _need(self, eng, tok, waits):
        if tok is None:
            return
        if tok[0] == "c" and tok[1] == eng and (eng == "pe" or not self.same):
            return
        key, val = self._tok_sem(tok)
        if self.known[eng].get(key, 0) >= val:
            return
        if waits.get(key, 0) < val:
            waits[key] = val

    def _deps(self, eng, reads, writes):
        waits = {}
        for b in reads:
            self._need(eng, b.w, waits)
        for b in writes:
            self._need(eng, b.w, waits)
            for t in b.r:
                if t[0] == "c" and t[1] == eng:
                    continue
                self._need(eng, t, waits)
        for k, v in waits.items():
            self.known[eng][k] = v
            self.semkeys.add(k)
        return waits

    def _commit(self, tok, reads, writes):
        for b in reads:
            b.r.append(tok)
        for b in writes:
            b.w = tok
            b.r = []

    def op(self, eng, fn, reads=(), writes=()):
        waits = self._deps(eng, reads, writes)
        self.cnt[eng] += 1
        tok = ("c", eng, self.cnt[eng])
        key, _ = self._tok_sem(tok)
        self.semkeys.add(key)
        self.streams[eng].append((waits, fn, key, 1))
        self._commit(tok, reads, writes)
        return tok

    def dma(self, q, fn, reads=(), writes=()):
        waits = self._deps(q, reads, writes)
        i = self.dcnt[q]
        self.dcnt[q] += 1
        tok = ("d", q, i)
        key, val = self._tok_sem(tok)
        self.semkeys.add(key)
        if i >= DMA_RING:
            pkey, pval = self._tok_sem(("d", q, i - DMA_RING))
            if self.known[q].get(pkey, 0) < pval:
                waits[pkey] = max(waits.get(pkey, 0), pval)
                self.known[q][pkey] = pval
        self.streams[q].append((waits, fn, key, 16))
        self._commit(tok, reads, writes)
        return tok

    def barrier(self):
        toks = []
        for e in self.streams:
            if self.cnt[e] > 0:
                toks.append(("c", e, self.cnt[e]))
            for i in range(max(0, self.dcnt[e] - DMA_RING), self.dcnt[e]):
                toks.append(("d", e, i))
        for e in self.streams:
            waits = {}
            for t in toks:
                if t[0] == "c" and t[1] == e:
                    continue
                self._need(e, t, waits)
            for k, v in waits.items():
                self.known[e][k] = v
                self.semkeys.add(k)
            self.streams[e].append((waits, None, None, 0))

    def emit(self):
        nc = self.nc
        keys = sorted(self.semkeys)
        with contextlib.ExitStack() as es:
            sems = {}
            for k in keys:
                sems[k] = es.enter_context(nc.semaphore("s_%s_%s_%d" % k))
            block = es.enter_context(nc.Block())

            def run(engname):
                def body(eng):
                    for waits, fn, key, inc in self.streams[engname]:
                        for k, v in waits.items():
                            eng.wait_ge(sems[k], v)
                        if fn is not None:
                            fn(eng).then_inc(sems[key], inc)
                return body
            block.tensor(run("pe"))
            block.scalar(run("act"))
            block.vector(run("dve"))
            block.gpsimd(run("pool"))
            block.sync(run("sp"))


_UID = [0]


def _uname(name):
    _UID[0] += 1
    return "%s_u%d" % (name, _UID[0])


class TPool:
    def __init__(self, es, nc, name, shape, dtype, n, psum=False):
        self.tiles = []
        for i in range(n):
            mk = nc.psum_tensor if psum else nc.sbuf_tensor
            t = es.enter_context(mk(_uname("%s%d" % (name, i)), shape, dtype))
            self.tiles.append((t, Buf("%s%d" % (name, i))))
        self.i = 0

    def next(self):
        t = self.tiles[self.i % len(self.tiles)]
        self.i += 1
        return t


OFF = dict(m_q=0, m_k=512, m_v=1024, m_o=1536, m_z=2048, r_q=2560, r_k=3072, r_v=3584, r_z=4096,
           a_q=4608, a_k=5120, a_v=5376, a_z=5632, d_q=6144, d_k=6656, d_v=7168, d_z=7680)
UW = 8192


def build_program(NCT, NLT, DEPTH, debug=False):
    NT = NCT + NLT
    T = NT * 128
    nc = bass.Bass("TRN2", target_bir_lowering=False)
    S = Sched(nc)

    def din(name, shape, dt=F32):
        return nc.dram_tensor(name, list(shape), dt, kind="ExternalInput").ap()

    def dscr(name, shape, dt=F32):
        return nc.dram_tensor(name, list(shape), dt, kind="Internal").ap()

    x_in = din("x", [NLT * 128, D])
    ctx_in = din("ctx", [NCT * 128, D])
    cvec = din("cvec", [32, 128])
    ada_w = din("ada_w", [DEPTH, D, 3 * D])
    ada_b = din("ada_b", [DEPTH, 3 * D])
    pre_w = din("pre_w", [DEPTH, D])
    post_w = din("post_w", [DEPTH, D])
    w_in = din("w_in", [DEPTH, D, 8224])
    w_out = din("w_out", [DEPTH, D, D])
    mib = din("mib", [DEPTH, 8])
    mfb = din("mfb", [DEPTH, 8])
    rlg = din("rlg", [DEPTH, 8])
    qnw = din("qnw", [DEPTH, 128])
    knw = din("knw", [DEPTH, 128])
    convw = din("convw", [DEPTH, 5, 1536])
    alog = din("alog", [DEPTH, 8])
    dtb = din("dtb", [DEPTH, 8])
    hnw = din("hnw", [DEPTH, 1536])
    c_ident = din("c_ident", [128, 128])
    c_ones = din("c_ones", [128, 128])
    c_tri = din("c_tri", [2, 128, 512])
    c_mbig = din("c_mbig", [2, 128, 512])
    c_blk = din("c_blk", [128, 128])
    c_chm = din("c_chm", [128, 8])
    c_offd = din("c_offd", [128, 512])
    c_sel = din("c_sel", [2, 2, 128])
    ropec = din("ropec", [NLT * 128, 128])
    ropes = din("ropes", [NLT * 128, 128])
    y_out = nc.dram_tensor("y", [NLT * 128, D], F32, kind="ExternalOutput").ap()

    X1 = dscr("X1", [T, D])
    U = dscr("U", [T, UW], BF16)
    UG = dscr("UG", [T, 32])
    OM = [dscr("OM%d" % d, [T, 512]) for d in range(2)]
    OR = [dscr("OR%d" % d, [T, 512]) for d in range(2)]
    OD = [dscr("OD%d" % d, [T, 512]) for d in range(2)]
    OA = dscr("OA", [T, 512])
    DQ = dscr("DQ", [T, 1536], BF16)
    dbg = {}
    if debug:
        dbg["U"] = nc.dram_tensor("dbgU", [T, UW], BF16, kind="ExternalOutput").ap()
        dbg["UG"] = nc.dram_tensor("dbgUG", [T, 32], F32, kind="ExternalOutput").ap()
        for nm in ("OM0", "OM1", "OR0", "OR1", "OD0", "OD1", "OA"):
            dbg[nm] = nc.dram_tensor("dbg" + nm, [T, 512], F32, kind="ExternalOutput").ap()
        dbg["X1"] = nc.dram_tensor("dbgX1", [T, D], F32, kind="ExternalOutput").ap()

    dbufs = {}

    def DB(name, n):
        k = (name, n)
        if k not in dbufs:
            dbufs[k] = Buf("%s_%d" % k)
        return dbufs[k]

    def mm(out, lhsT, rhs, start, stop, R, W):
        S.op("pe", lambda e: e.matmul(out, lhsT=lhsT, rhs=rhs, start=start, stop=stop), R, W)

    def act(out, in_, func, R, W, bias=None, scale=None, accum=None, eng="act"):
        kw = {}
        if bias is not None:
            kw["bias"] = bias
        if scale is not None:
            kw["scale"] = scale
        if accum is not None:
            kw["accum_out"] = accum
        S.op("act", lambda e: e.activation(out=out, in_=in_, func=func, **kw), R, W)

    def tt(eng, out, in0, in1, op, R, W):
        S.op(eng, lambda e: e.tensor_tensor(out=out, in0=in0, in1=in1, op=op), R, W)

    def ts(eng, out, in0, s1, s2, op0, op1, R, W):
        if s2 is None:
            S.op(eng, lambda e: e.tensor_scalar(out=out, in0=in0, scalar1=s1, scalar2=None, op0=op0), R, W)
        else:
            S.op(eng, lambda e: e.tensor_scalar(out=out, in0=in0, scalar1=s1, scalar2=s2, op0=op0, op1=op1), R, W)

    def stt(out, in0, scalar, in1, op0, op1, R, W):
        S.op("dve", lambda e: e.scalar_tensor_tensor(out=out, in0=in0, scalar=scalar, in1=in1, op0=op0, op1=op1), R, W)

    def cp(eng, out, in_, R, W):
        if eng == "act":
            S.op("act", lambda e: e.activation(out=out, in_=in_, func=AF.Copy), R, W)
        else:
            S.op(eng, lambda e: e.tensor_copy(out=out, in_=in_), R, W)

    def red(out, in_, R, W):
        S.op("dve", lambda e: e.tensor_reduce(out=out, in_=in_, axis=AX.X, op=ALU.add), R, W)

    def recip(out, in_, R, W):
        S.op("dve", lambda e: e.reciprocal(out=out, in_=in_), R, W)

    def mset(eng, ap, v, W):
        S.op(eng, lambda e: e.memset(ap, v), (), W)

    dq = [0]

    def dma(out, in_, R, W, q=None):
        if q is None:
            q = "sp"
            dq[0] += 1
        S.dma(q, lambda e: e.dma_start(out=out, in_=in_), R, W)

    def rsqrt_inplace(ap, tmp_scale, add, R):
        ts("dve", ap, ap, tmp_scale, add, ALU.mult, ALU.add, R, R)
        act(ap, ap, AF.Sqrt, R, R)
        recip(ap, ap, R, R)

    with contextlib.ExitStack() as es0:
        def sb(name, shape, dt=F32, es=es0):
            return es.enter_context(nc.sbuf_tensor(_uname(name), list(shape), dt)), Buf(name)

        ident, b_ident = sb("ident", [128, 128])
        ones, b_ones = sb("ones", [128, 128])
        tri = [sb("tri%d" % d, [128, 512]) for d in range(2)]
        mbig = [sb("mbig%d" % d, [128, 512]) for d in range(2)]
        blk, b_blk = sb("blk", [128, 128])
        chm, b_chm = sb("chm", [128, 8])
        offd, b_offd = sb("offd", [128, 512])
        dma(ident[:], c_ident[:, :], [], [b_ident])
        dma(ones[:], c_ones[:, :], [], [b_ones])
        for d in range(2):
            dma(tri[d][0][:], c_tri[d], [], [tri[d][1]])
            dma(mbig[d][0][:], c_mbig[d], [], [mbig[d][1]])
        dma(blk[:], c_blk[:, :], [], [b_blk])
        dma(chm[:], c_chm[:, :], [], [b_chm])
        dma(offd[:], c_offd[:, :], [], [b_offd])

        scT, b_scT = sb("scT", [128, DEPTH * 2 * 16])
        shT, b_shT = sb("shT", [128, DEPTH * 2 * 16])
        GPW = dscr("GPW", [DEPTH * 2, D])

        def scT_ap(l, which, k):
            i = (l * 2 + which) * 16 + k
            return scT[:, i:i + 1]

        def shT_ap(l, which, k):
            i = (l * 2 + which) * 16 + k
            return shT[:, i:i + 1]

        with contextlib.ExitStack() as es:
            pa, b_pa = sb("p0a", [64, 128], es=es)
            pb, b_pb = sb("p0b", [DEPTH * 48, 128], es=es)
            paT, b_paT = sb("p0aT", [128, 64], es=es)
            pbT, b_pbT = sb("p0bT", [128, DEPTH * 48], es=es)
            cs2, b_cs2 = sb("p0cs2", [128, 16, 2], es=es)
            modT, b_modT = sb("p0modT", [128, 48, 2], es=es)
            grow, b_grow = sb("p0grow", [2, D], es=es)
            brow, b_brow = sb("p0brow", [2, D], es=es)
            prow, b_prow = sb("p0prow", [2, D], es=es)
            slabs = TPool(es, nc, "p0slab", [128, 16, 512], F32, 2)
            ps_t = es.enter_context(nc.psum_tensor(_uname("p0ps_t"), [128, 512], F32)); b_ps_t = Buf()
            ps_m = es.enter_context(nc.psum_tensor(_uname("p0ps_m"), [128, 512], F32)); b_ps_m = Buf()
            ps_g = TPool(es, nc, "p0ps_g", [128, 512], F32, 2, psum=True)

            dma(pa[0:32, :], cvec[:, :], [], [b_pa])
            dma(pa[32:32 + DEPTH * 16, :], pre_w.rearrange("l (k p) -> (l k) p", p=128), [], [b_pa])
            dma(pb[:], ada_b.rearrange("l (k p) -> (l k) p", p=128), [], [b_pb])
            act(pa[0:32, :], pa[0:32, :], AF.Silu, [b_pa], [b_pa])
            S.op("pe", lambda e: e.transpose(out=ps_t[:, 0:64], in_=pa[:, :], identity=ident[0:64, 0:64]), [b_pa, b_ident], [b_ps_t])
            cp("dve", paT[:], ps_t[:, 0:64], [b_ps_t], [b_paT])
            S.op("pe", lambda e: e.transpose(out=ps_t[:, 128:128 + DEPTH * 48], in_=pb[:, :], identity=ident[0:DEPTH * 48, 0:DEPTH * 48]), [b_pb, b_ident], [b_ps_t])
            cp("dve", pbT[:], ps_t[:, 128:128 + DEPTH * 48], [b_ps_t], [b_pbT])
            cp("dve", cs2[:, :, 0], paT[:, 0:16], [b_paT], [b_cs2])
            cp("dve", cs2[:, :, 1], paT[:, 16:32], [b_paT], [b_cs2])
            for l in range(DEPTH):
                dma(brow[0:1, :], ada_b[l:l + 1, 2 * D:3 * D], [], [b_brow])
                dma(brow[1:2, :], ada_b[l:l + 1, 2 * D:3 * D], [], [b_brow])
                dma(prow[0:1, :], post_w[l:l + 1, :], [], [b_prow])
                dma(prow[1:2, :], post_w[l:l + 1, :], [], [b_prow])
                awv = ada_w[l].rearrange("(k p) n -> p k n", p=128)
                for sl in range(12):
                    slab, b_slab = slabs.next()
                    for kk in range(0, 16, 4):
                        dma(slab[:, kk:kk + 4, :], awv[:, kk:kk + 4, sl * 512:(sl + 1) * 512], [], [b_slab])
                    for j in range(4):
                        cb = sl * 4 + j
                        for k in range(KC):
                            mm(ps_m[:, cb * 2:cb * 2 + 2], slab[:, k, j * 128:(j + 1) * 128], cs2[:, k, :],
                               k == 0, k == KC - 1, [b_slab, b_cs2], [b_ps_m])
                    if sl >= 8:
                        pg, b_pg = ps_g.next()
                        for k in range(KC):
                            mm(pg[0:2, :], cs2[:, k, :], slab[:, k, :], k == 0, k == KC - 1, [b_slab, b_cs2], [b_pg])
                        cc = (sl - 8) * 512
                        tt("dve", grow[:, cc:cc + 512], pg[0:2, :], brow[:, cc:cc + 512], ALU.add, [b_pg, b_brow], [b_grow])
                cp("dve", modT[:].rearrange("p a b -> p (a b)"), ps_m[:, 0:96], [b_ps_m], [b_modT])
                tt("dve", grow[:], grow[:], prow[:], ALU.mult, [b_grow, b_prow], [b_grow])
                dma(GPW[l * 2:l * 2 + 2, :], grow[:], [b_grow], [DB("GPW", l)])
                for which in range(2):
                    i0 = (l * 2 + which) * 16
                    tt("dve", shT[:, i0:i0 + 16], modT[:, 0:16, which], pbT[:, l * 48:l * 48 + 16], ALU.add, [b_modT, b_pbT], [b_shT])
                    tt("dve", scT[:, i0:i0 + 16], modT[:, 16:32, which], pbT[:, l * 48 + 16:l * 48 + 32], ALU.add, [b_modT, b_pbT], [b_scT])
                    stt(scT[:, i0:i0 + 16], scT[:, i0:i0 + 16], 1.0, paT[:, 32 + l * 16:48 + l * 16], ALU.add, ALU.mult, [b_scT, b_paT], [b_scT])
            S.barrier()

        for l in range(DEPTH):
            last = (l == DEPTH - 1)

            def xsrc(n):
                if l == 0:
                    return (ctx_in[n * 128:(n + 1) * 128, :] if n < NCT else x_in[(n - NCT) * 128:(n - NCT + 1) * 128, :]), []
                return X1[n * 128:(n + 1) * 128, :], [DB("X1", n)]

            with contextlib.ExitStack() as es:
                TBMAX = (NT + 1) // 2
                hT = [sb("a_hT%d" % i, [128, KC, 128], BF16, es=es) for i in range(TBMAX)]
                xt_p = TPool(es, nc, "a_x", [128, D], F32, 2)
                st_p = TPool(es, nc, "a_st", [128, 4], F32, 2)
                wst_p = TPool(es, nc, "a_wst", [128, KC, 512], F32, 2)
                wbf_p = TPool(es, nc, "a_wbf", [128, KC, 512], BF16, 2)
                ub_p = TPool(es, nc, "a_ub", [128, 512], BF16, 3)
                ug_p = TPool(es, nc, "a_ug", [128, 32], F32, 2)
                ps_p = TPool(es, nc, "a_ps", [128, 512], F32, 6, psum=True)
                wv = w_in[l].rearrange("(k p) n -> p k n", p=128)
                blocks = [list(range(0, TBMAX)), list(range(TBMAX, NT))]

                def prep_tile(i, n):
                    which = 1 if n < NCT else 0
                    xt, b_xt = xt_p.next()
                    st, b_st = st_p.next()
                    src, sr = xsrc(n)
                    dma(xt[:], src, sr, [b_xt])
                    ps, b_ps = ps_p.next()
                    for c4 in range(4):
                        act(ps[:, :], xt[:, c4 * 512:(c4 + 1) * 512], AF.Square, [b_xt], [b_ps, b_st], accum=st[:, c4:c4 + 1])
                    red(st[:, 0:1], st[:, 0:4], [b_st], [b_st])
                    rsqrt_inplace(st[:, 0:1], 1.0 / D, EPS, [b_st])
                    ts("dve", xt[:], xt[:], st[:, 0:1], None, ALU.mult, None, [b_xt, b_st], [b_xt])
                    ht, b_ht = hT[i]
                    for k4 in range(4):
                        ps, b_ps = ps_p.next()
                        for j in range(4):
                            k = k4 * 4 + j
                            S.op("pe", (lambda k=k, j=j, ps=ps, xt=xt: lambda e: e.transpose(out=ps[:, j * 128:(j + 1) * 128], in_=xt[:, k * 128:(k + 1) * 128], identity=ident[:]))(),
                                 [b_xt, b_ident], [b_ps])
                        for j in range(4):
                            k = k4 * 4 + j
                            act(ht[:, k, :], ps[:, j * 128:(j + 1) * 128], AF.Identity, [b_ps, b_scT, b_shT], [b_ht],
                                bias=shT_ap(l, which, k), scale=scT_ap(l, which, k))

                for bi, tb in enumerate(blocks):
                    if not tb:
                        continue
                    nxt_tb = blocks[bi + 1] if bi + 1 < len(blocks) else []
                    if bi == 0:
                        for i, n in enumerate(tb):
                            prep_tile(i, n)
                    def load_w(cb):
                        wst, b_wst = wst_p.next()
                        wbf, b_wbf = wbf_p.next()
                        if cb < 16:
                            o0 = cb * 512 + (16 if cb >= 5 else 0)
                            wdt = 512
                            for kk in range(0, 16, 4):
                                dma(wst[:, kk:kk + 4, :], wv[:, kk:kk + 4, o0:o0 + 512], [], [b_wst])
                        else:
                            wdt = 32
                            dma(wst[:, :, 0:16], wv[:, :, 2560:2576], [], [b_wst])
                            dma(wst[:, :, 16:32], wv[:, :, 8208:8224], [], [b_wst])
                        cp("dve", wbf[:, 0:8, 0:wdt], wst[:, 0:8, 0:wdt], [b_wst], [b_wbf])
                        cp("dve", wbf[:, 8:16, 0:wdt], wst[:, 8:16, 0:wdt], [b_wst], [b_wbf])
                        return wbf, b_wbf, wdt
                    cb_order = [16] + list(range(16))
                    nxt_w = load_w(cb_order[0])
                    for ci, cb in enumerate(cb_order):
                        wbf, b_wbf, wdt = nxt_w
                        if ci + 1 < 17:
                            nxt_w = load_w(cb_order[ci + 1])
                        for i, n in enumerate(tb):
                            ht, b_ht = hT[i]
                            ps, b_ps = ps_p.next()
                            for k in range(KC):
                                mm(ps[:, 0:wdt], ht[:, k, :], wbf[:, k, 0:wdt], k == 0, k == KC - 1, [b_ht, b_wbf], [b_ps])
                            if cb < 16:
                                ub, b_ub = ub_p.next()
                                cp("act", ub[:, :], ps[:, 0:512], [b_ps], [b_ub])
                                dma(U[n * 128:(n + 1) * 128, cb * 512:(cb + 1) * 512], ub[:, :], [b_ub], [DB("U", n)], q="act")
                            else:
                                ug, b_ug = ug_p.next()
                                cp("act", ug[:, :], ps[:, 0:32], [b_ps], [b_ug])
                                dma(UG[n * 128:(n + 1) * 128, :], ug[:, :], [b_ug], [DB("UG", n)], q="act")
                            if ci == 16 and i < len(nxt_tb):
                                prep_tile(i, nxt_tb[i])
                S.barrier()

            if debug and l == debug - 1:
                pass

            ctx_needed = not last
            fwd_order = list(range(NT))
            bwd_order = list(range(NCT - 1, -1, -1)) + list(range(NT - 1, NCT - 1, -1))

            with contextlib.ExitStack() as es:
                QT, b_QT = sb("at_QT", [128, 4, T], BF16, es=es)
                KT, b_KT = sb("at_KT", [128, 2, T], BF16, es=es)
                V1, b_V1 = sb("at_V1", [128, NT, 2, 132], BF16, es=es)
                nwq, b_nwq = sb("at_nwq", [128, 128], es=es)
                nwk, b_nwk = sb("at_nwk", [128, 128], es=es)
                st_p = TPool(es, nc, "at_st", [128, 8], F32, 4)
                pt_p = TPool(es, nc, "at_pt", [128, 512], BF16, 3)
                ob_p = TPool(es, nc, "at_ob", [128, 128], F32, 3)
                ps_p = TPool(es, nc, "at_ps", [128, 512], F32, 4, psum=True)
                acc_p = TPool(es, nc, "at_acc", [128, 512], F32, 4, psum=True)
                dma(nwq[:], qnw[l:l + 1, :].partition_broadcast(128), [], [b_nwq])
                dma(nwk[:], knw[l:l + 1, :].partition_broadcast(128), [], [b_nwk])
                mset("dve", V1[:, :, :, 128:129], 1.0, [b_V1])
                ts("dve", nwq[:], nwq[:], 128.0 ** -0.5, None, ALU.mult, None, [b_nwq], [b_nwq])

                with contextlib.ExitStack() as es2:
                    raw_p = TPool(es2, nc, "at_raw", [128, 1024], BF16, 3)
                    q_p = TPool(es2, nc, "at_q", [128, 768], F32, 3)
                    q2_p = TPool(es2, nc, "at_q2", [128, 768], F32, 3)
                    sw_p = TPool(es2, nc, "at_sw", [128, 768], F32, 3)
                    rc_p = TPool(es2, nc, "at_rc", [128, 128], F32, 3)
                    rs_p = TPool(es2, nc, "at_rs", [128, 128], F32, 3)

                    def a1_tile(n):
                        raw, b_raw = raw_p.next()
                        dma(raw[:, 0:1024], U[n * 128:(n + 1) * 128, OFF["a_q"]:OFF["a_q"] + 1024], [DB("U", n)], [b_raw])
                        q, b_q = q_p.next()
                        q2, b_q2 = q2_p.next()
                        st, b_st = st_p.next()
                        cp("dve", q[:, :], raw[:, 0:768], [b_raw], [b_q])
                        cp("pool", V1[:, n, :, 0:128], raw[:, 768:1024].rearrange("p (h d) -> p h d", h=2), [b_raw], [b_V1])
                        tt("dve", q2[:, :], q[:, :], q[:, :], ALU.mult, [b_q], [b_q2])
                        red(st[:, 0:6], q2[:, :].rearrange("p (h d) -> p h d", h=6), [b_q2], [b_st])
                        yield
                        rsqrt_inplace(st[:, 0:6], 1.0 / 128, EPS, [b_st])
                        yield
                        q3 = q[:, :].rearrange("p (h d) -> p h d", h=6)
                        tt("dve", q3, q3, st[:, 0:6].unsqueeze(2).to_broadcast([128, 6, 128]), ALU.mult, [b_q, b_st], [b_q])
                        tt("pool", q3[:, 0:4, :], q3[:, 0:4, :], nwq[:, :].unsqueeze(1).to_broadcast([128, 4, 128]), ALU.mult, [b_q, b_nwq], [b_q])
                        tt("pool", q3[:, 4:6, :], q3[:, 4:6, :], nwk[:, :].unsqueeze(1).to_broadcast([128, 2, 128]), ALU.mult, [b_q, b_nwk], [b_q])
                        yield
                        if n >= NCT:
                            rc, b_rc = rc_p.next()
                            rs, b_rs = rs_p.next()
                            sw, b_sw = sw_p.next()
                            r0 = (n - NCT) * 128
                            dma(rc[:], ropec[r0:r0 + 128, :], [], [b_rc])
                            dma(rs[:], ropes[r0:r0 + 128, :], [], [b_rs])
                            q4 = q[:, :].rearrange("p (g two f) -> p g two f", two=2, f=32)
                            s4 = sw[:, :].rearrange("p (g two f) -> p g two f", two=2, f=32)
                            cp("pool", s4[:, :, 0, :], q4[:, :, 1, :], [b_q], [b_sw])
                            cp("pool", s4[:, :, 1, :], q4[:, :, 0, :], [b_q], [b_sw])
                            s3 = sw[:, :].rearrange("p (h d) -> p h d", h=6)
                            tt("dve", q3, q3, rc[:, :].unsqueeze(1).to_broadcast([128, 6, 128]), ALU.mult, [b_q, b_rc], [b_q])
                            tt("pool", s3, s3, rs[:, :].unsqueeze(1).to_broadcast([128, 6, 128]), ALU.mult, [b_sw, b_rs], [b_sw])
                            yield
                            tt("dve", q3, q3, s3, ALU.add, [b_q, b_sw], [b_q])
                            yield
                        for half in range(2):
                            ps, b_ps = ps_p.next()
                            nh = 4 if half == 0 else 2
                            for j in range(nh):
                                hh = half * 4 + j
                                S.op("pe", (lambda ps=ps, j=j, q=q, hh=hh: lambda e: e.transpose(out=ps[:, j * 128:(j + 1) * 128], in_=q[:, hh * 128:(hh + 1) * 128], identity=ident[:]))(),
                                     [b_q, b_ident], [b_ps])
                            if half == 0:
                                cp("act", QT[:, :, n * 128:(n + 1) * 128], ps[:, 0:512].rearrange("p (h t) -> p h t", h=4), [b_ps], [b_QT])
                            else:
                                cp("act", KT[:, :, n * 128:(n + 1) * 128], ps[:, 0:256].rearrange("p (h t) -> p h t", h=2), [b_ps], [b_KT])

                    pend = list(range(NT))
                    act_g = []
                    while pend or act_g:
                        while pend and len(act_g) < 3:
                            act_g.append(a1_tile(pend.pop(0)))
                        alive = []
                        for g_ in act_g:
                            try:
                                next(g_)
                                alive.append(g_)
                            except StopIteration:
                                pass
                        act_g = alive
                    S.barrier()

                def attend(qtiles, ktiles):
                    for h in range(4):
                        kvh = h // 2
                        for qb0 in range(0, len(qtiles), 4):
                            qts = qtiles[qb0:qb0 + 4]
                            nq = len(qts)
                            q0 = qts[0] * 128
                            accs = [acc_p.next() for _ in range(nq)]
                            for si, s in enumerate(ktiles):
                                ps, b_ps = ps_p.next()
                                mm(ps[:, 0:nq * 128], KT[:, kvh, s * 128:(s + 1) * 128], QT[:, h, q0:q0 + nq * 128], True, True, [b_KT, b_QT], [b_ps])
                                pt, b_pt = pt_p.next()
                                act(pt[:, 0:nq * 128], ps[:, 0:nq * 128], AF.Exp, [b_ps], [b_pt])
                                for j in range(nq):
                                    mm(accs[j][0][:, 0:129], pt[:, j * 128:(j + 1) * 128], V1[:, s, kvh, 0:129], si == 0, si == len(ktiles) - 1,
                                       [b_pt, b_V1], [accs[j][1]])
                                yield
                            for j in range(nq):
                                ac, b_ac = accs[j]
                                ob, b_ob = ob_p.next()
                                st, b_st = st_p.next()
                                act(st[:, 0:1], ac[:, 128:129], AF.Ln, [b_ac], [b_st])
                                act(st[:, 0:1], st[:, 0:1], AF.Exp, [b_st], [b_st], scale=-1.0)
                                act(ob[:, :], ac[:, 0:128], AF.Identity, [b_ac, b_st], [b_ob], scale=st[:, 0:1])
                                n = qts[j]
                                dma(OA[n * 128:(n + 1) * 128, h * 128:(h + 1) * 128], ob[:, :], [b_ob], [DB("OA", n)], q="act")

                def gen_attn():
                    yield from attend(list(range(NCT, NT)), list(range(NT)))
                    if ctx_needed:
                        yield from attend(list(range(NCT)), list(range(NCT)))

                with contextlib.ExitStack() as es3:
                    cw = [sb("p_cw%d" % j, [128, 1536], es=es3) for j in range(5)]
                    for j in range(5):
                        dma(cw[j][0][:], convw[l, j:j + 1, :].partition_broadcast(128), [], [cw[j][1]])

                    def gen_prep(tiles, gi):
                        sh_p = TPool(es3, nc, "p%d_sh" % gi, [128, 1536], BF16, 2)
                        acc_p2 = TPool(es3, nc, "p%d_acc" % gi, [128, 1536], F32, 1)
                        tmp_p = TPool(es3, nc, "p%d_tmp" % gi, [128, 1536], F32, 2)
                        pst_p = TPool(es3, nc, "p%d_st" % gi, [128, 16], F32, 2)
                        pob_p = TPool(es3, nc, "p%d_ob" % gi, [128, 1536], BF16, 1)
                        rraw_p = TPool(es3, nc, "p%d_rraw" % gi, [128, 1024], BF16, 2)
                        rq_p = TPool(es3, nc, "p%d_rq" % gi, [128, 1024], F32, 1)
                        rsw_p = TPool(es3, nc, "p%d_rsw" % gi, [128, 1024], F32, 1)
                        prc_p = TPool(es3, nc, "p%d_rc" % gi, [128, 128], F32, 1)
                        prs_p = TPool(es3, nc, "p%d_rs" % gi, [128, 128], F32, 1)
                        yield
                        for n in tiles:
                            rows = slice(n * 128, (n + 1) * 128)
                            seg0, seg1 = (0, NCT * 128) if n < NCT else (NCT * 128, T)
                            acc, b_acc = acc_p2.next()
                            for j in range(5):
                                sh, b_sh = sh_p.next()
                                lo = n * 128 + j - 2
                                hi = lo + 128
                                clo, chi = max(lo, seg0), min(hi, seg1)
                                if clo > lo or chi < hi:
                                    mset("pool", sh[:, :], 0.0, [b_sh])
                                dma(sh[clo - lo:chi - lo, :], U[clo:chi, OFF["d_q"]:OFF["d_q"] + 1536],
                                    [DB("U", m) for m in range(max(0, n - 1), min(NT, n + 2))], [b_sh])
                                if j == 0:
                                    tt("pool", acc[:, :], sh[:, :], cw[0][0][:, :], ALU.mult, [b_sh, cw[0][1]], [b_acc])
                                else:
                                    tmp, b_tmp = tmp_p.next()
                                    tt("pool" if j == 1 else "dve", tmp[:, :], sh[:, :], cw[j][0][:, :], ALU.mult, [b_sh, cw[j][1]], [b_tmp])
                                    yield
                                    tt("dve" if j % 2 else "pool", acc[:, :], acc[:, :], tmp[:, :], ALU.add, [b_acc, b_tmp], [b_acc])
                                yield
                            for _w in range(8):
                                yield
                            act(acc[:, :], acc[:, :], AF.Silu, [b_acc], [b_acc])
                            yield
                            tmp, b_tmp = tmp_p.next()
                            st, b_st = pst_p.next()
                            tt("dve", tmp[:, 0:1024], acc[:, 0:1024], acc[:, 0:1024], ALU.mult, [b_acc], [b_tmp])
                            yield
                            red(st[:, 0:8], tmp[:, 0:1024].rearrange("p (h d) -> p h d", h=8), [b_tmp], [b_st])
                            yield
                            ts("dve", st[:, 0:8], st[:, 0:8], 1.0, EPS, ALU.mult, ALU.add, [b_st], [b_st])
                            for _w in range(8):
                                yield
                            act(st[:, 0:8], st[:, 0:8], AF.Sqrt, [b_st], [b_st])
                            yield
                            recip(st[:, 0:8], st[:, 0:8], [b_st], [b_st])
                            yield
                            ts("dve", st[:, 0:4], st[:, 0:4], 128.0 ** -0.5, None, ALU.mult, None, [b_st], [b_st])
                            yield
                            qk3 = acc[:, 0:1024].rearrange("p (h d) -> p h d", h=8)
                            tt("dve", qk3, qk3, st[:, 0:8].unsqueeze(2).to_broadcast([128, 8, 128]), ALU.mult, [b_acc, b_st], [b_acc])
                            yield
                            ob, b_ob = pob_p.next()
                            for _w in range(8):
                                yield
                            cp("act", ob[:, :], acc[:, :], [b_acc], [b_ob])
                            dma(DQ[rows, :], ob[:, :], [b_ob], [DB("DQ", n)], q="act")
                            yield
                            rr, b_rr = rraw_p.next()
                            rq, b_rq = rq_p.next()
                            dma(rr[:, :], U[rows, OFF["r_q"]:OFF["r_q"] + 1024], [DB("U", n)], [b_rr])
                            cp("pool", rq[:, :], rr[:, :], [b_rr], [b_rq])
                            yield
                            if n >= NCT:
                                rc, b_rc = prc_p.next()
                                rs, b_rs = prs_p.next()
                                sw, b_sw = rsw_p.next()
                                r0 = (n - NCT) * 128
                                dma(rc[:], ropec[r0:r0 + 128, :], [], [b_rc])
                                dma(rs[:], ropes[r0:r0 + 128, :], [], [b_rs])
                                q4 = rq[:, :].rearrange("p (g two f) -> p g two f", two=2, f=32)
                                s4 = sw[:, :].rearrange("p (g two f) -> p g two f", two=2, f=32)
                                cp("pool", s4[:, :, 0, :], q4[:, :, 1, :], [b_rq], [b_sw])
                                cp("pool", s4[:, :, 1, :], q4[:, :, 0, :], [b_rq], [b_sw])
                                q3 = rq[:, :].rearrange("p (h d) -> p h d", h=8)
                                s3 = sw[:, :].rearrange("p (h d) -> p h d", h=8)
                                tt("dve", q3, q3, rc[:, :].unsqueeze(1).to_broadcast([128, 8, 128]), ALU.mult, [b_rq, b_rc], [b_rq])
                                tt("pool", s3, s3, rs[:, :].unsqueeze(1).to_broadcast([128, 8, 128]), ALU.mult, [b_sw, b_rs], [b_sw])
                                yield
                                tt("dve", q3, q3, s3, ALU.add, [b_rq, b_sw], [b_rq])
                                yield
                            ts("dve", rq[:, 512:1024], rq[:, 512:1024], 128.0 ** -0.5, None, ALU.mult, None, [b_rq], [b_rq])
                            yield
                            rr2, b_rr2 = rraw_p.next()
                            for _w in range(8):
                                yield
                            cp("act", rr2[:, :], rq[:, :], [b_rq], [b_rr2])
                            dma(U[rows, OFF["r_q"]:OFF["r_q"] + 1024], rr2[:, :], [b_rr2], [DB("Ur", n)], q="act")
                            yield

                    gens = [gen_attn(), gen_prep(list(range(0, NT, 2)), 0), gen_prep(list(range(1, NT, 2)), 1)]
                    while gens:
                        alive = []
                        for g_ in gens:
                            try:
                                next(g_)
                                alive.append(g_)
                            except StopIteration:
                                pass
                        gens = alive
                    S.barrier()

            for mixer in ("m", "r", "d"):
                with contextlib.ExitStack() as es:
                    VW = 129 if mixer == "m" else 128
                    OUT = dict(m=OM, r=OR, d=OD)[mixer]
                    ugt, b_ugt = sb("g_ug", [128, NT, 32], es=es)
                    for n in range(NT):
                        dma(ugt[:, n, :], UG[n * 128:(n + 1) * 128, :], [DB("UG", n)], [b_ugt])
                    prm, b_prm = sb("g_prm", [128, 32], es=es)
                    G = {}
                    NG = NT * 4
                    for d in range(2):
                        for nm in ("lf", "li", "g", "gend", "eg", "kend", "rowp", "beta", "beg"):
                            G[(nm, d)] = sb("g_%s%d" % (nm, d), [128, NT, 4], es=es)
                        G[("gam", d)] = sb("g_gam%d" % d, [128, NT, 8], es=es)
                    gtmp, b_gtmp = sb("g_tmp", [128, NT, 8], es=es)
                    psA = TPool(es, nc, "r_psA", [128, 512], F32, 4, psum=True)
                    psB = TPool(es, nc, "r_psB", [128, 512], F32, 4, psum=True)
                    gps = psA
                    if mixer == "m":
                        dma(prm[:, 0:8], mib[l:l + 1, :].partition_broadcast(128), [], [b_prm])
                        dma(prm[:, 8:16], mfb[l:l + 1, :].partition_broadcast(128), [], [b_prm])
                    elif mixer == "r":
                        dma(prm[:, 0:8], rlg[l:l + 1, :].partition_broadcast(128), [], [b_prm])
                    else:
                        dma(prm[:, 0:8], alog[l:l + 1, :].partition_broadcast(128), [], [b_prm])
                        dma(prm[:, 8:16], dtb[l:l + 1, :].partition_broadcast(128), [], [b_prm])
                        act(prm[:, 0:8], prm[:, 0:8], AF.Exp, [b_prm], [b_prm])
                    for d in range(2):
                        lf, b_lf = G[("lf", d)]
                        li, b_li = G[("li", d)]
                        g, b_g = G[("g", d)]
                        gend, b_gend = G[("gend", d)]
                        eg, b_eg = G[("eg", d)]
                        kend, b_kend = G[("kend", d)]
                        rowp, b_rowp = G[("rowp", d)]
                        beta, b_beta = G[("beta", d)]
                        beg, b_beg = G[("beg", d)]
                        gam, b_gam = G[("gam", d)]

                        def prmb(c0):
                            return prm[:, c0 + d * 4:c0 + d * 4 + 4].unsqueeze(1).to_broadcast([128, NT, 4])
                        if mixer == "m":
                            tt("dve", li[:], ugt[:, :, d * 4:d * 4 + 4], prmb(0), ALU.add, [b_ugt, b_prm], [b_li])
                            tt("dve", lf[:], ugt[:, :, 8 + d * 4:12 + d * 4], prmb(8), ALU.add, [b_ugt, b_prm], [b_lf])
                            act(lf[:], lf[:], AF.Exp, [b_lf], [b_lf], scale=-1.0)
                            act(lf[:], lf[:], AF.Ln, [b_lf], [b_lf], bias=1.0)
                            ts("dve", lf[:], lf[:], -1.0, None, ALU.mult, None, [b_lf], [b_lf])
                        elif mixer == "r":
                            mset("dve", li[:], 0.0, [b_li])
                            mset("dve", lf[:], 0.0, [b_lf])
                            tt("dve", lf[:], lf[:], prmb(0), ALU.add, [b_lf, b_prm], [b_lf])
                        else:
                            mset("dve", li[:], 0.0, [b_li])
                            tt("dve", lf[:], ugt[:, :, 16 + d * 4:20 + d * 4], prmb(8), ALU.add, [b_ugt, b_prm], [b_lf])
                            act(lf[:], lf[:], AF.Exp, [b_lf], [b_lf])
                            act(lf[:], lf[:], AF.Ln, [b_lf], [b_lf], bias=1.0)
                            tt("dve", lf[:], lf[:], prmb(0), ALU.mult, [b_lf, b_prm], [b_lf])
                            ts("dve", lf[:], lf[:], -1.0, None, ALU.mult, None, [b_lf], [b_lf])
                            act(beta[:], ugt[:, :, 24 + d * 4:28 + d * 4], AF.Exp, [b_ugt], [b_beta], scale=-1.0)
                            ts("dve", beta[:], beta[:], 1.0, None, ALU.add, None, [b_beta], [b_beta])
                            recip(beta[:], beta[:], [b_beta], [b_beta])
                        lf2 = lf[:].rearrange("p n h -> p (n h)")
                        ps, b_ps = gps.next()
                        mm(ps[:, 0:NG], tri[d][0][:, 0:128], lf2, True, True, [tri[d][1], b_lf], [b_ps])
                        cp("dve", g[:].rearrange("p n h -> p (n h)"), ps[:, 0:NG], [b_ps], [b_g])
                        ps, b_ps = gps.next()
                        mm(ps[:, 0:NG], blk[:, :], lf2, True, True, [b_blk, b_lf], [b_ps])
                        cp("dve", gend[:].rearrange("p n h -> p (n h)"), ps[:, 0:NG], [b_ps], [b_gend])
                        act(eg[:], g[:], AF.Exp, [b_g], [b_eg])
                        tt("dve", rowp[:], li[:], g[:], ALU.subtract, [b_li, b_g], [b_rowp])
                        tt("dve", kend[:], gend[:], rowp[:], ALU.add, [b_gend, b_rowp], [b_kend])
                        act(kend[:], kend[:], AF.Exp, [b_kend], [b_kend])
                        if mixer == "d":
                            tt("dve", beg[:], beta[:], eg[:], ALU.mult, [b_beta, b_eg], [b_beg])
                        g4 = gtmp[:].rearrange("p n (c h) -> p n c h", c=2)
                        for c in range(2):
                            tt("dve", g4[:, :, c, :], lf[:], chm[:, c * 4:c * 4 + 4].unsqueeze(1).to_broadcast([128, NT, 4]), ALU.mult, [b_lf, b_chm], [b_gtmp])
                        ps, b_ps = gps.next()
                        mm(ps[:, 0:NT * 8], ones[:, :], gtmp[:].rearrange("p n c -> p (n c)"), True, True, [b_ones, b_gtmp], [b_ps])
                        act(gam[:].rearrange("p n c -> p (n c)"), ps[:, 0:NT * 8], AF.Exp, [b_ps], [b_gam])

                    KSLOT = 3 if mixer == "d" else 4
                    st_p = TPool(es, nc, "r_st", [128, 16], F32, 12)
                    slots = []
                    for si in range(KSLOT):
                        Bd = {}
                        Bd["raw"] = sb("s%d_raw" % si, [128, 1536], BF16, es=es)
                        Bd["qkv"] = sb("s%d_qkv" % si, [128, 1536], F32, es=es)
                        Bd["qs"] = sb("s%d_qs" % si, [128, 512], F32, es=es)
                        Bd["ke"] = sb("s%d_ke" % si, [128, 512], BF16, es=es)
                        Bd["v1"] = sb("s%d_v1" % si, [128, 4, 132], BF16, es=es)
                        for nm in ("QT", "QtT", "KT", "QKT"):
                            Bd[nm] = sb("s%d_%s" % (si, nm), [128, 512], BF16, es=es)
                        Bd["ep"] = sb("s%d_ep" % si, [128, 512], F32, es=es)
                        Bd["DT"] = sb("s%d_DT" % si, [128, 512], F32, es=es)
                        Bd["ob"] = sb("s%d_ob" % si, [128, 512], F32, es=es)
                        if mixer == "d":
                            Bd["kb"] = sb("s%d_kb" % si, [128, 512], F32, es=es)
                            Bd["wk"] = sb("s%d_wk" % si, [128, 512], F32, es=es)
                            Bd["KbT"] = sb("s%d_KbT" % si, [128, 512], BF16, es=es)
                            Bd["WkT"] = sb("s%d_WkT" % si, [128, 512], BF16, es=es)
                            Bd["AT"] = [sb("s%d_AT%d" % (si, j), [128, 512], F32, es=es) for j in range(3)]
                            Bd["A"] = [sb("s%d_A%d" % (si, j), [128, 512], F32, es=es) for j in range(3)]
                            Bd["R"] = [sb("s%d_R%d" % (si, j), [128, 4, 256], F32, es=es) for j in range(3)]
                            Bd["U"] = [sb("s%d_U%d" % (si, j), [128, 512], BF16, es=es) for j in range(2)]
                        slots.append(Bd)
                    state = [sb("r_S%d" % d, [128, 4, 132], es=es) for d in range(2)]
                    stateb = [sb("r_Sb%d" % d, [128, 4, 132], BF16, es=es) for d in range(2)]
                    for d in range(2):
                        mset("dve", state[d][0][:], 0.0, [state[d][1]])
                        mset("dve", stateb[d][0][:], 0.0, [stateb[d][1]])

                    DTc = None
                    if mixer == "r":
                        DTc = [sb("r_DTc%d" % d_, [128, 512], es=es) for d_ in range(2)]
                        epc, b_epc = sb("r_epc", [128, 512], es=es)
                        for d_ in range(2):
                            lf_, b_lf_ = G[("lf", d_)]
                            rowp_, b_rowp_ = G[("rowp", d_)]
                            tt("dve", epc[:, :].rearrange("p (h t) -> p h t", h=4), tri[d_][0][:, :].rearrange("p (h t) -> p h t", h=4),
                               lf_[:, 0, :].unsqueeze(2).to_broadcast([128, 4, 128]), ALU.mult, [tri[d_][1], b_lf_], [b_epc])
                            psE_, b_psE_ = psB.next()
                            mm(psE_[:, :], ones[:, :], epc[:, :], True, False, [b_ones, b_epc], [b_psE_])
                            mm(psE_[:, :], ident[:, :], mbig[d_][0][:, :], False, True, [b_ident, mbig[d_][1]], [b_psE_])
                            for h_ in range(4):
                                act(DTc[d_][0][:, h_ * 128:(h_ + 1) * 128], psE_[:, h_ * 128:(h_ + 1) * 128], AF.Exp, [b_psE_, b_rowp_], [DTc[d_][1]],
                                    bias=rowp_[:, 0, h_:h_ + 1])

                    def transpose4(src, b_src, dst, b_dst):
                        ps, b_ps = psA.next()
                        for h in range(4):
                            S.op("pe", (lambda ps=ps, h=h, src=src: lambda e: e.transpose(out=ps[:, h * 128:(h + 1) * 128], in_=src[:, h * 128:(h + 1) * 128], identity=ident[:]))(),
                                 [b_src, b_ident], [b_ps])
                        cp("act", dst[:, :], ps[:, :], [b_ps], [b_dst])

                    def unit(n, d, Bd, kseq):
                        eg, b_eg = G[("eg", d)]
                        kend, b_kend = G[("kend", d)]
                        rowp, b_rowp = G[("rowp", d)]
                        lf, b_lf = G[("lf", d)]
                        gam, b_gam = G[("gam", d)]
                        beta, b_beta = G[("beta", d)]
                        beg, b_beg = G[("beg", d)]
                        raw, b_raw = Bd["raw"]
                        qkv, b_qkv = Bd["qkv"]
                        rows = slice(n * 128, (n + 1) * 128)
                        if mixer == "d":
                            dma(raw[:, :], DQ[rows, :], [DB("DQ", n)], [b_raw])
                        else:
                            c0 = OFF["m_q"] if mixer == "m" else OFF["r_q"]
                            dma(raw[:, :], U[rows, c0:c0 + 1536], [DB("U", n), DB("Ur", n)], [b_raw])
                        cp("dve", qkv[:, :], raw[:, :], [b_raw], [b_qkv])
                        if mixer == "m":
                            ts("dve", qkv[:, 0:512], qkv[:, 0:512], 128.0 ** -0.5, None, ALU.mult, None, [b_qkv], [b_qkv])
                        yield
                        qv = qkv[:, 0:512]
                        kv = qkv[:, 512:1024]
                        vv = qkv[:, 1024:1536]
                        h4 = lambda ap: ap.rearrange("p (h d) -> p h d", h=4)
                        bc = lambda t_: t_[:, n, :].unsqueeze(2).to_broadcast([128, 4, 128])
                        qs, b_qs = Bd["qs"]
                        ke, b_ke = Bd["ke"]
                        tt("dve", h4(qs[:, :]), h4(qv), bc(eg), ALU.mult, [b_qkv, b_eg], [b_qs])
                        tt("pool", h4(ke[:, :]), h4(kv), bc(kend), ALU.mult, [b_qkv, b_kend], [b_ke])
                        if mixer == "d":
                            kb, b_kb = Bd["kb"]
                            tt("dve", h4(kb[:, :]), h4(kv), bc(beta), ALU.mult, [b_qkv, b_beta], [b_kb])
                            R, b_R = Bd["R"][0]
                            tt("dve", R[:, :, 0:128], h4(vv), bc(beta), ALU.mult, [b_qkv, b_beta], [b_R])
                            tt("pool", R[:, :, 128:256], h4(kv), bc(beg), ALU.mult, [b_qkv, b_beg], [b_R])
                        else:
                            v1, b_v1 = Bd["v1"]
                            cp("pool", v1[:, :, 0:128], h4(vv), [b_qkv], [b_v1])
                            if mixer == "m":
                                mset("pool", v1[:, :, 128:129], 1.0, [b_v1])
                        yield
                        QTt, b_QTt = Bd["QT"]
                        QtT, b_QtT = Bd["QtT"]
                        KTt, b_KTt = Bd["KT"]
                        transpose4(qv, b_qkv, QTt, b_QTt)
                        yield
                        transpose4(kv, b_qkv, KTt, b_KTt)
                        yield
                        transpose4(qs, b_qs, QtT, b_QtT)
                        yield
                        if mixer == "d":
                            KbT, b_KbT = Bd["KbT"]
                            transpose4(kb, b_kb, KbT, b_KbT)
                            yield
                        if mixer == "r":
                            DT, b_DT = DTc[d]
                        else:
                            ep, b_ep = Bd["ep"]
                            tt("dve", h4(ep[:, :]), h4(tri[d][0][:, :]), bc(lf), ALU.mult, [tri[d][1], b_lf], [b_ep])
                            psE, b_psE = psB.next()
                            mm(psE[:, :], ones[:, :], ep[:, :], True, False, [b_ones, b_ep], [b_psE])
                            mm(psE[:, :], ident[:, :], mbig[d][0][:, :], False, True, [b_ident, mbig[d][1]], [b_psE])
                            DT, b_DT = Bd["DT"]
                            for h in range(4):
                                act(DT[:, h * 128:(h + 1) * 128], psE[:, h * 128:(h + 1) * 128], AF.Exp, [b_psE, b_rowp], [b_DT], bias=rowp[:, n, h:h + 1])
                            yield
                        psQ, b_psQ = psB.next()
                        for h in range(4):
                            mm(psQ[:, h * 128:(h + 1) * 128], KTt[:, h * 128:(h + 1) * 128], QTt[:, h * 128:(h + 1) * 128], True, True, [b_KTt, b_QTt], [b_psQ])
                        QKT, b_QKT = Bd["QKT"]
                        tt("dve", QKT[:, :], psQ[:, :], DT[:, :], ALU.mult, [b_psQ, b_DT], [b_QKT])
                        yield
                        ob, b_ob = Bd["ob"]
                        St, b_St = state[d]
                        Sb, b_Sb = stateb[d]
                        if mixer == "d":
                            psK, b_psK = psB.next()
                            for h in range(4):
                                mm(psK[:, h * 128:(h + 1) * 128], KTt[:, h * 128:(h + 1) * 128], KbT[:, h * 128:(h + 1) * 128], True, True, [b_KTt, b_KbT], [b_psK])
                            ai = 0
                            AT, b_AT = Bd["AT"][0]
                            A, b_A = Bd["A"][0]
                            tt("dve", AT[:, :], psK[:, :], DT[:, :], ALU.mult, [b_psK, b_DT], [b_AT])
                            tt("dve", AT[:, :], AT[:, :], offd[:, :], ALU.mult, [b_AT, b_offd], [b_AT])
                            yield
                            psT, b_psT = psA.next()
                            for h in range(4):
                                S.op("pe", (lambda psT=psT, h=h, AT=AT: lambda e: e.transpose(out=psT[:, h * 128:(h + 1) * 128], in_=AT[:, h * 128:(h + 1) * 128], identity=ident[:]))(),
                                     [b_AT, b_ident], [b_psT])
                            cp("act", A[:, :], psT[:, :], [b_psT], [b_A])
                            yield
                            ri = 0
                            for lev in range(6):
                                p1, b_p1 = psB.next()
                                p2, b_p2 = psB.next()
                                for h in range(4):
                                    pp, b_pp = (p1, b_p1) if h < 2 else (p2, b_p2)
                                    mm(pp[:, (h % 2) * 256:(h % 2) * 256 + 256], AT[:, h * 128:(h + 1) * 128], R[:, h, :], True, True, [b_AT, b_R], [b_pp])
                                ri = (ri + 1) % 3
                                Rn, b_Rn = Bd["R"][ri]
                                op = ALU.subtract if lev == 0 else ALU.add
                                tt("dve", Rn[:, 0:2, :], R[:, 0:2, :], p1[:, :].rearrange("p (h e) -> p h e", h=2), op, [b_R, b_p1], [b_Rn])
                                tt("dve", Rn[:, 2:4, :], R[:, 2:4, :], p2[:, :].rearrange("p (h e) -> p h e", h=2), op, [b_R, b_p2], [b_Rn])
                                R, b_R = Rn, b_Rn
                                yield
                                if lev < 5:
                                    pA, b_pA = psA.next()
                                    pT, b_pT = psA.next()
                                    for h in range(4):
                                        hs = slice(h * 128, (h + 1) * 128)
                                        mm(pA[:, hs], AT[:, hs], A[:, hs], True, True, [b_AT, b_A], [b_pA])
                                        mm(pT[:, hs], A[:, hs], AT[:, hs], True, True, [b_AT, b_A], [b_pT])
                                    ai = (ai + 1) % 3
                                    A2, b_A2 = Bd["A"][ai]
                                    AT2, b_AT2 = Bd["AT"][ai]
                                    cp("act", A2[:, :], pA[:, :], [b_pA], [b_A2])
                                    cp("dve", AT2[:, :], pT[:, :], [b_pT], [b_AT2])
                                    A, b_A, AT, b_AT = A2, b_A2, AT2, b_AT2
                                    yield
                            wk, b_wk = Bd["wk"]
                            cp("dve", h4(wk[:, :]), R[:, :, 128:256], [b_R], [b_wk])
                            WkT, b_WkT = Bd["WkT"]
                            transpose4(wk, b_wk, WkT, b_WkT)
                            yield
                        while done_seq[d] < kseq:
                            yield
                        for ci, c in enumerate((0, 1) if d == 0 else (1, 0)):
                            P = slice(c * 64, c * 64 + 64)
                            if mixer == "d":
                                psU, b_psU = psB.next()
                                for h in range(4):
                                    mm(psU[:, h * 128:(h + 1) * 128], WkT[:, h * 128:(h + 1) * 128], Sb[:, h, 0:128], True, True, [b_WkT, b_Sb], [b_psU])
                                Ut, b_Ut = Bd["U"][ci]
                                tt("dve", Ut[P, :].rearrange("p (h e) -> p h e", h=4), R[P, :, 0:128], psU[P, :].rearrange("p (h e) -> p h e", h=4),
                                   ALU.subtract, [b_R, b_psU], [b_Ut])
                                rhsU = (lambda Ut=Ut, P=P: lambda h: Ut[P, h * 128:(h + 1) * 128])()
                                b_rhs = b_Ut
                                yield
                            else:
                                rhsU = (lambda v1=v1, P=P: lambda h: v1[P, h, 0:VW])()
                                b_rhs = b_v1
                            po1, b_po1 = psB.next()
                            po2, b_po2 = psB.next()
                            ps1, b_ps1 = psA.next()
                            ps2, b_ps2 = psA.next()
                            for h in range(4):
                                po, b_po = (po1, b_po1) if h < 2 else (po2, b_po2)
                                o_ap = po[:, (h % 2) * 256:(h % 2) * 256 + VW]
                                mm(o_ap, QtT[:, h * 128:(h + 1) * 128], Sb[:, h, 0:VW], True, False, [b_QtT, b_Sb], [b_po])
                                mm(o_ap, QKT[P, h * 128:(h + 1) * 128], rhsU(h), False, True, [b_QKT, b_rhs], [b_po])
                            for h in range(4):
                                pss, b_pss = (ps1, b_ps1) if h < 2 else (ps2, b_ps2)
                                mm(pss[:, (h % 2) * 256:(h % 2) * 256 + VW], ke[P, h * 128:(h + 1) * 128], rhsU(h), True, True, [b_ke, b_rhs], [b_pss])
                            for h in range(4):
                                pss, b_pss = (ps1, b_ps1) if h < 2 else (ps2, b_ps2)
                                stt(St[:, h, 0:VW], St[:, h, 0:VW], gam[:, n, c * 4 + h:c * 4 + h + 1], pss[:, (h % 2) * 256:(h % 2) * 256 + VW],
                                    ALU.mult, ALU.add, [b_St, b_gam, b_pss], [b_St])
                            cp("act", Sb[:, :, 0:VW], St[:, :, 0:VW], [b_St], [b_Sb])
                            for hp in range(2):
                                po, b_po = (po1, b_po1) if hp == 0 else (po2, b_po2)
                                po3 = po[:, :].rearrange("p (h e) -> p h e", h=2)
                                o3 = ob[:, hp * 256:(hp + 1) * 256].rearrange("p (h e) -> p h e", h=2)
                                if mixer == "m":
                                    st2, b_st2 = st_p.next()
                                    act(st2[P, 0:2], po3[P, :, 128], AF.Abs, [b_po], [b_st2])
                                    ts("dve", st2[P, 0:2], st2[P, 0:2], 1.0, None, ALU.max, None, [b_st2], [b_st2])
                                    recip(st2[P, 0:2], st2[P, 0:2], [b_st2], [b_st2])
                                    tt("dve", o3[P], po3[P, :, 0:128], st2[P, 0:2].unsqueeze(2).to_broadcast([64, 2, 128]), ALU.mult, [b_po, b_st2], [b_ob])
                                else:
                                    cp("act", o3[P], po3[P, :, 0:128], [b_po], [b_ob])
                            yield
                        done_seq[d] += 1
                        dma(OUT[d][rows, :], ob[:, :], [b_ob], [DB("O%s%d" % (mixer, d), n)], q="act")

                    done_seq = [0, 0]
                    order = []
                    for i in range(NT):
                        order.append((fwd_order[i], 0, i))
                        order.append((bwd_order[i], 1, i))
                    active = []
                    free = list(range(KSLOT))
                    nxt = 0
                    while nxt < len(order) or active:
                        while free and nxt < len(order):
                            si = free.pop(0)
                            n_, d_, k_ = order[nxt]
                            nxt += 1
                            active.append((si, unit(n_, d_, slots[si], k_)))
                        still = []
                        for si, g_ in active:
                            try:
                                next(g_)
                                still.append((si, g_))
                            except StopIteration:
                                free.append(si)
                        active = still
                    S.barrier()

            with contextlib.ExitStack() as es:
                wo, b_wo = sb("c_wo", [128, KC, D], BF16, es=es)
                hw, b_hw = sb("c_hw", [128, 1536], es=es)
                gp = [sb("c_gp%d" % w, [128, D], es=es) for w in range(2)]
                wov = w_out[l].rearrange("(k p) n -> p k n", p=128)
                with contextlib.ExitStack() as es2:
                    wst_p = TPool(es2, nc, "c_wst", [128, 4, 512], F32, 2)
                    for k4 in range(0, KC, 4):
                        for cbk in range(4):
                            wst, b_wst = wst_p.next()
                            dma(wst[:, :, :], wov[:, k4:k4 + 4, cbk * 512:(cbk + 1) * 512], [], [b_wst])
                            cp("pool" if cbk % 2 else "dve", wo[:, k4:k4 + 4, cbk * 512:(cbk + 1) * 512], wst[:, :, :], [b_wst], [b_wo])
                    dma(hw[:], hnw[l:l + 1, :].partition_broadcast(128), [], [b_hw])
                    for w in range(2):
                        dma(gp[w][0][:], GPW[l * 2 + w:l * 2 + w + 1, :].partition_broadcast(128), [DB("GPW", l)], [gp[w][1]])
                    S.barrier()
                sq_p = TPool(es, nc, "c_sq", [128, 512], F32, 3)
                st_p = TPool(es, nc, "c_st", [128, 8], F32, 6)
                ps_p = TPool(es, nc, "c_ps", [128, 512], F32, 4, psum=True)
                po_p = TPool(es, nc, "c_po", [128, 512], F32, 4, psum=True)
                cslots = []
                for si in range(2):
                    Bc = {}
                    Bc["z"] = sb("c%d_z" % si, [128, 2560], BF16, es=es)
                    Bc["zf"] = sb("c%d_zf" % si, [128, 2560], F32, es=es)
                    Bc["o"] = [sb("c%d_o%d" % (si, j), [128, 512], F32, es=es) for j in range(7)]
                    Bc["y"] = sb("c%d_y" % si, [128, D], F32, es=es)
                    Bc["yT"] = sb("c%d_yT" % si, [128, KC, 128], BF16, es=es)
                    Bc["x"] = sb("c%d_x" % si, [128, D], F32, es=es)
                    cslots.append(Bc)

                def ctile(n, Bc):
                    which = 1 if n < NCT else 0
                    rows = slice(n * 128, (n + 1) * 128)
                    z, b_z = Bc["z"]
                    zf, b_zf = Bc["zf"]
                    y, b_y = Bc["y"]
                    yT, b_yT = Bc["yT"]
                    xt, b_xt = Bc["x"]
                    dma(z[:, 0:1024], U[rows, OFF["m_o"]:OFF["m_o"] + 1024], [DB("U", n)], [b_z])
                    dma(z[:, 1024:1536], U[rows, OFF["r_z"]:OFF["r_z"] + 512], [DB("U", n)], [b_z])
                    dma(z[:, 1536:2048], U[rows, OFF["a_z"]:OFF["a_z"] + 512], [DB("U", n)], [b_z])
                    dma(z[:, 2048:2560], U[rows, OFF["d_z"]:OFF["d_z"] + 512], [DB("U", n)], [b_z])
                    obufs = {}
                    oi = 0
                    for mx, OUTS in (("m", OM), ("r", OR), ("d", OD)):
                        for d_ in range(2):
                            o_, b_o = Bc["o"][oi]
                            oi += 1
                            dma(o_[:, :], OUTS[d_][rows, :], [DB("O%s%d" % (mx, d_), n)], [b_o])
                            obufs[(mx, d_)] = (o_, b_o)
                    o_, b_o = Bc["o"][6]
                    dma(o_[:, :], OA[rows, :], [DB("OA", n)], [b_o])
                    obufs[("a", 0)] = (o_, b_o)
                    src, sr = xsrc(n)
                    dma(xt[:], src, sr, [b_xt])
                    yield
                    act(zf[:, 0:512], z[:, 0:512], AF.Sigmoid, [b_z], [b_zf])
                    act(zf[:, 512:2560], z[:, 512:2560], AF.Silu, [b_z], [b_zf])
                    yield
                    for gi, mx in enumerate(("m", "r", "a", "d")):
                        ycol = y[:, gi * 512:(gi + 1) * 512]
                        if mx == "a":
                            o0, b_o0 = obufs[("a", 0)]
                            tt("dve", ycol, o0[:, :], zf[:, 1536:2048], ALU.mult, [b_o0, b_zf], [b_y])
                            continue
                        o0, b_o0 = obufs[(mx, 0)]
                        o1, b_o1 = obufs[(mx, 1)]
                        st, b_st = st_p.next()
                        tt("pool", o0[:, :], o0[:, :], o1[:, :], ALU.add, [b_o0, b_o1], [b_o0])
                        if mx == "m":
                            tt("pool", o0[:, :], o0[:, :], zf[:, 0:512], ALU.mult, [b_o0, b_zf], [b_o0])
                        sq, b_sq = sq_p.next()
                        tt("dve", sq[:, :], o0[:, :], o0[:, :], ALU.mult, [b_o0], [b_sq])
                        red(st[:, 0:4], sq[:, :].rearrange("p (h d) -> p h d", h=4), [b_sq], [b_st])
                        yield
                        rsqrt_inplace(st[:, 0:4], 1.0 / 128, EPS, [b_st])
                        yield
                        hoff = dict(m=0, r=512, d=1024)[mx]
                        zoff = dict(m=512, r=1024, d=2048)[mx]
                        tt("dve", o0[:, :].rearrange("p (h d) -> p h d", h=4), o0[:, :].rearrange("p (h d) -> p h d", h=4),
                           st[:, 0:4].unsqueeze(2).to_broadcast([128, 4, 128]), ALU.mult, [b_o0, b_st], [b_o0])
                        tt("pool", o0[:, :], o0[:, :], hw[:, hoff:hoff + 512], ALU.mult, [b_o0, b_hw], [b_o0])
                        tt("dve", ycol, o0[:, :], zf[:, zoff:zoff + 512], ALU.mult, [b_o0, b_zf], [b_y])
                        yield
                    for k4 in range(4):
                        ps, b_ps = ps_p.next()
                        for j in range(4):
                            k = k4 * 4 + j
                            S.op("pe", (lambda ps=ps, j=j, k=k, y=y: lambda e: e.transpose(out=ps[:, j * 128:(j + 1) * 128], in_=y[:, k * 128:(k + 1) * 128], identity=ident[:]))(),
                                 [b_y, b_ident], [b_ps])
                        cp("act", yT[:, k4 * 4:k4 * 4 + 4, :], ps[:, :].rearrange("p (k t) -> p k t", k=4), [b_ps], [b_yT])
                    yield
                    pos = [po_p.next() for _ in range(4)]
                    for cbk in range(4):
                        po, b_po = pos[cbk]
                        for k in range(KC):
                            mm(po[:, :], yT[:, k, :], wo[:, k, cbk * 512:(cbk + 1) * 512], k == 0, k == KC - 1, [b_yT, b_wo], [b_po])
                    st2, b_st2 = st_p.next()
                    for cbk in range(4):
                        po, b_po = pos[cbk]
                        sq, b_sq = sq_p.next()
                        act(sq[:, :], po[:, :], AF.Square, [b_po], [b_sq, b_st2], accum=st2[:, cbk:cbk + 1])
                    red(st2[:, 4:5], st2[:, 0:4], [b_st2], [b_st2])
                    rsqrt_inplace(st2[:, 4:5], 1.0 / D, EPS, [b_st2])
                    for cbk in range(4):
                        po, b_po = pos[cbk]
                        cs = slice(cbk * 512, (cbk + 1) * 512)
                        sq, b_sq = sq_p.next()
                        stt(sq[:, :], po[:, :], st2[:, 4:5], gp[which][0][:, cs], ALU.mult, ALU.mult, [b_po, b_st2, gp[which][1]], [b_sq])
                        tt("pool", xt[:, cs], xt[:, cs], sq[:, :], ALU.add, [b_xt, b_sq], [b_xt])
                    if last:
                        dma(y_out[(n - NCT) * 128:(n - NCT + 1) * 128, :], xt[:], [b_xt], [DB("Y", n)], q="pool")
                    else:
                        dma(X1[rows, :], xt[:], [b_xt], [DB("X1", n)], q="pool")

                tiles = list(range(NCT, NT)) if last else list(range(NT))
                pend = list(tiles)
                cact = []
                cfree = [0, 1]
                tick = 0
                while pend or cact:
                    if pend and cfree and (not cact or tick >= 5):
                        si = cfree.pop(0)
                        cact.append((si, ctile(pend.pop(0), cslots[si])))
                    still = []
                    for si, g_ in cact:
                        try:
                            next(g_)
                            still.append((si, g_))
                        except StopIteration:
                            cfree.append(si)
                    cact = still
                    tick += 1
                S.barrier()

            if debug and l == 0:
                with contextlib.ExitStack() as es:
                    bt, b_bt = sb("dbg_t", [128, 2048], es=es)
                    btb, b_btb = sb("dbg_tb", [128, 2048], BF16, es=es)
                    for n in range(NT):
                        rows = slice(n * 128, (n + 1) * 128)
                        for c in range(4):
                            dma(btb[:, :], U[rows, c * 2048:(c + 1) * 2048], [], [b_btb], q="sp")
                            dma(dbg["U"][rows, c * 2048:(c + 1) * 2048], btb[:, :], [b_btb], [DB("dbgU", n)], q="sp")
                        for nm, src in (("UG", UG), ("OM0", OM[0]), ("OM1", OM[1]), ("OR0", OR[0]), ("OR1", OR[1]), ("OD0", OD[0]), ("OD1", OD[1]), ("OA", OA), ("X1", X1)):
                            w_ = src.shape[1]
                            dma(bt[:, 0:w_], src[rows, :], [], [b_bt], q="sp")
                            dma(dbg[nm][rows, :], bt[:, 0:w_], [b_bt], [DB("dbg" + nm, n)], q="sp")
                    S.barrier()

        S.barrier()
        S.emit()
    return nc


def _consts(NLT):
    idx = np.arange(128)
    same = (idx[:, None] // 64) == (idx[None, :] // 64)
    tri0 = (same & (idx[:, None] <= idx[None, :])).astype(np.float32)
    tri1 = (same & (idx[:, None] >= idx[None, :])).astype(np.float32)
    tri = np.stack([np.tile(tri0, (1, 4)), np.tile(tri1, (1, 4))]).astype(np.float32)
    mbig = ((tri - 1.0) * BIG).astype(np.float32)
    blk = same.astype(np.float32)
    chm = np.zeros((128, 8), np.float32)
    chm[:64, 0:4] = 1.0
    chm[64:, 4:8] = 1.0
    offd = np.tile(1.0 - np.eye(128, dtype=np.float32), (1, 4)).astype(np.float32)
    sel = np.zeros((2, 2, 128), np.float32)
    sel[0, 0, :] = 1.0
    sel[1, 1, :] = 1.0
    L = NLT * 128
    t = np.arange(L)
    row, col = t // 64, t % 64
    inv = (10000.0 ** (-np.arange(32, dtype=np.float32) / 32)).astype(np.float32)
    ang = np.stack([row[:, None].astype(np.float32) * inv, col[:, None].astype(np.float32) * inv], axis=1)
    cos, sin = np.cos(ang).astype(np.float32), np.sin(ang).astype(np.float32)
    cf = np.zeros((L, 2, 2, 32), np.float32)
    sf = np.zeros((L, 2, 2, 32), np.float32)
    cf[:, :, 0, :] = cos
    cf[:, :, 1, :] = cos
    sf[:, :, 0, :] = -sin
    sf[:, :, 1, :] = sin
    return dict(c_ident=np.eye(128, dtype=np.float32), c_ones=np.ones((128, 128), np.float32), c_tri=tri, c_mbig=mbig,
                c_blk=blk, c_chm=chm, c_offd=offd, c_sel=sel, ropec=cf.reshape(L, 128), ropes=sf.reshape(L, 128))


_PROG = {}


def run(inputs, NCT, NLT, DEPTH, debug=False, n_cores=8):
    key = (NCT, NLT, DEPTH, debug)
    if key not in _PROG:
        _PROG[key] = build_program(NCT, NLT, DEPTH, debug)
    nc = _PROG[key]
    f = lambda a: np.ascontiguousarray(np.asarray(a, dtype=np.float32))
    B = inputs["x"].shape[0]
    shared = dict(
        ada_w=f(inputs["ada_w"]), ada_b=f(inputs["ada_b"]), pre_w=f(inputs["pre_norm_w"]), post_w=f(inputs["post_norm_w"]),
        w_in=f(inputs["w_in"]), w_out=f(inputs["w_out"]),
        mib=f(inputs["mlstm_i_bias"]).reshape(DEPTH, 8), mfb=f(inputs["mlstm_f_bias"]).reshape(DEPTH, 8),
        rlg=f(inputs["ret_log_gamma"]).reshape(DEPTH, 8), qnw=f(inputs["attn_q_norm_w"]), knw=f(inputs["attn_k_norm_w"]),
        convw=f(inputs["dn_conv_w"]), alog=f(inputs["dn_a_log"]).reshape(DEPTH, 8), dtb=f(inputs["dn_dt_bias"]).reshape(DEPTH, 8),
        hnw=f(inputs["head_norm_w"]))
    shared.update(_consts(NLT))
    cc = f(inputs["c_ctx"]).reshape(16, 128)
    in_maps = []
    for i in range(n_cores):
        b = i % B
        m = dict(shared)
        m["x"] = f(inputs["x"][b])
        m["ctx"] = f(inputs["ctx"][b])
        m["cvec"] = np.ascontiguousarray(np.concatenate([f(inputs["c"][b]).reshape(16, 128), cc], axis=0))
        in_maps.append(m)
    res = run_bass_kernel_spmd(nc, in_maps, core_ids=list(range(n_cores)))
    return res


def kernel(**inputs):
    res = run(inputs, 2, 32, 2)
    B = inputs["x"].shape[0]
    out = np.stack([np.asarray(res.results[b]["y"], dtype=np.float32) for b in range(B)], axis=0)
    return out
```

```python
import contextlib
import math
import numpy as np
import ml_dtypes
import concourse.bass as bass
import concourse.mybir as mybir
from concourse.bass_utils import run_bass_kernel_spmd

F32 = mybir.dt.float32
BF16 = mybir.dt.bfloat16
AF = mybir.ActivationFunctionType
ALU = mybir.AluOpType
AX = mybir.AxisListType

EPOCH = 30000
DMA_RING = 8
BIG = 30000.0
D = 2048
KC = 16
EPS = 1e-6


class Buf:
    __slots__ = ("name", "w", "r")

    def __init__(self, name=""):
        self.name = name
        self.w = None
        self.r = []


class Sched:
    def __init__(self, nc, same_engine_sync=True):
        self.nc = nc
        self.same = same_engine_sync
        self.streams = {e: [] for e in ("pe", "act", "dve", "pool", "sp")}
        self.cnt = {e: 0 for e in self.streams}
        self.dcnt = {q: 0 for q in self.streams}
        self.known = {e: {} for e in self.streams}
        self.semkeys = set()

    def _tok_sem(self, tok):
        if tok[0] == "c":
            _, e, n = tok
            return ("c", e, (n - 1) // EPOCH), (n - 1) % EPOCH + 1
        _, q, i = tok
        return ("d", q, i % DMA_RING), 16 * (i // DMA_RING + 1)

    def _need(self, eng, tok, waits):
        if tok is None:
            return
        if tok[0] == "c" and tok[1] == eng and (eng == "pe" or not self.same):
            return
        key, val = self._tok_sem(tok)
        if self.known[eng].get(key, 0) >= val:
            return
        if waits.get(key, 0) < val:
            waits[key] = val

    def _deps(self, eng, reads, writes):
        waits = {}
        for b in reads:
            self._need(eng, b.w, waits)
        for b in writes:
            self._need(eng, b.w, waits)
            for t in b.r:
                if t[0] == "c" and t[1] == eng:
                    continue
                self._need(eng, t, waits)
        for k, v in waits.items():
            self.known[eng][k] = v
            self.semkeys.add(k)
        return waits

    def _commit(self, tok, reads, writes):
        for b in reads:
            b.r.append(tok)
        for b in writes:
            b.w = tok
            b.r = []

    def op(self, eng, fn, reads=(), writes=()):
        waits = self._deps(eng, reads, writes)
        self.cnt[eng] += 1
        tok = ("c", eng, self.cnt[eng])
        key, _ = self._tok_sem(tok)
        self.semkeys.add(key)
        self.streams[eng].append((waits, fn, key, 1))
        self._commit(tok, reads, writes)
        return tok

    def dma(self, q, fn, reads=(), writes=()):
        waits = self._deps(q, reads, writes)
        i = self.dcnt[q]
        self.dcnt[q] += 1
        tok = ("d", q, i)
        key, val = self._tok_sem(tok)
        self.semkeys.add(key)
        if i >= DMA_RING:
            pkey, pval = self._tok_sem(("d", q, i - DMA_RING))
            if self.known[q].get(pkey, 0) < pval:
                waits[pkey] = max(waits.get(pkey, 0), pval)
                self.known[q][pkey] = pval
        self.streams[q].append((waits, fn, key, 16))
        self._commit(tok, reads, writes)
        return tok

    def barrier(self):
        toks = []
        for e in self.streams:
            if self.cnt[e] > 0:
                toks.append(("c", e, self.cnt[e]))
            for i in range(max(0, self.dcnt[e] - DMA_RING), self.dcnt[e]):
                toks.append(("d", e, i))
        for e in self.streams:
            waits = {}
            for t in toks:
                if t[0] == "c" and t[1] == e:
                    continue
                self._need(e, t, waits)
            for k, v in waits.items():
                self.known[e][k] = v
                self.semkeys.add(k)
            self.streams[e].append((waits, None, None, 0))

    def emit(self):
        nc = self.nc
        keys = sorted(self.semkeys)
        with contextlib.ExitStack() as es:
            sems = {}
            for k in keys:
                sems[k] = es.enter_context(nc.semaphore("s_%s_%s_%d" % k))
            block = es.enter_context(nc.Block())

            def run(engname):
                def body(eng):
                    for waits, fn, key, inc in self.streams[engname]:
                        for k, v in waits.items():
                            eng.wait_ge(sems[k], v)
                        if fn is not None:
                            fn(eng).then_inc(sems[key], inc)
                return body
            block.tensor(run("pe"))
            block.scalar(run("act"))
            block.vector(run("dve"))
            block.gpsimd(run("pool"))
            block.sync(run("sp"))


_UID = [0]


def _uname(name):
    _UID[0] += 1
    return "%s_u%d" % (name, _UID[0])


class TPool:
    def __init__(self, es, nc, name, shape, dtype, n, psum=False):
        self.tiles = []
        for i in range(n):
            mk = nc.psum_tensor if psum else nc.sbuf_tensor
            t = es.enter_context(mk(_uname("%s%d" % (name, i)), shape, dtype))
            self.tiles.append((t, Buf("%s%d" % (name, i))))
        self.i = 0

    def next(self):
        t = self.tiles[self.i % len(self.tiles)]
        self.i += 1
        return t


OFF = dict(m_q=0, m_k=512, m_v=1024, m_o=1536, m_z=2048, r_q=2560, r_k=3072, r_v=3584, r_z=4096,
           a_q=4608, a_k=5120, a_v=5376, a_z=5632, d_q=6144, d_k=6656, d_v=7168, d_z=7680)
UW = 8192


def build_program(NCT, NLT, DEPTH, debug=False):
    NT = NCT + NLT
    T = NT * 128
    nc = bass.Bass("TRN2", target_bir_lowering=False)
    S = Sched(nc)

    def din(name, shape, dt=F32):
        return nc.dram_tensor(name, list(shape), dt, kind="ExternalInput").ap()

    def dscr(name, shape, dt=F32):
        return nc.dram_tensor(name, list(shape), dt, kind="Internal").ap()

    x_in = din("x", [NLT * 128, D])
    ctx_in = din("ctx", [NCT * 128, D])
    cvec = din("cvec", [32, 128])
    ada_w = din("ada_w", [DEPTH, D, 3 * D])
    ada_b = din("ada_b", [DEPTH, 3 * D])
    pre_w = din("pre_w", [DEPTH, D])
    post_w = din("post_w", [DEPTH, D])
    w_in = din("w_in", [DEPTH, D, 8224])
    w_out = din("w_out", [DEPTH, D, D])
    mib = din("mib", [DEPTH, 8])
    mfb = din("mfb", [DEPTH, 8])
    rlg = din("rlg", [DEPTH, 8])
    qnw = din("qnw", [DEPTH, 128])
    knw = din("knw", [DEPTH, 128])
    convw = din("convw", [DEPTH, 5, 1536])
    alog = din("alog", [DEPTH, 8])
    dtb = din("dtb", [DEPTH, 8])
    hnw = din("hnw", [DEPTH, 1536])
    c_ident = din("c_ident", [128, 128])
    c_ones = din("c_ones", [128, 128])
    c_tri = din("c_tri", [2, 128, 512])
    c_mbig = din("c_mbig", [2, 128, 512])
    c_blk = din("c_blk", [128, 128])
    c_chm = din("c_chm", [128, 8])
    c_offd = din("c_offd", [128, 512])
    c_sel = din("c_sel", [2, 2, 128])
    ropec = din("ropec", [NLT * 128, 128])
    ropes = din("ropes", [NLT * 128, 128])
    y_out = nc.dram_tensor("y", [NLT * 128, D], F32, kind="ExternalOutput").ap()

    X1 = dscr("X1", [T, D])
    U = dscr("U", [T, UW], BF16)
    UG = dscr("UG", [T, 32])
    OM = [dscr("OM%d" % d, [T, 512]) for d in range(2)]
    OR = [dscr("OR%d" % d, [T, 512]) for d in range(2)]
    OD = [dscr("OD%d" % d, [T, 512]) for d in range(2)]
    OA = dscr("OA", [T, 512])
    DQ = dscr("DQ", [T, 1536], BF16)
    dbg = {}
    if debug:
        dbg["U"] = nc.dram_tensor("dbgU", [T, UW], BF16, kind="ExternalOutput").ap()
        dbg["UG"] = nc.dram_tensor("dbgUG", [T, 32], F32, kind="ExternalOutput").ap()
        for nm in ("OM0", "OM1", "OR0", "OR1", "OD0", "OD1", "OA"):
            dbg[nm] = nc.dram_tensor("dbg" + nm, [T, 512], F32, kind="ExternalOutput").ap()
        dbg["X1"] = nc.dram_tensor("dbgX1", [T, D], F32, kind="ExternalOutput").ap()

    dbufs = {}

    def DB(name, n):
        k = (name, n)
        if k not in dbufs:
            dbufs[k] = Buf("%s_%d" % k)
        return dbufs[k]

    def mm(out, lhsT, rhs, start, stop, R, W):
        S.op("pe", lambda e: e.matmul(out, lhsT=lhsT, rhs=rhs, start=start, stop=stop), R, W)

    def act(out, in_, func, R, W, bias=None, scale=None, accum=None, eng="act"):
        kw = {}
        if bias is not None:
            kw["bias"] = bias
        if scale is not None:
            kw["scale"] = scale
        if accum is not None:
            kw["accum_out"] = accum
        S.op("act", lambda e: e.activation(out=out, in_=in_, func=func, **kw), R, W)

    def tt(eng, out, in0, in1, op, R, W):
        S.op(eng, lambda e: e.tensor_tensor(out=out, in0=in0, in1=in1, op=op), R, W)

    def ts(eng, out, in0, s1, s2, op0, op1, R, W):
        if s2 is None:
            S.op(eng, lambda e: e.tensor_scalar(out=out, in0=in0, scalar1=s1, scalar2=None, op0=op0), R, W)
        else:
            S.op(eng, lambda e: e.tensor_scalar(out=out, in0=in0, scalar1=s1, scalar2=s2, op0=op0, op1=op1), R, W)

    def stt(out, in0, scalar, in1, op0, op1, R, W):
        S.op("dve", lambda e: e.scalar_tensor_tensor(out=out, in0=in0, scalar=scalar, in1=in1, op0=op0, op1=op1), R, W)

    def cp(eng, out, in_, R, W):
        if eng == "act":
            S.op("act", lambda e: e.activation(out=out, in_=in_, func=AF.Copy), R, W)
        else:
            S.op(eng, lambda e: e.tensor_copy(out=out, in_=in_), R, W)

    def red(out, in_, R, W):
        S.op("dve", lambda e: e.tensor_reduce(out=out, in_=in_, axis=AX.X, op=ALU.add), R, W)

    def recip(out, in_, R, W):
        S.op("dve", lambda e: e.reciprocal(out=out, in_=in_), R, W)

    def mset(eng, ap, v, W):
        S.op(eng, lambda e: e.memset(ap, v), (), W)

    dq = [0]

    def dma(out, in_, R, W, q=None):
        if q is None:
            q = "sp"
            dq[0] += 1
        S.dma(q, lambda e: e.dma_start(out=out, in_=in_), R, W)

    def rsqrt_inplace(ap, tmp_scale, add, R):
        ts("dve", ap, ap, tmp_scale, add, ALU.mult, ALU.add, R, R)
        act(ap, ap, AF.Sqrt, R, R)
        recip(ap, ap, R, R)

    with contextlib.ExitStack() as es0:
        def sb(name, shape, dt=F32, es=es0):
            return es.enter_context(nc.sbuf_tensor(_uname(name), list(shape), dt)), Buf(name)

        ident, b_ident = sb("ident", [128, 128])
        ones, b_ones = sb("ones", [128, 128])
        tri = [sb("tri%d" % d, [128, 512]) for d in range(2)]
        mbig = [sb("mbig%d" % d, [128, 512]) for d in range(2)]
        blk, b_blk = sb("blk", [128, 128])
        chm, b_chm = sb("chm", [128, 8])
        offd, b_offd = sb("offd", [128, 512])
        dma(ident[:], c_ident[:, :], [], [b_ident])
        dma(ones[:], c_ones[:, :], [], [b_ones])
        for d in range(2):
            dma(tri[d][0][:], c_tri[d], [], [tri[d][1]])
            dma(mbig[d][0][:], c_mbig[d], [], [mbig[d][1]])
        dma(blk[:], c_blk[:, :], [], [b_blk])
        dma(chm[:], c_chm[:, :], [], [b_chm])
        dma(offd[:], c_offd[:, :], [], [b_offd])

        scT, b_scT = sb("scT", [128, DEPTH * 2 * 16])
        shT, b_shT = sb("shT", [128, DEPTH * 2 * 16])
        GPW = dscr("GPW", [DEPTH * 2, D])

        def scT_ap(l, which, k):
            i = (l * 2 + which) * 16 + k
            return scT[:, i:i + 1]

        def shT_ap(l, which, k):
            i = (l * 2 + which) * 16 + k
            return shT[:, i:i + 1]

        with contextlib.ExitStack() as es:
            pa, b_pa = sb("p0a", [64, 128], es=es)
            pb, b_pb = sb("p0b", [DEPTH * 48, 128], es=es)
            paT, b_paT = sb("p0aT", [128, 64], es=es)
            pbT, b_pbT = sb("p0bT", [128, DEPTH * 48], es=es)
            cs2, b_cs2 = sb("p0cs2", [128, 16, 2], es=es)
            modT, b_modT = sb("p0modT", [128, 48, 2], es=es)
            grow, b_grow = sb("p0grow", [2, D], es=es)
            brow, b_brow = sb("p0brow", [2, D], es=es)
            prow, b_prow = sb("p0prow", [2, D], es=es)
            slabs = TPool(es, nc, "p0slab", [128, 16, 512], F32, 2)
            ps_t = es.enter_context(nc.psum_tensor(_uname("p0ps_t"), [128, 512], F32)); b_ps_t = Buf()
            ps_m = es.enter_context(nc.psum_tensor(_uname("p0ps_m"), [128, 512], F32)); b_ps_m = Buf()
            ps_g = TPool(es, nc, "p0ps_g", [128, 512], F32, 2, psum=True)

            dma(pa[0:32, :], cvec[:, :], [], [b_pa])
            dma(pa[32:32 + DEPTH * 16, :], pre_w.rearrange("l (k p) -> (l k) p", p=128), [], [b_pa])
            dma(pb[:], ada_b.rearrange("l (k p) -> (l k) p", p=128), [], [b_pb])
            act(pa[0:32, :], pa[0:32, :], AF.Silu, [b_pa], [b_pa])
            S.op("pe", lambda e: e.transpose(out=ps_t[:, 0:64], in_=pa[:, :], identity=ident[0:64, 0:64]), [b_pa, b_ident], [b_ps_t])
            cp("dve", paT[:], ps_t[:, 0:64], [b_ps_t], [b_paT])
            S.op("pe", lambda e: e.transpose(out=ps_t[:, 128:128 + DEPTH * 48], in_=pb[:, :], identity=ident[0:DEPTH * 48, 0:DEPTH * 48]), [b_pb, b_ident], [b_ps_t])
            cp("dve", pbT[:], ps_t[:, 128:128 + DEPTH * 48], [b_ps_t], [b_pbT])
            cp("dve", cs2[:, :, 0], paT[:, 0:16], [b_paT], [b_cs2])
            cp("dve", cs2[:, :, 1], paT[:, 16:32], [b_paT], [b_cs2])
            for l in range(DEPTH):
                dma(brow[0:1, :], ada_b[l:l + 1, 2 * D:3 * D], [], [b_brow])
                dma(brow[1:2, :], ada_b[l:l + 1, 2 * D:3 * D], [], [b_brow])
                dma(prow[0:1, :], post_w[l:l + 1, :], [], [b_prow])
                dma(prow[1:2, :], post_w[l:l + 1, :], [], [b_prow])
                awv = ada_w[l].rearrange("(k p) n -> p k n", p=128)
                for sl in range(12):
                    slab, b_slab = slabs.next()
                    for kk in range(0, 16, 4):
                        dma(slab[:, kk:kk + 4, :], awv[:, kk:kk + 4, sl * 512:(sl + 1) * 512], [], [b_slab])
                    for j in range(4):
                        cb = sl * 4 + j
                        for k in range(KC):
                            mm(ps_m[:, cb * 2:cb * 2 + 2], slab[:, k, j * 128:(j + 1) * 128], cs2[:, k, :],
                               k == 0, k == KC - 1, [b_slab, b_cs2], [b_ps_m])
                    if sl >= 8:
                        pg, b_pg = ps_g.next()
                        for k in range(KC):
                            mm(pg[0:2, :], cs2[:, k, :], slab[:, k, :], k == 0, k == KC - 1, [b_slab, b_cs2], [b_pg])
                        cc = (sl - 8) * 512
                        tt("dve", grow[:, cc:cc + 512], pg[0:2, :], brow[:, cc:cc + 512], ALU.add, [b_pg, b_brow], [b_grow])
                cp("dve", modT[:].rearrange("p a b -> p (a b)"), ps_m[:, 0:96], [b_ps_m], [b_modT])
                tt("dve", grow[:], grow[:], prow[:], ALU.mult, [b_grow, b_prow], [b_grow])
                dma(GPW[l * 2:l * 2 + 2, :], grow[:], [b_grow], [DB("GPW", l)])
                for which in range(2):
                    i0 = (l * 2 + which) * 16
                    tt("dve", shT[:, i0:i0 + 16], modT[:, 0:16, which], pbT[:, l * 48:l * 48 + 16], ALU.add, [b_modT, b_pbT], [b_shT])
                    tt("dve", scT[:, i0:i0 + 16], modT[:, 16:32, which], pbT[:, l * 48 + 16:l * 48 + 32], ALU.add, [b_modT, b_pbT], [b_scT])
                    stt(scT[:, i0:i0 + 16], scT[:, i0:i0 + 16], 1.0, paT[:, 32 + l * 16:48 + l * 16], ALU.add, ALU.mult, [b_scT, b_paT], [b_scT])
            S.barrier()

        for l in range(DEPTH):
            last = (l == DEPTH - 1)

            def xsrc(n):
                if l == 0:
                    return (ctx_in[n * 128:(n + 1) * 128, :] if n < NCT else x_in[(n - NCT) * 128:(n - NCT + 1) * 128, :]), []
                return X1[n * 128:(n + 1) * 128, :], [DB("X1", n)]

            with contextlib.ExitStack() as es:
                TBMAX = (NT + 1) // 2
                hT = [sb("a_hT%d" % i, [128, KC, 128], BF16, es=es) for i in range(TBMAX)]
                xt_p = TPool(es, nc, "a_x", [128, D], F32, 2)
                st_p = TPool(es, nc, "a_st", [128, 4], F32, 2)
                wst_p = TPool(es, nc, "a_wst", [128, KC, 512], F32, 2)
                wbf_p = TPool(es, nc, "a_wbf", [128, KC, 512], BF16, 2)
                ub_p = TPool(es, nc, "a_ub", [128, 512], BF16, 3)
                ug_p = TPool(es, nc, "a_ug", [128, 32], F32, 2)
                ps_p = TPool(es, nc, "a_ps", [128, 512], F32, 6, psum=True)
                wv = w_in[l].rearrange("(k p) n -> p k n", p=128)
                blocks = [list(range(0, TBMAX)), list(range(TBMAX, NT))]

                def prep_tile(i, n):
                    which = 1 if n < NCT else 0
                    xt, b_xt = xt_p.next()
                    st, b_st = st_p.next()
                    src, sr = xsrc(n)
                    dma(xt[:], src, sr, [b_xt])
                    ps, b_ps = ps_p.next()
                    for c4 in range(4):
                        act(ps[:, :], xt[:, c4 * 512:(c4 + 1) * 512], AF.Square, [b_xt], [b_ps, b_st], accum=st[:, c4:c4 + 1])
                    red(st[:, 0:1], st[:, 0:4], [b_st], [b_st])
                    rsqrt_inplace(st[:, 0:1], 1.0 / D, EPS, [b_st])
                    ts("dve", xt[:], xt[:], st[:, 0:1], None, ALU.mult, None, [b_xt, b_st], [b_xt])
                    ht, b_ht = hT[i]
                    for k4 in range(4):
                        ps, b_ps = ps_p.next()
                        for j in range(4):
                            k = k4 * 4 + j
                            S.op("pe", (lambda k=k, j=j, ps=ps, xt=xt: lambda e: e.transpose(out=ps[:, j * 128:(j + 1) * 128], in_=xt[:, k * 128:(k + 1) * 128], identity=ident[:]))(),
                                 [b_xt, b_ident], [b_ps])
                        for j in range(4):
                            k = k4 * 4 + j
                            act(ht[:, k, :], ps[:, j * 128:(j + 1) * 128], AF.Identity, [b_ps, b_scT, b_shT], [b_ht],
                                bias=shT_ap(l, which, k), scale=scT_ap(l, which, k))

                for bi, tb in enumerate(blocks):
                    if not tb:
                        continue
                    nxt_tb = blocks[bi + 1] if bi + 1 < len(blocks) else []
                    if bi == 0:
                        for i, n in enumerate(tb):
                            prep_tile(i, n)
                    def load_w(cb):
                        wst, b_wst = wst_p.next()
                        wbf, b_wbf = wbf_p.next()
                        if cb < 16:
                            o0 = cb * 512 + (16 if cb >= 5 else 0)
                            wdt = 512
                            for kk in range(0, 16, 4):
                                dma(wst[:, kk:kk + 4, :], wv[:, kk:kk + 4, o0:o0 + 512], [], [b_wst])
                        else:
                            wdt = 32
                            dma(wst[:, :, 0:16], wv[:, :, 2560:2576], [], [b_wst])
                            dma(wst[:, :, 16:32], wv[:, :, 8208:8224], [], [b_wst])
                        cp("dve", wbf[:, 0:8, 0:wdt], wst[:, 0:8, 0:wdt], [b_wst], [b_wbf])
                        cp("dve", wbf[:, 8:16, 0:wdt], wst[:, 8:16, 0:wdt], [b_wst], [b_wbf])
                        return wbf, b_wbf, wdt
                    cb_order = [16] + list(range(16))
                    nxt_w = load_w(cb_order[0])
                    for ci, cb in enumerate(cb_order):
                        wbf, b_wbf, wdt = nxt_w
                        if ci + 1 < 17:
                            nxt_w = load_w(cb_order[ci + 1])
                        for i, n in enumerate(tb):
                            ht, b_ht = hT[i]
                            ps, b_ps = ps_p.next()
                            for k in range(KC):
                                mm(ps[:, 0:wdt], ht[:, k, :], wbf[:, k, 0:wdt], k == 0, k == KC - 1, [b_ht, b_wbf], [b_ps])
                            if cb < 16:
                                ub, b_ub = ub_p.next()
                                cp("act", ub[:, :], ps[:, 0:512], [b_ps], [b_ub])
                                dma(U[n * 128:(n + 1) * 128, cb * 512:(cb + 1) * 512], ub[:, :], [b_ub], [DB("U", n)], q="act")
                            else:
                                ug, b_ug = ug_p.next()
                                cp("act", ug[:, :], ps[:, 0:32], [b_ps], [b_ug])
                                dma(UG[n * 128:(n + 1) * 128, :], ug[:, :], [b_ug], [DB("UG", n)], q="act")
                            if ci == 16 and i < len(nxt_tb):
                                prep_tile(i, nxt_tb[i])
                S.barrier()

            if debug and l == debug - 1:
                pass

            ctx_needed = not last
            fwd_order = list(range(NT))
            bwd_order = list(range(NCT - 1, -1, -1)) + list(range(NT - 1, NCT - 1, -1))

            with contextlib.ExitStack() as es:
                QT, b_QT = sb("at_QT", [128, 4, T], BF16, es=es)
                KT, b_KT = sb("at_KT", [128, 2, T], BF16, es=es)
                V1, b_V1 = sb("at_V1", [128, NT, 2, 132], BF16, es=es)
                nwq, b_nwq = sb("at_nwq", [128, 128], es=es)
                nwk, b_nwk = sb("at_nwk", [128, 128], es=es)
                st_p = TPool(es, nc, "at_st", [128, 8], F32, 4)
                pt_p = TPool(es, nc, "at_pt", [128, 512], BF16, 3)
                ob_p = TPool(es, nc, "at_ob", [128, 128], F32, 3)
                ps_p = TPool(es, nc, "at_ps", [128, 512], F32, 4, psum=True)
                acc_p = TPool(es, nc, "at_acc", [128, 512], F32, 4, psum=True)
                dma(nwq[:], qnw[l:l + 1, :].partition_broadcast(128), [], [b_nwq])
                dma(nwk[:], knw[l:l + 1, :].partition_broadcast(128), [], [b_nwk])
                mset("dve", V1[:, :, :, 128:129], 1.0, [b_V1])
                ts("dve", nwq[:], nwq[:], 128.0 ** -0.5, None, ALU.mult, None, [b_nwq], [b_nwq])

                with contextlib.ExitStack() as es2:
                    raw_p = TPool(es2, nc, "at_raw", [128, 1024], BF16, 3)
                    q_p = TPool(es2, nc, "at_q", [128, 768], F32, 3)
                    q2_p = TPool(es2, nc, "at_q2", [128, 768], F32, 3)
                    sw_p = TPool(es2, nc, "at_sw", [128, 768], F32, 3)
                    rc_p = TPool(es2, nc, "at_rc", [128, 128], F32, 3)
                    rs_p = TPool(es2, nc, "at_rs", [128, 128], F32, 3)

                    def a1_tile(n):
                        raw, b_raw = raw_p.next()
                        dma(raw[:, 0:1024], U[n * 128:(n + 1) * 128, OFF["a_q"]:OFF["a_q"] + 1024], [DB("U", n)], [b_raw])
                        q, b_q = q_p.next()
                        q2, b_q2 = q2_p.next()
                        st, b_st = st_p.next()
                        cp("dve", q[:, :], raw[:, 0:768], [b_raw], [b_q])
                        cp("pool", V1[:, n, :, 0:128], raw[:, 768:1024].rearrange("p (h d) -> p h d", h=2), [b_raw], [b_V1])
                        tt("dve", q2[:, :], q[:, :], q[:, :], ALU.mult, [b_q], [b_q2])
                        red(st[:, 0:6], q2[:, :].rearrange("p (h d) -> p h d", h=6), [b_q2], [b_st])
                        yield
                        rsqrt_inplace(st[:, 0:6], 1.0 / 128, EPS, [b_st])
                        yield
                        q3 = q[:, :].rearrange("p (h d) -> p h d", h=6)
                        tt("dve", q3, q3, st[:, 0:6].unsqueeze(2).to_broadcast([128, 6, 128]), ALU.mult, [b_q, b_st], [b_q])
                        tt("pool", q3[:, 0:4, :], q3[:, 0:4, :], nwq[:, :].unsqueeze(1).to_broadcast([128, 4, 128]), ALU.mult, [b_q, b_nwq], [b_q])
                        tt("pool", q3[:, 4:6, :], q3[:, 4:6, :], nwk[:, :].unsqueeze(1).to_broadcast([128, 2, 128]), ALU.mult, [b_q, b_nwk], [b_q])
                        yield
                        if n >= NCT:
                            rc, b_rc = rc_p.next()
                            rs, b_rs = rs_p.next()
                            sw, b_sw = sw_p.next()
                            r0 = (n - NCT) * 128
                            dma(rc[:], ropec[r0:r0 + 128, :], [], [b_rc])
                            dma(rs[:], ropes[r0:r0 + 128, :], [], [b_rs])
                            q4 = q[:, :].rearrange("p (g two f) -> p g two f", two=2, f=32)
                            s4 = sw[:, :].rearrange("p (g two f) -> p g two f", two=2, f=32)
                            cp("pool", s4[:, :, 0, :], q4[:, :, 1, :], [b_q], [b_sw])
                            cp("pool", s4[:, :, 1, :], q4[:, :, 0, :], [b_q], [b_sw])
                            s3 = sw[:, :].rearrange("p (h d) -> p h d", h=6)
                            tt("dve", q3, q3, rc[:, :].unsqueeze(1).to_broadcast([128, 6, 128]), ALU.mult, [b_q, b_rc], [b_q])
                            tt("pool", s3, s3, rs[:, :].unsqueeze(1).to_broadcast([128, 6, 128]), ALU.mult, [b_sw, b_rs], [b_sw])
                            yield
                            tt("dve", q3, q3, s3, ALU.add, [b_q, b_sw], [b_q])
                            yield
                        for half in range(2):
                            ps, b_ps = ps_p.next()
                            nh = 4 if half == 0 else 2
                            for j in range(nh):
                                hh = half * 4 + j
                                S.op("pe", (lambda ps=ps, j=j, q=q, hh=hh: lambda e: e.transpose(out=ps[:, j * 128:(j + 1) * 128], in_=q[:, hh * 128:(hh + 1) * 128], identity=ident[:]))(),
                                     [b_q, b_ident], [b_ps])
                            if half == 0:
                                cp("act", QT[:, :, n * 128:(n + 1) * 128], ps[:, 0:512].rearrange("p (h t) -> p h t", h=4), [b_ps], [b_QT])
                            else:
                                cp("act", KT[:, :, n * 128:(n + 1) * 128], ps[:, 0:256].rearrange("p (h t) -> p h t", h=2), [b_ps], [b_KT])

                    pend = list(range(NT))
                    act_g = []
                    while pend or act_g:
                        while pend and len(act_g) < 3:
                            act_g.append(a1_tile(pend.pop(0)))
                        alive = []
                        for g_ in act_g:
                            try:
                                next(g_)
                                alive.append(g_)
                            except StopIteration:
                                pass
                        act_g = alive
                    S.barrier()

                def attend(qtiles, ktiles):
                    for h in range(4):
                        kvh = h // 2
                        for qb0 in range(0, len(qtiles), 4):
                            qts = qtiles[qb0:qb0 + 4]
                            nq = len(qts)
                            q0 = qts[0] * 128
                            accs = [acc_p.next() for _ in range(nq)]
                            for si, s in enumerate(ktiles):
                                ps, b_ps = ps_p.next()
                                mm(ps[:, 0:nq * 128], KT[:, kvh, s * 128:(s + 1) * 128], QT[:, h, q0:q0 + nq * 128], True, True, [b_KT, b_QT], [b_ps])
                                pt, b_pt = pt_p.next()
                                act(pt[:, 0:nq * 128], ps[:, 0:nq * 128], AF.Exp, [b_ps], [b_pt])
                                for j in range(nq):
                                    mm(accs[j][0][:, 0:129], pt[:, j * 128:(j + 1) * 128], V1[:, s, kvh, 0:129], si == 0, si == len(ktiles) - 1,
                                       [b_pt, b_V1], [accs[j][1]])
                                yield
                            for j in range(nq):
                                ac, b_ac = accs[j]
                                ob, b_ob = ob_p.next()
                                st, b_st = st_p.next()
                                act(st[:, 0:1], ac[:, 128:129], AF.Ln, [b_ac], [b_st])
                                act(st[:, 0:1], st[:, 0:1], AF.Exp, [b_st], [b_st], scale=-1.0)
                                act(ob[:, :], ac[:, 0:128], AF.Identity, [b_ac, b_st], [b_ob], scale=st[:, 0:1])
                                n = qts[j]
                                dma(OA[n * 128:(n + 1) * 128, h * 128:(h + 1) * 128], ob[:, :], [b_ob], [DB("OA", n)], q="act")

                def gen_attn():
                    yield from attend(list(range(NCT, NT)), list(range(NT)))
                    if ctx_needed:
                        yield from attend(list(range(NCT)), list(range(NCT)))

                with contextlib.ExitStack() as es3:
                    cw = [sb("p_cw%d" % j, [128, 1536], es=es3) for j in range(5)]
                    for j in range(5):
                        dma(cw[j][0][:], convw[l, j:j + 1, :].partition_broadcast(128), [], [cw[j][1]])

                    def gen_prep(tiles, gi):
                        sh_p = TPool(es3, nc, "p%d_sh" % gi, [128, 1536], BF16, 2)
                        acc_p2 = TPool(es3, nc, "p%d_acc" % gi, [128, 1536], F32, 1)
                        tmp_p = TPool(es3, nc, "p%d_tmp" % gi, [128, 1536], F32, 2)
                        pst_p = TPool(es3, nc, "p%d_st" % gi, [128, 16], F32, 2)
                        pob_p = TPool(es3, nc, "p%d_ob" % gi, [128, 1536], BF16, 1)
                        rraw_p = TPool(es3, nc, "p%d_rraw" % gi, [128, 1024], BF16, 2)
                        rq_p = TPool(es3, nc, "p%d_rq" % gi, [128, 1024], F32, 1)
                        rsw_p = TPool(es3, nc, "p%d_rsw" % gi, [128, 1024], F32, 1)
                        prc_p = TPool(es3, nc, "p%d_rc" % gi, [128, 128], F32, 1)
                        prs_p = TPool(es3, nc, "p%d_rs" % gi, [128, 128], F32, 1)
                        yield
                        for n in tiles:
                            rows = slice(n * 128, (n + 1) * 128)
                            seg0, seg1 = (0, NCT * 128) if n < NCT else (NCT * 128, T)
                            acc, b_acc = acc_p2.next()
                            for j in range(5):
                                sh, b_sh = sh_p.next()
                                lo = n * 128 + j - 2
                                hi = lo + 128
                                clo, chi = max(lo, seg0), min(hi, seg1)
                                if clo > lo or chi < hi:
                                    mset("pool", sh[:, :], 0.0, [b_sh])
                                dma(sh[clo - lo:chi - lo, :], U[clo:chi, OFF["d_q"]:OFF["d_q"] + 1536],
                                    [DB("U", m) for m in range(max(0, n - 1), min(NT, n + 2))], [b_sh])
                                if j == 0:
                                    tt("pool", acc[:, :], sh[:, :], cw[0][0][:, :], ALU.mult, [b_sh, cw[0][1]], [b_acc])
                                else:
                                    tmp, b_tmp = tmp_p.next()
                                    tt("pool" if j == 1 else "dve", tmp[:, :], sh[:, :], cw[j][0][:, :], ALU.mult, [b_sh, cw[j][1]], [b_tmp])
                                    yield
                                    tt("dve" if j % 2 else "pool", acc[:, :], acc[:, :], tmp[:, :], ALU.add, [b_acc, b_tmp], [b_acc])
                                yield
                            for _w in range(8):
                                yield
                            act(acc[:, :], acc[:, :], AF.Silu, [b_acc], [b_acc])
                            yield
                            tmp, b_tmp = tmp_p.next()
                            st, b_st = pst_p.next()
                            tt("dve", tmp[:, 0:1024], acc[:, 0:1024], acc[:, 0:1024], ALU.mult, [b_acc], [b_tmp])
                            yield
                            red(st[:, 0:8], tmp[:, 0:1024].rearrange("p (h d) -> p h d", h=8), [b_tmp], [b_st])
                            yield
                            ts("dve", st[:, 0:8], st[:, 0:8], 1.0, EPS, ALU.mult, ALU.add, [b_st], [b_st])
                            for _w in range(8):
                                yield
                            act(st[:, 0:8], st[:, 0:8], AF.Sqrt, [b_st], [b_st])
                            yield
                            recip(st[:, 0:8], st[:, 0:8], [b_st], [b_st])
                            yield
                            ts("dve", st[:, 0:4], st[:, 0:4], 128.0 ** -0.5, None, ALU.mult, None, [b_st], [b_st])
                            yield
                            qk3 = acc[:, 0:1024].rearrange("p (h d) -> p h d", h=8)
                            tt("dve", qk3, qk3, st[:, 0:8].unsqueeze(2).to_broadcast([128, 8, 128]), ALU.mult, [b_acc, b_st], [b_acc])
                            yield
                            ob, b_ob = pob_p.next()
                            for _w in range(8):
                                yield
                            cp("act", ob[:, :], acc[:, :], [b_acc], [b_ob])
                            dma(DQ[rows, :], ob[:, :], [b_ob], [DB("DQ", n)], q="act")
                            yield
                            rr, b_rr = rraw_p.next()
                            rq, b_rq = rq_p.next()
                            dma(rr[:, :], U[rows, OFF["r_q"]:OFF["r_q"] + 1024], [DB("U", n)], [b_rr])
                            cp("pool", rq[:, :], rr[:, :], [b_rr], [b_rq])
                            yield
                            if n >= NCT:
                                rc, b_rc = prc_p.next()
                                rs, b_rs = prs_p.next()
                                sw, b_sw = rsw_p.next()
                                r0 = (n - NCT) * 128
                                dma(rc[:], ropec[r0:r0 + 128, :], [], [b_rc])
                                dma(rs[:], ropes[r0:r0 + 128, :], [], [b_rs])
                                q4 = rq[:, :].rearrange("p (g two f) -> p g two f", two=2, f=32)
                                s4 = sw[:, :].rearrange("p (g two f) -> p g two f", two=2, f=32)
                                cp("pool", s4[:, :, 0, :], q4[:, :, 1, :], [b_rq], [b_sw])
                                cp("pool", s4[:, :, 1, :], q4[:, :, 0, :], [b_rq], [b_sw])
                                q3 = rq[:, :].rearrange("p (h d) -> p h d", h=8)
                                s3 = sw[:, :].rearrange("p (h d) -> p h d", h=8)
                                tt("dve", q3, q3, rc[:, :].unsqueeze(1).to_broadcast([128, 8, 128]), ALU.mult, [b_rq, b_rc], [b_rq])
                                tt("pool", s3, s3, rs[:, :].unsqueeze(1).to_broadcast([128, 8, 128]), ALU.mult, [b_sw, b_rs], [b_sw])
                                yield
                                tt("dve", q3, q3, s3, ALU.add, [b_rq, b_sw], [b_rq])
                                yield
                            ts("dve", rq[:, 512:1024], rq[:, 512:1024], 128.0 ** -0.5, None, ALU.mult, None, [b_rq], [b_rq])
                            yield
                            rr2, b_rr2 = rraw_p.next()
                            for _w in range(8):
                                yield
                            cp("act", rr2[:, :], rq[:, :], [b_rq], [b_rr2])
                            dma(U[rows, OFF["r_q"]:OFF["r_q"] + 1024], rr2[:, :], [b_rr2], [DB("Ur", n)], q="act")
                            yield

                    gens = [gen_attn(), gen_prep(list(range(0, NT, 2)), 0), gen_prep(list(range(1, NT, 2)), 1)]
                    while gens:
                        alive = []
                        for g_ in gens:
                            try:
                                next(g_)
                                alive.append(g_)
                            except StopIteration:
                                pass
                        gens = alive
                    S.barrier()

            for mixer in ("m", "r", "d"):
                with contextlib.ExitStack() as es:
                    VW = 129 if mixer == "m" else 128
                    OUT = dict(m=OM, r=OR, d=OD)[mixer]
                    ugt, b_ugt = sb("g_ug", [128, NT, 32], es=es)
                    for n in range(NT):
                        dma(ugt[:, n, :], UG[n * 128:(n + 1) * 128, :], [DB("UG", n)], [b_ugt])
                    prm, b_prm = sb("g_prm", [128, 32], es=es)
                    G = {}
                    NG = NT * 4
                    for d in range(2):
                        for nm in ("lf", "li", "g", "gend", "eg", "kend", "rowp", "beta", "beg"):
                            G[(nm, d)] = sb("g_%s%d" % (nm, d), [128, NT, 4], es=es)
                        G[("gam", d)] = sb("g_gam%d" % d, [128, NT, 8], es=es)
                    gtmp, b_gtmp = sb("g_tmp", [128, NT, 8], es=es)
                    psA = TPool(es, nc, "r_psA", [128, 512], F32, 4, psum=True)
                    psB = TPool(es, nc, "r_psB", [128, 512], F32, 4, psum=True)
                    gps = psA
                    if mixer == "m":
                        dma(prm[:, 0:8], mib[l:l + 1, :].partition_broadcast(128), [], [b_prm])
                        dma(prm[:, 8:16], mfb[l:l + 1, :].partition_broadcast(128), [], [b_prm])
                    elif mixer == "r":
                        dma(prm[:, 0:8], rlg[l:l + 1, :].partition_broadcast(128), [], [b_prm])
                    else:
                        dma(prm[:, 0:8], alog[l:l + 1, :].partition_broadcast(128), [], [b_prm])
                        dma(prm[:, 8:16], dtb[l:l + 1, :].partition_broadcast(128), [], [b_prm])
                        act(prm[:, 0:8], prm[:, 0:8], AF.Exp, [b_prm], [b_prm])
                    for d in range(2):
                        lf, b_lf = G[("lf", d)]
                        li, b_li = G[("li", d)]
                        g, b_g = G[("g", d)]
                        gend, b_gend = G[("gend", d)]
                        eg, b_eg = G[("eg", d)]
                        kend, b_kend = G[("kend", d)]
                        rowp, b_rowp = G[("rowp", d)]
                        beta, b_beta = G[("beta", d)]
                        beg, b_beg = G[("beg", d)]
                        gam, b_gam = G[("gam", d)]

                        def prmb(c0):
                            return prm[:, c0 + d * 4:c0 + d * 4 + 4].unsqueeze(1).to_broadcast([128, NT, 4])
                        if mixer == "m":
                            tt("dve", li[:], ugt[:, :, d * 4:d * 4 + 4], prmb(0), ALU.add, [b_ugt, b_prm], [b_li])
                            tt("dve", lf[:], ugt[:, :, 8 + d * 4:12 + d * 4], prmb(8), ALU.add, [b_ugt, b_prm], [b_lf])
                            act(lf[:], lf[:], AF.Exp, [b_lf], [b_lf], scale=-1.0)
                            act(lf[:], lf[:], AF.Ln, [b_lf], [b_lf], bias=1.0)
                            ts("dve", lf[:], lf[:], -1.0, None, ALU.mult, None, [b_lf], [b_lf])
                        elif mixer == "r":
                            mset("dve", li[:], 0.0, [b_li])
                            mset("dve", lf[:], 0.0, [b_lf])
                            tt("dve", lf[:], lf[:], prmb(0), ALU.add, [b_lf, b_prm], [b_lf])
                        else:
                            mset("dve", li[:], 0.0, [b_li])
                            tt("dve", lf[:], ugt[:, :, 16 + d * 4:20 + d * 4], prmb(8), ALU.add, [b_ugt, b_prm], [b_lf])
                            act(lf[:], lf[:], AF.Exp, [b_lf], [b_lf])
                            act(lf[:], lf[:], AF.Ln, [b_lf], [b_lf], bias=1.0)
                            tt("dve", lf[:], lf[:], prmb(0), ALU.mult, [b_lf, b_prm], [b_lf])
                            ts("dve", lf[:], lf[:], -1.0, None, ALU.mult, None, [b_lf], [b_lf])
                            act(beta[:], ugt[:, :, 24 + d * 4:28 + d * 4], AF.Exp, [b_ugt], [b_beta], scale=-1.0)
                            ts("dve", beta[:], beta[:], 1.0, None, ALU.add, None, [b_beta], [b_beta])
                            recip(beta[:], beta[:], [b_beta], [b_beta])
                        lf2 = lf[:].rearrange("p n h -> p (n h)")
                        ps, b_ps = gps.next()
                        mm(ps[:, 0:NG], tri[d][0][:, 0:128], lf2, True, True, [tri[d][1], b_lf], [b_ps])
                        cp("dve", g[:].rearrange("p n h -> p (n h)"), ps[:, 0:NG], [b_ps], [b_g])
                        ps, b_ps = gps.next()
                        mm(ps[:, 0:NG], blk[:, :], lf2, True, True, [b_blk, b_lf], [b_ps])
                        cp("dve", gend[:].rearrange("p n h -> p (n h)"), ps[:, 0:NG], [b_ps], [b_gend])
                        act(eg[:], g[:], AF.Exp, [b_g], [b_eg])
                        tt("dve", rowp[:], li[:], g[:], ALU.subtract, [b_li, b_g], [b_rowp])
                        tt("dve", kend[:], gend[:], rowp[:], ALU.add, [b_gend, b_rowp], [b_kend])
                        act(kend[:], kend[:], AF.Exp, [b_kend], [b_kend])
                        if mixer == "d":
                            tt("dve", beg[:], beta[:], eg[:], ALU.mult, [b_beta, b_eg], [b_beg])
                        g4 = gtmp[:].rearrange("p n (c h) -> p n c h", c=2)
                        for c in range(2):
                            tt("dve", g4[:, :, c, :], lf[:], chm[:, c * 4:c * 4 + 4].unsqueeze(1).to_broadcast([128, NT, 4]), ALU.mult, [b_lf, b_chm], [b_gtmp])
                        ps, b_ps = gps.next()
                        mm(ps[:, 0:NT * 8], ones[:, :], gtmp[:].rearrange("p n c -> p (n c)"), True, True, [b_ones, b_gtmp], [b_ps])
                        act(gam[:].rearrange("p n c -> p (n c)"), ps[:, 0:NT * 8], AF.Exp, [b_ps], [b_gam])

                    KSLOT = 3 if mixer == "d" else 4
                    st_p = TPool(es, nc, "r_st", [128, 16], F32, 12)
                    slots = []
                    for si in range(KSLOT):
                        Bd = {}
                        Bd["raw"] = sb("s%d_raw" % si, [128, 1536], BF16, es=es)
                        Bd["qkv"] = sb("s%d_qkv" % si, [128, 1536], F32, es=es)
                        Bd["qs"] = sb("s%d_qs" % si, [128, 512], F32, es=es)
                        Bd["ke"] = sb("s%d_ke" % si, [128, 512], BF16, es=es)
                        Bd["v1"] = sb("s%d_v1" % si, [128, 4, 132], BF16, es=es)
                        for nm in ("QT", "QtT", "KT", "QKT"):
                            Bd[nm] = sb("s%d_%s" % (si, nm), [128, 512], BF16, es=es)
                        Bd["ep"] = sb("s%d_ep" % si, [128, 512], F32, es=es)
                        Bd["DT"] = sb("s%d_DT" % si, [128, 512], F32, es=es)
                        Bd["ob"] = sb("s%d_ob" % si, [128, 512], F32, es=es)
                        if mixer == "d":
                            Bd["kb"] = sb("s%d_kb" % si, [128, 512], F32, es=es)
                            Bd["wk"] = sb("s%d_wk" % si, [128, 512], F32, es=es)
                            Bd["KbT"] = sb("s%d_KbT" % si, [128, 512], BF16, es=es)
                            Bd["WkT"] = sb("s%d_WkT" % si, [128, 512], BF16, es=es)
                            Bd["AT"] = [sb("s%d_AT%d" % (si, j), [128, 512], F32, es=es) for j in range(3)]
                            Bd["A"] = [sb("s%d_A%d" % (si, j), [128, 512], F32, es=es) for j in range(3)]
                            Bd["R"] = [sb("s%d_R%d" % (si, j), [128, 4, 256], F32, es=es) for j in range(3)]
                            Bd["U"] = [sb("s%d_U%d" % (si, j), [128, 512], BF16, es=es) for j in range(2)]
                        slots.append(Bd)
                    state = [sb("r_S%d" % d, [128, 4, 132], es=es) for d in range(2)]
                    stateb = [sb("r_Sb%d" % d, [128, 4, 132], BF16, es=es) for d in range(2)]
                    for d in range(2):
                        mset("dve", state[d][0][:], 0.0, [state[d][1]])
                        mset("dve", stateb[d][0][:], 0.0, [stateb[d][1]])

                    DTc = None
                    if mixer == "r":
                        DTc = [sb("r_DTc%d" % d_, [128, 512], es=es) for d_ in range(2)]
                        epc, b_epc = sb("r_epc", [128, 512], es=es)
                        for d_ in range(2):
                            lf_, b_lf_ = G[("lf", d_)]
                            rowp_, b_rowp_ = G[("rowp", d_)]
                            tt("dve", epc[:, :].rearrange("p (h t) -> p h t", h=4), tri[d_][0][:, :].rearrange("p (h t) -> p h t", h=4),
                               lf_[:, 0, :].unsqueeze(2).to_broadcast([128, 4, 128]), ALU.mult, [tri[d_][1], b_lf_], [b_epc])
                            psE_, b_psE_ = psB.next()
                            mm(psE_[:, :], ones[:, :], epc[:, :], True, False, [b_ones, b_epc], [b_psE_])
                            mm(psE_[:, :], ident[:, :], mbig[d_][0][:, :], False, True, [b_ident, mbig[d_][1]], [b_psE_])
                            for h_ in range(4):
                                act(DTc[d_][0][:, h_ * 128:(h_ + 1) * 128], psE_[:, h_ * 128:(h_ + 1) * 128], AF.Exp, [b_psE_, b_rowp_], [DTc[d_][1]],
                                    bias=rowp_[:, 0, h_:h_ + 1])

                    def transpose4(src, b_src, dst, b_dst):
                        ps, b_ps = psA.next()
                        for h in range(4):
                            S.op("pe", (lambda ps=ps, h=h, src=src: lambda e: e.transpose(out=ps[:, h * 128:(h + 1) * 128], in_=src[:, h * 128:(h + 1) * 128], identity=ident[:]))(),
                                 [b_src, b_ident], [b_ps])
                        cp("act", dst[:, :], ps[:, :], [b_ps], [b_dst])

                    def unit(n, d, Bd, kseq):
                        eg, b_eg = G[("eg", d)]
                        kend, b_kend = G[("kend", d)]
                        rowp, b_rowp = G[("rowp", d)]
                        lf, b_lf = G[("lf", d)]
                        gam, b_gam = G[("gam", d)]
                        beta, b_beta = G[("beta", d)]
                        beg, b_beg = G[("beg", d)]
                        raw, b_raw = Bd["raw"]
                        qkv, b_qkv = Bd["qkv"]
                        rows = slice(n * 128, (n + 1) * 128)
                        if mixer == "d":
                            dma(raw[:, :], DQ[rows, :], [DB("DQ", n)], [b_raw])
                        else:
                            c0 = OFF["m_q"] if mixer == "m" else OFF["r_q"]
                            dma(raw[:, :], U[rows, c0:c0 + 1536], [DB("U", n), DB("Ur", n)], [b_raw])
                        cp("dve", qkv[:, :], raw[:, :], [b_raw], [b_qkv])
                        if mixer == "m":
                            ts("dve", qkv[:, 0:512], qkv[:, 0:512], 128.0 ** -0.5, None, ALU.mult, None, [b_qkv], [b_qkv])
                        yield
                        qv = qkv[:, 0:512]
                        kv = qkv[:, 512:1024]
                        vv = qkv[:, 1024:1536]
                        h4 = lambda ap: ap.rearrange("p (h d) -> p h d", h=4)
                        bc = lambda t_: t_[:, n, :].unsqueeze(2).to_broadcast([128, 4, 128])
                        qs, b_qs = Bd["qs"]
                        ke, b_ke = Bd["ke"]
                        tt("dve", h4(qs[:, :]), h4(qv), bc(eg), ALU.mult, [b_qkv, b_eg], [b_qs])
                        tt("pool", h4(ke[:, :]), h4(kv), bc(kend), ALU.mult, [b_qkv, b_kend], [b_ke])
                        if mixer == "d":
                            kb, b_kb = Bd["kb"]
                            tt("dve", h4(kb[:, :]), h4(kv), bc(beta), ALU.mult, [b_qkv, b_beta], [b_kb])
                            R, b_R = Bd["R"][0]
                            tt("dve", R[:, :, 0:128], h4(vv), bc(beta), ALU.mult, [b_qkv, b_beta], [b_R])
                            tt("dve", R[:, :, 128:256], h4(kv), bc(beg), ALU.mult, [b_qkv, b_beg], [b_R])
                        else:
                            v1, b_v1 = Bd["v1"]
                            cp("pool", v1[:, :, 0:128], h4(vv), [b_qkv], [b_v1])
                            if mixer == "m":
                                mset("pool", v1[:, :, 128:129], 1.0, [b_v1])
                        yield
                        QTt, b_QTt = Bd["QT"]
                        QtT, b_QtT = Bd["QtT"]
                        KTt, b_KTt = Bd["KT"]
                        transpose4(qv, b_qkv, QTt, b_QTt)
                        yield
                        transpose4(kv, b_qkv, KTt, b_KTt)
                        yield
                        transpose4(qs, b_qs, QtT, b_QtT)
                        yield
                        if mixer == "d":
                            KbT, b_KbT = Bd["KbT"]
                            transpose4(kb, b_kb, KbT, b_KbT)
                            yield
                        if mixer == "r":
                            DT, b_DT = DTc[d]
                        else:
                            ep, b_ep = Bd["ep"]
                            tt("dve", h4(ep[:, :]), h4(tri[d][0][:, :]), bc(lf), ALU.mult, [tri[d][1], b_lf], [b_ep])
                            psE, b_psE = psB.next()
                            mm(psE[:, :], ones[:, :], ep[:, :], True, False, [b_ones, b_ep], [b_psE])
                            mm(psE[:, :], ident[:, :], mbig[d][0][:, :], False, True, [b_ident, mbig[d][1]], [b_psE])
                            DT, b_DT = Bd["DT"]
                            for h in range(4):
                                act(DT[:, h * 128:(h + 1) * 128], psE[:, h * 128:(h + 1) * 128], AF.Exp, [b_psE, b_rowp], [b_DT], bias=rowp[:, n, h:h + 1])
                            yield
                        psQ, b_psQ = psB.next()
                        for h in range(4):
                            mm(psQ[:, h * 128:(h + 1) * 128], KTt[:, h * 128:(h + 1) * 128], QTt[:, h * 128:(h + 1) * 128], True, True, [b_KTt, b_QTt], [b_psQ])
                        QKT, b_QKT = Bd["QKT"]
                        tt("dve", QKT[:, :], psQ[:, :], DT[:, :], ALU.mult, [b_psQ, b_DT], [b_QKT])
                        yield
                        ob, b_ob = Bd["ob"]
                        St, b_St = state[d]
                        Sb, b_Sb = stateb[d]
                        if mixer == "d":
                            psK, b_psK = psB.next()
                            for h in range(4):
                                mm(psK[:, h * 128:(h + 1) * 128], KTt[:, h * 128:(h + 1) * 128], KbT[:, h * 128:(h + 1) * 128], True, True, [b_KTt, b_KbT], [b_psK])
                            ai = 0
                            AT, b_AT = Bd["AT"][0]
                            A, b_A = Bd["A"][0]
                            tt("dve", AT[:, :], psK[:, :], DT[:, :], ALU.mult, [b_psK, b_DT], [b_AT])
                            tt("dve", AT[:, :], AT[:, :], offd[:, :], ALU.mult, [b_AT, b_offd], [b_AT])
                            yield
                            psT, b_psT = psA.next()
                            for h in range(4):
                                S.op("pe", (lambda psT=psT, h=h, AT=AT: lambda e: e.transpose(out=psT[:, h * 128:(h + 1) * 128], in_=AT[:, h * 128:(h + 1) * 128], identity=ident[:]))(),
                                     [b_AT, b_ident], [b_psT])
                            cp("act", A[:, :], psT[:, :], [b_psT], [b_A])
                            yield
                            ri = 0
                            for lev in range(6):
                                p1, b_p1 = psB.next()
                                p2, b_p2 = psB.next()
                                for h in range(4):
                                    pp, b_pp = (p1, b_p1) if h < 2 else (p2, b_p2)
                                    mm(pp[:, (h % 2) * 256:(h % 2) * 256 + 256], AT[:, h * 128:(h + 1) * 128], R[:, h, :], True, True, [b_AT, b_R], [b_pp])
                                ri = (ri + 1) % 3
                                Rn, b_Rn = Bd["R"][ri]
                                op = ALU.subtract if lev == 0 else ALU.add
                                tt("dve", Rn[:, 0:2, :], R[:, 0:2, :], p1[:, :].rearrange("p (h e) -> p h e", h=2), op, [b_R, b_p1], [b_Rn])
                                tt("dve", Rn[:, 2:4, :], R[:, 2:4, :], p2[:, :].rearrange("p (h e) -> p h e", h=2), op, [b_R, b_p2], [b_Rn])
                                R, b_R = Rn, b_Rn
                                yield
                                if lev < 5:
                                    pA, b_pA = psA.next()
                                    pT, b_pT = psA.next()
                                    for h in range(4):
                                        hs = slice(h * 128, (h + 1) * 128)
                                        mm(pA[:, hs], AT[:, hs], A[:, hs], True, True, [b_AT, b_A], [b_pA])
                                        mm(pT[:, hs], A[:, hs], AT[:, hs], True, True, [b_AT, b_A], [b_pT])
                                    ai = (ai + 1) % 3
                                    A2, b_A2 = Bd["A"][ai]
                                    AT2, b_AT2 = Bd["AT"][ai]
                                    cp("act", A2[:, :], pA[:, :], [b_pA], [b_A2])
                                    cp("dve", AT2[:, :], pT[:, :], [b_pT], [b_AT2])
                                    A, b_A, AT, b_AT = A2, b_A2, AT2, b_AT2
                                    yield
                            wk, b_wk = Bd["wk"]
                            cp("dve", h4(wk[:, :]), R[:, :, 128:256], [b_R], [b_wk])
                            WkT, b_WkT = Bd["WkT"]
                            transpose4(wk, b_wk, WkT, b_WkT)
                            yield
                        while done_seq[d] < kseq:
                            yield
                        for ci, c in enumerate((0, 1) if d == 0 else (1, 0)):
                            P = slice(c * 64, c * 64 + 64)
                            if mixer == "d":
                                psU, b_psU = psB.next()
                                for h in range(4):
                                    mm(psU[:, h * 128:(h + 1) * 128], WkT[:, h * 128:(h + 1) * 128], Sb[:, h, 0:128], True, True, [b_WkT, b_Sb], [b_psU])
                                Ut, b_Ut = Bd["U"][ci]
                                tt("dve", Ut[P, :].rearrange("p (h e) -> p h e", h=4), R[P, :, 0:128], psU[P, :].rearrange("p (h e) -> p h e", h=4),
                                   ALU.subtract, [b_R, b_psU], [b_Ut])
                                rhsU = (lambda Ut=Ut, P=P: lambda h: Ut[P, h * 128:(h + 1) * 128])()
                                b_rhs = b_Ut
                                yield
                            else:
                                rhsU = (lambda v1=v1, P=P: lambda h: v1[P, h, 0:VW])()
                                b_rhs = b_v1
                            po1, b_po1 = psB.next()
                            po2, b_po2 = psB.next()
                            ps1, b_ps1 = psA.next()
                            ps2, b_ps2 = psA.next()
                            for h in range(4):
                                po, b_po = (po1, b_po1) if h < 2 else (po2, b_po2)
                                o_ap = po[:, (h % 2) * 256:(h % 2) * 256 + VW]
                                mm(o_ap, QtT[:, h * 128:(h + 1) * 128], Sb[:, h, 0:VW], True, False, [b_QtT, b_Sb], [b_po])
                                mm(o_ap, QKT[P, h * 128:(h + 1) * 128], rhsU(h), False, True, [b_QKT, b_rhs], [b_po])
                            for h in range(4):
                                pss, b_pss = (ps1, b_ps1) if h < 2 else (ps2, b_ps2)
                                mm(pss[:, (h % 2) * 256:(h % 2) * 256 + VW], ke[P, h * 128:(h + 1) * 128], rhsU(h), True, True, [b_ke, b_rhs], [b_pss])
                            for h in range(4):
                                pss, b_pss = (ps1, b_ps1) if h < 2 else (ps2, b_ps2)
                                stt(St[:, h, 0:VW], St[:, h, 0:VW], gam[:, n, c * 4 + h:c * 4 + h + 1], pss[:, (h % 2) * 256:(h % 2) * 256 + VW],
                                    ALU.mult, ALU.add, [b_St, b_gam, b_pss], [b_St])
                            cp("act", Sb[:, :, 0:VW], St[:, :, 0:VW], [b_St], [b_Sb])
                            for hp in range(2):
                                po, b_po = (po1, b_po1) if hp == 0 else (po2, b_po2)
                                po3 = po[:, :].rearrange("p (h e) -> p h e", h=2)
                                o3 = ob[:, hp * 256:(hp + 1) * 256].rearrange("p (h e) -> p h e", h=2)
                                if mixer == "m":
                                    st2, b_st2 = st_p.next()
                                    act(st2[P, 0:2], po3[P, :, 128], AF.Abs, [b_po], [b_st2])
                                    ts("dve", st2[P, 0:2], st2[P, 0:2], 1.0, None, ALU.max, None, [b_st2], [b_st2])
                                    recip(st2[P, 0:2], st2[P, 0:2], [b_st2], [b_st2])
                                    tt("dve", o3[P], po3[P, :, 0:128], st2[P, 0:2].unsqueeze(2).to_broadcast([64, 2, 128]), ALU.mult, [b_po, b_st2], [b_ob])
                                else:
                                    cp("act", o3[P], po3[P, :, 0:128], [b_po], [b_ob])
                            yield
                        done_seq[d] += 1
                        dma(OUT[d][rows, :], ob[:, :], [b_ob], [DB("O%s%d" % (mixer, d), n)], q="act")

                    done_seq = [0, 0]
                    order = []
                    for i in range(NT):
                        order.append((fwd_order[i], 0, i))
                        order.append((bwd_order[i], 1, i))
                    active = []
                    free = list(range(KSLOT))
                    nxt = 0
                    while nxt < len(order) or active:
                        while free and nxt < len(order):
                            si = free.pop(0)
                            n_, d_, k_ = order[nxt]
                            nxt += 1
                            active.append((si, unit(n_, d_, slots[si], k_)))
                        still = []
                        for si, g_ in active:
                            try:
                                next(g_)
                                still.append((si, g_))
                            except StopIteration:
                                free.append(si)
                        active = still
                    S.barrier()

            with contextlib.ExitStack() as es:
                wo, b_wo = sb("c_wo", [128, KC, D], BF16, es=es)
                hw, b_hw = sb("c_hw", [128, 1536], es=es)
                gp = [sb("c_gp%d" % w, [128, D], es=es) for w in range(2)]
                wov = w_out[l].rearrange("(k p) n -> p k n", p=128)
                with contextlib.ExitStack() as es2:
                    wst_p = TPool(es2, nc, "c_wst", [128, 4, 512], F32, 2)
                    for k4 in range(0, KC, 4):
                        for cbk in range(4):
                            wst, b_wst = wst_p.next()
                            dma(wst[:, :, :], wov[:, k4:k4 + 4, cbk * 512:(cbk + 1) * 512], [], [b_wst])
                            cp("pool" if cbk % 2 else "dve", wo[:, k4:k4 + 4, cbk * 512:(cbk + 1) * 512], wst[:, :, :], [b_wst], [b_wo])
                    dma(hw[:], hnw[l:l + 1, :].partition_broadcast(128), [], [b_hw])
                    for w in range(2):
                        dma(gp[w][0][:], GPW[l * 2 + w:l * 2 + w + 1, :].partition_broadcast(128), [DB("GPW", l)], [gp[w][1]])
                    S.barrier()
                sq_p = TPool(es, nc, "c_sq", [128, 512], F32, 3)
                st_p = TPool(es, nc, "c_st", [128, 8], F32, 6)
                ps_p = TPool(es, nc, "c_ps", [128, 512], F32, 4, psum=True)
                po_p = TPool(es, nc, "c_po", [128, 512], F32, 4, psum=True)
                cslots = []
                for si in range(2):
                    Bc = {}
                    Bc["z"] = sb("c%d_z" % si, [128, 2560], BF16, es=es)
                    Bc["zf"] = sb("c%d_zf" % si, [128, 2560], F32, es=es)
                    Bc["o"] = [sb("c%d_o%d" % (si, j), [128, 512], F32, es=es) for j in range(7)]
                    Bc["y"] = sb("c%d_y" % si, [128, D], F32, es=es)
                    Bc["yT"] = sb("c%d_yT" % si, [128, KC, 128], BF16, es=es)
                    Bc["x"] = sb("c%d_x" % si, [128, D], F32, es=es)
                    cslots.append(Bc)

                def ctile(n, Bc):
                    which = 1 if n < NCT else 0
                    rows = slice(n * 128, (n + 1) * 128)
                    z, b_z = Bc["z"]
                    zf, b_zf = Bc["zf"]
                    y, b_y = Bc["y"]
                    yT, b_yT = Bc["yT"]
                    xt, b_xt = Bc["x"]
                    dma(z[:, 0:1024], U[rows, OFF["m_o"]:OFF["m_o"] + 1024], [DB("U", n)], [b_z])
                    dma(z[:, 1024:1536], U[rows, OFF["r_z"]:OFF["r_z"] + 512], [DB("U", n)], [b_z])
                    dma(z[:, 1536:2048], U[rows, OFF["a_z"]:OFF["a_z"] + 512], [DB("U", n)], [b_z])
                    dma(z[:, 2048:2560], U[rows, OFF["d_z"]:OFF["d_z"] + 512], [DB("U", n)], [b_z])
                    obufs = {}
                    oi = 0
                    for mx, OUTS in (("m", OM), ("r", OR), ("d", OD)):
                        for d_ in range(2):
                            o_, b_o = Bc["o"][oi]
                            oi += 1
                            dma(o_[:, :], OUTS[d_][rows, :], [DB("O%s%d" % (mx, d_), n)], [b_o])
                            obufs[(mx, d_)] = (o_, b_o)
                    o_, b_o = Bc["o"][6]
                    dma(o_[:, :], OA[rows, :], [DB("OA", n)], [b_o])
                    obufs[("a", 0)] = (o_, b_o)
                    src, sr = xsrc(n)
                    dma(xt[:], src, sr, [b_xt])
                    yield
                    act(zf[:, 0:512], z[:, 0:512], AF.Sigmoid, [b_z], [b_zf])
                    act(zf[:, 512:2560], z[:, 512:2560], AF.Silu, [b_z], [b_zf])
                    yield
                    for gi, mx in enumerate(("m", "r", "a", "d")):
                        ycol = y[:, gi * 512:(gi + 1) * 512]
                        if mx == "a":
                            o0, b_o0 = obufs[("a", 0)]
                            tt("dve", ycol, o0[:, :], zf[:, 1536:2048], ALU.mult, [b_o0, b_zf], [b_y])
                            continue
                        o0, b_o0 = obufs[(mx, 0)]
                        o1, b_o1 = obufs[(mx, 1)]
                        st, b_st = st_p.next()
                        tt("dve", o0[:, :], o0[:, :], o1[:, :], ALU.add, [b_o0, b_o1], [b_o0])
                        if mx == "m":
                            tt("pool", o0[:, :], o0[:, :], zf[:, 0:512], ALU.mult, [b_o0, b_zf], [b_o0])
                        sq, b_sq = sq_p.next()
                        tt("dve", sq[:, :], o0[:, :], o0[:, :], ALU.mult, [b_o0], [b_sq])
                        red(st[:, 0:4], sq[:, :].rearrange("p (h d) -> p h d", h=4), [b_sq], [b_st])
                        yield
                        rsqrt_inplace(st[:, 0:4], 1.0 / 128, EPS, [b_st])
                        yield
                        hoff = dict(m=0, r=512, d=1024)[mx]
                        zoff = dict(m=512, r=1024, d=2048)[mx]
                        tt("dve", o0[:, :].rearrange("p (h d) -> p h d", h=4), o0[:, :].rearrange("p (h d) -> p h d", h=4),
                           st[:, 0:4].unsqueeze(2).to_broadcast([128, 4, 128]), ALU.mult, [b_o0, b_st], [b_o0])
                        tt("dve", o0[:, :], o0[:, :], hw[:, hoff:hoff + 512], ALU.mult, [b_o0, b_hw], [b_o0])
                        tt("dve", ycol, o0[:, :], zf[:, zoff:zoff + 512], ALU.mult, [b_o0, b_zf], [b_y])
                        yield
                    for k4 in range(4):
                        ps, b_ps = ps_p.next()
                        for j in range(4):
                            k = k4 * 4 + j
                            S.op("pe", (lambda ps=ps, j=j, k=k, y=y: lambda e: e.transpose(out=ps[:, j * 128:(j + 1) * 128], in_=y[:, k * 128:(k + 1) * 128], identity=ident[:]))(),
                                 [b_y, b_ident], [b_ps])
                        cp("act", yT[:, k4 * 4:k4 * 4 + 4, :], ps[:, :].rearrange("p (k t) -> p k t", k=4), [b_ps], [b_yT])
                    yield
                    pos = [po_p.next() for _ in range(4)]
                    for cbk in range(4):
                        po, b_po = pos[cbk]
                        for k in range(KC):
                            mm(po[:, :], yT[:, k, :], wo[:, k, cbk * 512:(cbk + 1) * 512], k == 0, k == KC - 1, [b_yT, b_wo], [b_po])
                    st2, b_st2 = st_p.next()
                    for cbk in range(4):
                        po, b_po = pos[cbk]
                        sq, b_sq = sq_p.next()
                        act(sq[:, :], po[:, :], AF.Square, [b_po], [b_sq, b_st2], accum=st2[:, cbk:cbk + 1])
                    red(st2[:, 4:5], st2[:, 0:4], [b_st2], [b_st2])
                    rsqrt_inplace(st2[:, 4:5], 1.0 / D, EPS, [b_st2])
                    for cbk in range(4):
                        po, b_po = pos[cbk]
                        cs = slice(cbk * 512, (cbk + 1) * 512)
                        sq, b_sq = sq_p.next()
                        stt(sq[:, :], po[:, :], st2[:, 4:5], gp[which][0][:, cs], ALU.mult, ALU.mult, [b_po, b_st2, gp[which][1]], [b_sq])
                        tt("pool", xt[:, cs], xt[:, cs], sq[:, :], ALU.add, [b_xt, b_sq], [b_xt])
                    if last:
                        dma(y_out[(n - NCT) * 128:(n - NCT + 1) * 128, :], xt[:], [b_xt], [DB("Y", n)], q="pool")
                    else:
                        dma(X1[rows, :], xt[:], [b_xt], [DB("X1", n)], q="pool")

                tiles = list(range(NCT, NT)) if last else list(range(NT))
                pend = list(tiles)
                cact = []
                cfree = [0, 1]
                tick = 0
                while pend or cact:
                    if pend and cfree and (not cact or tick >= 5):
                        si = cfree.pop(0)
                        cact.append((si, ctile(pend.pop(0), cslots[si])))
                    still = []
                    for si, g_ in cact:
                        try:
                            next(g_)
                            still.append((si, g_))
                        except StopIteration:
                            cfree.append(si)
                    cact = still
                    tick += 1
                S.barrier()

            if debug and l == 0:
                with contextlib.ExitStack() as es:
                    bt, b_bt = sb("dbg_t", [128, 2048], es=es)
                    btb, b_btb = sb("dbg_tb", [128, 2048], BF16, es=es)
                    for n in range(NT):
                        rows = slice(n * 128, (n + 1) * 128)
                        for c in range(4):
                            dma(btb[:, :], U[rows, c * 2048:(c + 1) * 2048], [], [b_btb], q="sp")
                            dma(dbg["U"][rows, c * 2048:(c + 1) * 2048], btb[:, :], [b_btb], [DB("dbgU", n)], q="sp")
                        for nm, src in (("UG", UG), ("OM0", OM[0]), ("OM1", OM[1]), ("OR0", OR[0]), ("OR1", OR[1]), ("OD0", OD[0]), ("OD1", OD[1]), ("OA", OA), ("X1", X1)):
                            w_ = src.shape[1]
                            dma(bt[:, 0:w_], src[rows, :], [], [b_bt], q="sp")
                            dma(dbg[nm][rows, :], bt[:, 0:w_], [b_bt], [DB("dbg" + nm, n)], q="sp")
                    S.barrier()

        S.barrier()
        S.emit()
    return nc


def _consts(NLT):
    idx = np.arange(128)
    same = (idx[:, None] // 64) == (idx[None, :] // 64)
    tri0 = (same & (idx[:, None] <= idx[None, :])).astype(np.float32)
    tri1 = (same & (idx[:, None] >= idx[None, :])).astype(np.float32)
    tri = np.stack([np.tile(tri0, (1, 4)), np.tile(tri1, (1, 4))]).astype(np.float32)
    mbig = ((tri - 1.0) * BIG).astype(np.float32)
    blk = same.astype(np.float32)
    chm = np.zeros((128, 8), np.float32)
    chm[:64, 0:4] = 1.0
    chm[64:, 4:8] = 1.0
    offd = np.tile(1.0 - np.eye(128, dtype=np.float32), (1, 4)).astype(np.float32)
    sel = np.zeros((2, 2, 128), np.float32)
    sel[0, 0, :] = 1.0
    sel[1, 1, :] = 1.0
    L = NLT * 128
    t = np.arange(L)
    row, col = t // 64, t % 64
    inv = (10000.0 ** (-np.arange(32, dtype=np.float32) / 32)).astype(np.float32)
    ang = np.stack([row[:, None].astype(np.float32) * inv, col[:, None].astype(np.float32) * inv], axis=1)
    cos, sin = np.cos(ang).astype(np.float32), np.sin(ang).astype(np.float32)
    cf = np.zeros((L, 2, 2, 32), np.float32)
    sf = np.zeros((L, 2, 2, 32), np.float32)
    cf[:, :, 0, :] = cos
    cf[:, :, 1, :] = cos
    sf[:, :, 0, :] = -sin
    sf[:, :, 1, :] = sin
    return dict(c_ident=np.eye(128, dtype=np.float32), c_ones=np.ones((128, 128), np.float32), c_tri=tri, c_mbig=mbig,
                c_blk=blk, c_chm=chm, c_offd=offd, c_sel=sel, ropec=cf.reshape(L, 128), ropes=sf.reshape(L, 128))


_PROG = {}


def run(inputs, NCT, NLT, DEPTH, debug=False, n_cores=8):
    key = (NCT, NLT, DEPTH, debug)
    if key not in _PROG:
        _PROG[key] = build_program(NCT, NLT, DEPTH, debug)
    nc = _PROG[key]
    f = lambda a: np.ascontiguousarray(np.asarray(a, dtype=np.float32))
    B = inputs["x"].shape[0]
    shared = dict(
        ada_w=f(inputs["ada_w"]), ada_b=f(inputs["ada_b"]), pre_w=f(inputs["pre_norm_w"]), post_w=f(inputs["post_norm_w"]),
        w_in=f(inputs["w_in"]), w_out=f(inputs["w_out"]),
        mib=f(inputs["mlstm_i_bias"]).reshape(DEPTH, 8), mfb=f(inputs["mlstm_f_bias"]).reshape(DEPTH, 8),
        rlg=f(inputs["ret_log_gamma"]).reshape(DEPTH, 8), qnw=f(inputs["attn_q_norm_w"]), knw=f(inputs["attn_k_norm_w"]),
        convw=f(inputs["dn_conv_w"]), alog=f(inputs["dn_a_log"]).reshape(DEPTH, 8), dtb=f(inputs["dn_dt_bias"]).reshape(DEPTH, 8),
        hnw=f(inputs["head_norm_w"]))
    shared.update(_consts(NLT))
    cc = f(inputs["c_ctx"]).reshape(16, 128)
    in_maps = []
    for i in range(n_cores):
        b = i % B
        m = dict(shared)
        m["x"] = f(inputs["x"][b])
        m["ctx"] = f(inputs["ctx"][b])
        m["cvec"] = np.ascontiguousarray(np.concatenate([f(inputs["c"][b]).reshape(16, 128), cc], axis=0))
        in_maps.append(m)
    res = run_bass_kernel_spmd(nc, in_maps, core_ids=list(range(n_cores)))
    return res


def kernel(**inputs):
    res = run(inputs, 2, 32, 2)
    B = inputs["x"].shape[0]
    out = np.stack([np.asarray(res.results[b]["y"], dtype=np.float32) for b in range(B)], axis=0)
    return out
```
